# Optimizing a Trainium2 kernel written in Bass

```python
import math
import jax, jax.numpy as jnp
from jax import lax
import numpy as np

D_MODEL = 4096
BATCH = 2
SEQ = 4096
DEPTH = 4

CHUNK = 64
PLE_DIM = 256

D_MIX = D_MODEL
D_DELTA = D_MIX // 2
D_RWKV = D_MIX - D_DELTA
GDN_HEAD_DIM = 128
GDN_HEADS = D_DELTA // GDN_HEAD_DIM
GDN_CONV = 4
RWKV_HEAD_DIM = 64
RWKV_HEADS = D_RWKV // RWKV_HEAD_DIM
DECAY_LORA = 96
AAA_LORA = 96
GATE_LORA = 64
RWKV_LNX_EPS = 64e-5

GDN_COLS = 4 * D_DELTA + 2 * GDN_HEADS
RWKV_COLS = 3 * D_RWKV + DECAY_LORA + AAA_LORA + GATE_LORA
IN_COLS = GDN_COLS + RWKV_COLS

N_GROUPS = 4
EXPERTS_PER_GROUP = 8
N_EXPERTS = N_GROUPS * EXPERTS_PER_GROUP
TOP_K_IN_GROUP = 2
D_EXPERT = 256

DN_ALPHA = (2 * DEPTH) ** 0.25
DN_BETA = (8 * DEPTH) ** -0.25

kernel_name = 'hybrid_gdn_rwkv7_hier_moe_deepnorm'


def _split(h, sizes):
    parts, start = [], 0
    for s in sizes:
        parts.append(h[..., start:start + s])
        start += s
    return parts


def _l2norm(t, eps=1e-6):
    return t * lax.rsqrt(jnp.sum(t * t, axis=-1, keepdims=True) + eps)


def _layer_norm(x, g, b, eps=1e-5):
    xf = x.astype(jnp.float32)
    xc = xf - jnp.mean(xf, axis=-1, keepdims=True)
    var = jnp.mean(xc * xc, axis=-1, keepdims=True)
    y = xc * lax.rsqrt(var + eps) * g.astype(jnp.float32) + b.astype(jnp.float32)
    return y.astype(x.dtype)


def _causal_depthwise_conv(x, w):
    K, T = w.shape[0], x.shape[1]
    xp = jnp.pad(x, ((0, 0), (K - 1, 0), (0, 0)))
    y = xp[:, 0:T] * w[0]
    for j in range(1, K):
        y = y + xp[:, j:j + T] * w[j]
    return y


def _chunk_gated_delta_rule(q, k, v, g, beta):
    B, T, H, K = q.shape
    V = v.shape[-1]
    N = T // CHUNK

    def blocks(t):
        t = t.reshape((B, N, CHUNK, H) + t.shape[3:])
        return jnp.moveaxis(t, 3, 1)

    q, k, v, g, beta = blocks(q), blocks(k), blocks(v), blocks(g), blocks(beta)
    g = jnp.cumsum(g, axis=-1)
    idx = jnp.arange(CHUNK)
    causal = idx[:, None] >= idx[None, :]
    strict = idx[:, None] > idx[None, :]
    decay = jnp.exp(jnp.where(causal, g[..., :, None] - g[..., None, :], -jnp.inf))
    k_beta = k * beta[..., None]
    a_low = jnp.where(strict, jnp.einsum('bhnik,bhnjk->bhnij', k_beta, k) * decay, 0.0)
    tmat = a_low + jnp.eye(CHUNK, dtype=a_low.dtype)
    rhs = jnp.concatenate([v * beta[..., None], k_beta * jnp.exp(g)[..., None]], axis=-1)
    sol = lax.linalg.triangular_solve(tmat, rhs, left_side=True, lower=True, unit_diagonal=True)
    u, w = sol[..., :V], sol[..., V:]
    attn = jnp.einsum('bhnik,bhnjk->bhnij', q, k) * decay
    q_dec = q * jnp.exp(g)[..., None]
    k_dec = k * jnp.exp(g[..., -1:] - g)[..., None]
    g_last = jnp.exp(g[..., -1])

    def step(S, inp):
        q_i, k_i, u_i, w_i, a_i, gl_i = inp
        v_new = u_i - jnp.einsum('bhck,bhkv->bhcv', w_i, S)
        o = jnp.einsum('bhck,bhkv->bhcv', q_i, S) + jnp.einsum('bhij,bhjv->bhiv', a_i, v_new)
        S = S * gl_i[..., None, None] + jnp.einsum('bhck,bhcv->bhkv', k_i, v_new)
        return S, o

    xs = tuple(jnp.moveaxis(t, 2, 0) for t in (q_dec, k_dec, u, w, attn, g_last))
    S0 = jnp.zeros((B, H, K, V), q.dtype)
    _, o = lax.scan(step, S0, xs)
    o = jnp.moveaxis(o, 0, 2).reshape(B, H, T, V)
    return jnp.transpose(o, (0, 2, 1, 3))


def _gated_deltanet_group(q, k, v, z, a, b, conv_w, a_log, dt_bias, norm_w):
    B, T, _ = q.shape
    f32 = jnp.float32
    qkv = jax.nn.silu(_causal_depthwise_conv(jnp.concatenate([q, k, v], axis=-1), conv_w))
    q, k, v = (t.astype(f32).reshape(B, T, GDN_HEADS, GDN_HEAD_DIM) for t in _split(qkv, [D_DELTA] * 3))
    q = _l2norm(q) * (GDN_HEAD_DIM ** -0.5)
    k = _l2norm(k)
    g = -jnp.exp(a_log.astype(f32)) * jax.nn.softplus(a.astype(f32) + dt_bias.astype(f32))
    beta = jax.nn.sigmoid(b.astype(f32))
    o = _chunk_gated_delta_rule(q, k, v, g, beta)
    o = o * lax.rsqrt(jnp.mean(o * o, axis=-1, keepdims=True) + 1e-6) * norm_w.astype(f32)
    o = o.reshape(B, T, D_DELTA) * jax.nn.silu(z.astype(f32))
    return o.astype(z.dtype)


def _rwkv7_recurrence(r, w, k, v, a, b):
    B, T, H, N = r.shape

    def step(S, inp):
        r_t, w_t, k_t, v_t, a_t, b_t = inp
        sa = jnp.einsum('bhij,bhj->bhi', S, a_t)
        S = S * w_t[:, :, None, :] + sa[..., None] * b_t[:, :, None, :] + v_t[..., None] * k_t[:, :, None, :]
        return S, jnp.einsum('bhij,bhj->bhi', S, r_t)

    xs = tuple(jnp.moveaxis(t, 1, 0) for t in (r, w, k, v, a, b))
    S0 = jnp.zeros((B, H, N, N), r.dtype)
    _, y = lax.scan(step, S0, xs)
    return jnp.moveaxis(y, 0, 1)


def _rwkv7_group(h, mu, w0, w2, a0, a2, g2, k_k, k_a, r_k, lnx_g, lnx_b):
    B, T, _ = h.shape
    f32 = jnp.float32
    h_prev = jnp.pad(h, ((0, 0), (1, 0), (0, 0)))[:, :-1]
    h = h + mu * (h_prev - h)
    r, k, v, xw, xa, xg = _split(h, [D_RWKV] * 3 + [DECAY_LORA, AAA_LORA, GATE_LORA])
    w_log = -jax.nn.softplus(-(w0 + jnp.tanh(xw) @ w2).astype(f32)) - 0.5
    decay = jnp.exp(-jnp.exp(w_log))
    a = jax.nn.sigmoid((a0 + xa @ a2).astype(f32))
    g = jax.nn.sigmoid(xg) @ g2
    heads = lambda t: t.astype(f32).reshape(t.shape[:-1] + (RWKV_HEADS, RWKV_HEAD_DIM))
    rf, kf, vf, af, wf = heads(r), heads(k), heads(v), heads(a), heads(decay)
    kk = _l2norm(kf * heads(k_k))
    kf = kf * (1.0 + (af - 1.0) * heads(k_a))
    y = _rwkv7_recurrence(rf, wf, kf, vf, -kk, kk * af)
    yc = y - jnp.mean(y, axis=-1, keepdims=True)
    y = yc * lax.rsqrt(jnp.mean(yc * yc, axis=-1, keepdims=True) + RWKV_LNX_EPS)
    y = y * heads(lnx_g) + heads(lnx_b)
    y = y + jnp.sum(rf * kf * r_k.astype(f32), axis=-1, keepdims=True) * vf
    return (y.reshape(B, T, D_RWKV) * g.astype(f32)).astype(h.dtype)


def _hierarchical_moe(x, w_grp, b_grp, w_rt, b_rt, w_gate, w_up, w_down):
    B, T, _ = x.shape
    f32 = jnp.float32
    xf = x.astype(f32)
    grp_prob = jax.nn.softmax(xf @ w_grp.astype(f32) + b_grp.astype(f32), axis=-1)
    p_grp, g_sel = lax.top_k(grp_prob, 1)
    ex_logits = (xf @ w_rt.astype(f32) + b_rt.astype(f32)).reshape(B, T, N_GROUPS, EXPERTS_PER_GROUP)
    ex_in_grp = jnp.take_along_axis(ex_logits, g_sel[..., None], axis=2)[:, :, 0]
    top_v, top_i = lax.top_k(ex_in_grp, TOP_K_IN_GROUP)
    gate = jax.nn.softmax(top_v, axis=-1) * p_grp
    e_idx = g_sel * EXPERTS_PER_GROUP + top_i
    combine = jnp.sum(jax.nn.one_hot(e_idx, N_EXPERTS, dtype=f32) * gate[..., None], axis=-2)
    hg = jnp.einsum('btd,edf->btef', x, w_gate)
    hu = jnp.einsum('btd,edf->btef', x, w_up)
    hact = jax.nn.silu(hg) * hu * combine.astype(x.dtype)[..., None]
    return jnp.einsum('btef,efd->btd', hact, w_down)


def setup_inputs(seed: int = 0) -> dict:
    key = jax.random.key(seed)
    ks = jax.random.split(key, 40)
    f32 = jnp.float32
    L = DEPTH
    nrm = lambda kk, shape, s: jax.random.normal(kk, shape, f32) * s
    dt = jnp.exp(jax.random.uniform(ks[5], (L, GDN_HEADS), f32, math.log(1e-3), math.log(1e-1)))
    return {
        'x': nrm(ks[0], (BATCH, SEQ, D_MODEL), 1.0),
        'p': nrm(ks[1], (DEPTH, BATCH, SEQ, PLE_DIM), 1.0),
        'w_in': nrm(ks[2], (L, D_MODEL, IN_COLS), D_MODEL ** -0.5),
        'gdn_conv_w': nrm(ks[3], (L, GDN_CONV, 3 * D_DELTA), GDN_CONV ** -0.5),
        'gdn_a_log': jnp.log(jax.random.uniform(ks[4], (L, GDN_HEADS), f32, 1.0, 16.0)),
        'gdn_dt_bias': dt + jnp.log(-jnp.expm1(-dt)),
        'gdn_norm_w': 1.0 + nrm(ks[6], (L, GDN_HEAD_DIM), 0.02),
        'rwkv_mu': jax.random.uniform(ks[7], (L, RWKV_COLS), f32),
        'rwkv_w0': jax.random.uniform(ks[8], (L, D_RWKV), f32, -5.0, -1.0),
        'rwkv_w2': nrm(ks[9], (L, DECAY_LORA, D_RWKV), 0.5 * DECAY_LORA ** -0.5),
        'rwkv_a0': nrm(ks[10], (L, D_RWKV), 0.1),
        'rwkv_a2': nrm(ks[11], (L, AAA_LORA, D_RWKV), AAA_LORA ** -0.5),
        'rwkv_g2': nrm(ks[12], (L, GATE_LORA, D_RWKV), GATE_LORA ** -0.5),
        'rwkv_k_k': 0.85 + nrm(ks[13], (L, D_RWKV), 0.02),
        'rwkv_k_a': 1.0 + nrm(ks[14], (L, D_RWKV), 0.02),
        'rwkv_r_k': nrm(ks[15], (L, RWKV_HEADS, RWKV_HEAD_DIM), 0.1),
        'rwkv_lnx_g': 1.0 + nrm(ks[16], (L, D_RWKV), 0.02),
        'rwkv_lnx_b': nrm(ks[17], (L, D_RWKV), 0.02),
        'w_out': nrm(ks[18], (L, D_MIX, D_MODEL), D_MIX ** -0.5 * DN_BETA),
        'ln1_g': 1.0 + nrm(ks[19], (L, D_MODEL), 0.02),
        'ln1_b': nrm(ks[20], (L, D_MODEL), 0.02),
        'moe_w_grp': nrm(ks[21], (L, D_MODEL, N_GROUPS), D_MODEL ** -0.5),
        'moe_b_grp': nrm(ks[22], (L, N_GROUPS), 0.01),
        'moe_w_rt': nrm(ks[23], (L, D_MODEL, N_EXPERTS), D_MODEL ** -0.5),
        'moe_b_rt': nrm(ks[24], (L, N_EXPERTS), 0.01),
        'moe_w_gate': nrm(ks[25], (L, N_EXPERTS, D_MODEL, D_EXPERT), D_MODEL ** -0.5),
        'moe_w_up': nrm(ks[26], (L, N_EXPERTS, D_MODEL, D_EXPERT), D_MODEL ** -0.5),
        'moe_w_down': nrm(ks[27], (L, N_EXPERTS, D_EXPERT, D_MODEL), D_EXPERT ** -0.5 * DN_BETA),
        'ple_w_proj': nrm(ks[28], (L, PLE_DIM, D_MODEL), PLE_DIM ** -0.5 * DN_BETA),
        'ple_w_gate_down': nrm(ks[29], (L, D_MODEL, PLE_DIM), D_MODEL ** -0.5),
        'ple_w_gate_up': nrm(ks[30], (L, PLE_DIM, D_MODEL), PLE_DIM ** -0.5),
        'ln2_g': 1.0 + nrm(ks[31], (L, D_MODEL), 0.02),
        'ln2_b': nrm(ks[32], (L, D_MODEL), 0.02),
    }


def reference(x, p, w_in, gdn_conv_w, gdn_a_log, gdn_dt_bias, gdn_norm_w,
              rwkv_mu, rwkv_w0, rwkv_w2, rwkv_a0, rwkv_a2, rwkv_g2, rwkv_k_k, rwkv_k_a,
              rwkv_r_k, rwkv_lnx_g, rwkv_lnx_b, w_out, ln1_g, ln1_b,
              moe_w_grp, moe_b_grp, moe_w_rt, moe_b_rt, moe_w_gate, moe_w_up, moe_w_down,
              ple_w_proj, ple_w_gate_down, ple_w_gate_up, ln2_g, ln2_b):
    for i in range(DEPTH):
        h = x @ w_in[i]
        dq, dk, dv, dz, da, db, h_rwkv = _split(h, [D_DELTA] * 4 + [GDN_HEADS] * 2 + [RWKV_COLS])
        o_gdn = _gated_deltanet_group(dq, dk, dv, dz, da, db, gdn_conv_w[i], gdn_a_log[i],
                                      gdn_dt_bias[i], gdn_norm_w[i])
        o_rwkv = _rwkv7_group(h_rwkv, rwkv_mu[i], rwkv_w0[i], rwkv_w2[i], rwkv_a0[i], rwkv_a2[i],
                              rwkv_g2[i], rwkv_k_k[i], rwkv_k_a[i], rwkv_r_k[i],
                              rwkv_lnx_g[i], rwkv_lnx_b[i])
        mix = jnp.concatenate([o_gdn, o_rwkv], axis=-1) @ w_out[i]
        x = _layer_norm(DN_ALPHA * x + mix, ln1_g[i], ln1_b[i])
        ffn = _hierarchical_moe(x, moe_w_grp[i], moe_b_grp[i], moe_w_rt[i], moe_b_rt[i],
                                moe_w_gate[i], moe_w_up[i], moe_w_down[i])
        ple = (p[i] @ ple_w_proj[i]) * jax.nn.sigmoid((x @ ple_w_gate_down[i]) @ ple_w_gate_up[i])
        x = _layer_norm(DN_ALPHA * x + ffn + ple, ln2_g[i], ln2_b[i])
    return x
```

```python
import os

import contextlib
import numpy as np
import concourse.bass as bass
import concourse.mybir as mybir
from concourse.bass_utils import run_bass_kernel_spmd

F32 = mybir.dt.float32
BF16 = mybir.dt.bfloat16
AF = mybir.ActivationFunctionType
ALU = mybir.AluOpType
AX = mybir.AxisListType


class Buf:
    __slots__ = ("t", "last_w", "readers", "name")

    def __init__(self, t, name):
        self.t = t
        self.name = name
        self.last_w = None
        self.readers = []

    def __getitem__(self, idx):
        return self.t[idx]


class Op:
    __slots__ = ("eng", "fn", "deps", "sig", "semkey", "semval", "is_dma", "idx")


class Prog:
    NDMASEM = 6

    def __init__(self, nc):
        self.nc = nc
        self.ops = []
        self.stack = contextlib.ExitStack()
        self.dma_count = {}
        self.dma_ops = {}
        self.nbuf = 0

    def sb(self, shape, dtype=F32, name=None):
        self.nbuf += 1
        name = "s_" + (name or f"sb{self.nbuf}")
        t = self.stack.enter_context(self.nc.sbuf_tensor(name, list(shape), dtype))
        return Buf(t, name)

    def ps(self, shape, dtype=F32, name=None):
        self.nbuf += 1
        name = "p_" + (name or f"ps{self.nbuf}")
        t = self.stack.enter_context(self.nc.psum_tensor(name, list(shape), dtype))
        return Buf(t, name)

    def dram(self, name, shape, dtype=F32, kind="Internal"):
        t = self.nc.dram_tensor(name, list(shape), dtype, kind=kind)
        return Buf(t, name)

    def op(self, eng, fn, reads=(), writes=(), dma=False, ser=False):
        o = Op()
        o.eng = eng
        o.fn = fn
        o.is_dma = dma
        o.sig = False
        o.idx = len(self.ops)
        deps = set()
        for b in reads:
            if b is None:
                continue
            if b.last_w is not None:
                deps.add(b.last_w)
        for b in writes:
            if b.last_w is not None:
                deps.add(b.last_w)
            for r in b.readers:
                deps.add(r)
        if dma:
            n = self.dma_count.get(eng, 0)
            self.dma_count[eng] = n + 1
            lst = self.dma_ops.setdefault(eng, [])
            if n >= self.NDMASEM:
                deps.add(lst[n - self.NDMASEM])
            lst.append(o.idx)
            o.semkey = ("dma", eng, n % self.NDMASEM)
            o.semval = 16 * (n // self.NDMASEM + 1)
        else:
            o.semkey = ("eng", eng)
            o.semval = None
        pruned = set()
        for d in deps:
            od = self.ops[d]
            if (not od.is_dma) and od.eng == eng and eng == "pe" and not dma:
                continue
            pruned.add(d)
        if ser and getattr(self, "last_pe", None) is not None:
            pruned.add(self.last_pe)
        if eng == "pe" and not dma:
            self.last_pe = o.idx
        o.deps = pruned
        for b in reads:
            if b is not None:
                b.readers.append(o.idx)
        for b in writes:
            b.last_w = o.idx
            b.readers = []
        self.ops.append(o)
        return o

    def dma(self, q, out, in_, reads=(), writes=(), **kw):
        return self.op(q, lambda e: e.dma_start(out=out, in_=in_, **kw), reads, writes, dma=True)

    def mm(self, out_ap, lhsT, rhs, reads, writes, start=True, stop=True, ser=False, **kw):
        return self.op("pe", lambda e: e.matmul(out_ap, lhsT, rhs, start=start, stop=stop, **kw), reads, writes, ser=ser)

    def tr(self, out_ap, in_ap, ident_ap, reads, writes):
        return self.op("pe", lambda e: e.transpose(out_ap, in_ap, ident_ap), reads, writes)

    def act(self, out, in_, func, reads, writes, eng="act", **kw):
        return self.op(eng, lambda e: e.activation(out=out, in_=in_, func=func, **kw), reads, writes)

    def tt(self, eng, out, in0, in1, op, reads, writes):
        return self.op(eng, lambda e: e.tensor_tensor(out=out, in0=in0, in1=in1, op=op), reads, writes)

    def ts(self, eng, out, in0, s1, s2, op0, op1=None, reads=(), writes=(), **kw):
        if op1 is None:
            return self.op(eng, lambda e: e.tensor_scalar(out=out, in0=in0, scalar1=s1, scalar2=None, op0=op0, **kw), reads, writes)
        return self.op(eng, lambda e: e.tensor_scalar(out=out, in0=in0, scalar1=s1, scalar2=s2, op0=op0, op1=op1, **kw), reads, writes)

    def stt(self, eng, out, in0, scalar, in1, op0, op1, reads, writes):
        return self.op(eng, lambda e: e.scalar_tensor_tensor(out=out, in0=in0, scalar=scalar, in1=in1, op0=op0, op1=op1), reads, writes)

    def copy(self, eng, out, in_, reads, writes):
        if eng == "act":
            return self.op(eng, lambda e: e.activation(out=out, in_=in_, func=AF.Copy), reads, writes)
        return self.op(eng, lambda e: e.tensor_copy(out=out, in_=in_), reads, writes)

    def memset(self, eng, ap, val, writes):
        return self.op(eng, lambda e: e.memset(ap, val), (), writes)

    def emit(self, final_wait_ops=()):
        nc = self.nc
        ops = self.ops
        for o in ops:
            for d in o.deps:
                ops[d].sig = True
        for i in final_wait_ops:
            ops[i].sig = True
        cnt = {}
        for o in ops:
            if not o.is_dma and o.sig:
                cnt[o.eng] = cnt.get(o.eng, 0) + 1
                o.semval = cnt[o.eng]
        semkeys = sorted({o.semkey for o in ops if o.sig or o.is_dma}, key=str)
        sems = {}
        for k in semkeys:
            sems[k] = self.stack.enter_context(nc.semaphore("s_" + "_".join(str(x) for x in k)))
        waited = {}
        per_eng = {}
        for o in ops:
            need = {}
            for d in o.deps:
                od = ops[d]
                k = od.semkey
                if od.semval > need.get(k, 0):
                    need[k] = od.semval
            wl = []
            for k, v in need.items():
                if waited.get((o.eng, k), 0) >= v:
                    continue
                waited[(o.eng, k)] = v
                wl.append((k, v))
            per_eng.setdefault(o.eng, []).append((o, wl))
        fin = {}
        for i in final_wait_ops:
            od = ops[i]
            fin[od.semkey] = max(fin.get(od.semkey, 0), od.semval)
        self.stats = {e: len(l) for e, l in per_eng.items()}

        engmap = {"pe": "tensor", "act": "scalar", "dve": "vector", "pool": "gpsimd", "sp": "sync"}
        with nc.Block() as block:
            for ename, lst in per_eng.items():
                def body(eng, lst=lst, ename=ename):
                    for o, wl in lst:
                        for k, v in wl:
                            eng.wait_ge(sems[k], v)
                        ins = o.fn(eng)
                        if o.is_dma:
                            ins.then_inc(sems[o.semkey], 16)
                        elif o.sig:
                            ins.then_inc(sems[o.semkey], 1)
                    if ename == "sp":
                        for k, v in fin.items():
                            eng.wait_ge(sems[k], v)
                getattr(block, engmap[ename])(body)
            if "sp" not in per_eng and fin:
                def body2(eng):
                    for k, v in fin.items():
                        eng.wait_ge(sems[k], v)
                block.sync(body2)
        self.stack.close()


def view(ap, name="v"):
    return Buf(ap, name)


class SubBuf:
    __slots__ = ("p", "t", "name")

    def __init__(self, parent, ap, name="sub"):
        self.p = parent
        self.t = ap
        self.name = name

    @property
    def last_w(self):
        return self.p.last_w

    @last_w.setter
    def last_w(self, v):
        self.p.last_w = v

    @property
    def readers(self):
        return self.p.readers

    @readers.setter
    def readers(self, v):
        self.p.readers = v

    def __getitem__(self, idx):
        return self.t[idx]


D = 4096
T = 4096
NB = 256
NCH = NB // 64


def consts():
    k = np.arange(128)[:, None]
    i = np.arange(128)[None, :]
    c = {}
    c["ident"] = (k == i)
    c["Eblk"] = (k // 64) == (i // 64)
    s = k % 64
    t = i % 64
    c["MaskZ"] = np.where(i < 64, s < t, s <= t)
    c["ones"] = np.ones((128, 128))
    c["UU"] = np.where(i < 64, k < t, k <= t) & (k < 64)
    c["negUU"] = -1.0 * ((k <= t) & (k < 64))
    c["NegM"] = np.where(c["MaskZ"], 0.0, -30000.0)
    c["Dup"] = (k == t) & (k < 64)
    c["Urev"] = (k > i) & (k < 64) & (i < 64)
    c["Uinc"] = (k <= i) & (k < 64) & (i < 64)
    c["Dup1"] = (k == 64 + t)
    c["UblkI"] = ((k // 64) == (i // 64)) & (k <= i)
    c["SelL0"] = (k == 63) & (i >= 0)
    c["SelL1"] = (k == 127) & (i >= 0)
    c["sgn"] = np.where(i == 0, np.where(k < 64, -1.0, 0.0), np.where(k < 64, 0.0, 1.0))
    names = list(c.keys())
    arr = np.concatenate([np.asarray(c[n], dtype=np.float32) for n in names], axis=1)
    rmask = np.ones((128, NB), np.float32)
    rmask[:, ::64] = 0.0
    arr = np.concatenate([arr, rmask], axis=1)
    return names, np.ascontiguousarray(arr)


C_NAMES, C_ARR = consts()
NCST = C_ARR.shape[1]


def load_consts(P, cst_d):
    cst = P.sb([128, NCST], name="cst")
    P.dma("sp", cst[:], cst_d, writes=[cst])
    C = {n: cst[:, i * 128:(i + 1) * 128] for i, n in enumerate(C_NAMES)}
    C["rmask"] = cst[:, len(C_NAMES) * 128: len(C_NAMES) * 128 + NB]
    return cst, C


class CH:
    pass


class Engine:
    def __init__(self, P, n, cst, C, banks):
        self.P = P
        self.n = n
        self.cst = cst
        self.C = C
        bZ, bP, bQ, bY, bS = banks
        self.psZ = [SubBuf(bZ, bZ[:, j * 128:(j + 1) * 128], f"psZ{j}") for j in range(4)]
        self.psP = [SubBuf(bP, bP[0:64, j * 64:(j + 1) * 64], f"psP{j}") for j in range(8)]
        self.psQ = [SubBuf(bQ, bQ[0:64, j * 64:(j + 1) * 64], f"psQ{j}") for j in range(8)]
        self.psY = [SubBuf(bY, bY[0:64, j * 64:(j + 1) * 64], f"psY{j}") for j in range(8)]
        self.bS = bS
        self.Zm = [P.sb([128, 128], name=f"eZm{i}") for i in range(n)]
        self.Pm = [[P.sb([64, 64], name=f"eP{i}_{j}") for j in range(2)] for i in range(n)]
        self.Qm = [[P.sb([64, 64], name=f"eQ{i}_{j}") for j in range(2)] for i in range(n)]
        self.Ym = [[P.sb([64, 64], name=f"eY{i}_{j}") for j in range(2)] for i in range(n)]
        self.ZK = [P.sb([128, 64], name=f"eZK{i}") for i in range(n)]
        for i in range(n):
            P.memset("pool", self.ZK[i][:], 0.0, [self.ZK[i]])

    def phaseA(self, chs):
        P, C, cst = self.P, self.C, self.cst
        assert len(chs) <= self.n
        for i, ch in enumerate(chs):
            pz = self.psZ[i % 4]
            ch.Zm = self.Zm[i]
            P.mm(pz[:], ch.LT, ch.RT, ch.rd, [pz])
            P.tt("dve", ch.Zm[:], pz[:], ch.Dm, ALU.mult, [pz] + ch.Dm_rd, [ch.Zm])
            ch.ZK = self.ZK[i]
            P.copy("pool", ch.ZK[64:128, :], ch.Zm[64:128, 0:64], [ch.Zm], [ch.ZK])
        for i, ch in enumerate(chs):
            P.tr(self.psQ[i][:], ch.Zm[0:64, 0:64], C["ident"][0:64, 0:64], [ch.Zm, cst], [self.psQ[i]])
        for i, ch in enumerate(chs):
            P.copy("act", self.Qm[i][0][:], self.psQ[i][:], [self.psQ[i]], [self.Qm[i][0]])
            P.tt("pool", self.Ym[i][0][:], ch.Zm[0:64, 0:64], C["ident"][0:64, 0:64], ALU.add, [ch.Zm, cst], [self.Ym[i][0]])
        for k in range(1, 6):
            a, b = (k - 1) % 2, k % 2
            for i, ch in enumerate(chs):
                Pp = ch.Zm[0:64, 0:64] if k == 1 else self.Pm[i][a][:]
                Pb = ch.Zm if k == 1 else self.Pm[i][a]
                Qp = self.Qm[i][a]
                if k < 5:
                    P.mm(self.psP[i][:], Qp[:], Pp, [Qp, Pb], [self.psP[i]])
                P.mm(self.psQ[i][:], Pp, Qp[:], [Qp, Pb], [self.psQ[i]])
            for i, ch in enumerate(chs):
                P.copy("act", self.Qm[i][b][:], self.psQ[i][:], [self.psQ[i]], [self.Qm[i][b]])
                if k < 5:
                    P.copy("dve", self.Pm[i][b][:], self.psP[i][:], [self.psP[i]], [self.Pm[i][b]])
            for i, ch in enumerate(chs):
                P.mm(self.psY[i][:], self.Qm[i][b][:], self.Ym[i][a][:], [self.Qm[i][b], self.Ym[i][a]], [self.psY[i]])
            for i, ch in enumerate(chs):
                P.tt("dve", self.Ym[i][b][:], self.psY[i][:], self.Ym[i][a][:], ALU.add, [self.psY[i], self.Ym[i][a]], [self.Ym[i][b]])
        for i, ch in enumerate(chs):
            ch.Tt = self.Ym[i][1]


def scan_bufs(P, bS, nh, Nv):
    sb = []
    for h in range(nh):
        o = CH()
        o.Xs = P.sb([64, Nv], name=f"sXs{h}")
        o.Mt = P.sb([128, 128], name=f"sMt{h}")
        sb.append(o)
    return sb


PB = 99


def phaseB(P, chs, scb, bS_slots, groups):
    if PB < -1:
        return
    for i, ch in enumerate(chs):
        psX = bS_slots[i][0]
        Mr = ch.M[ch.Mrows, :]
        P.mm(psX[:], ch.AT, ch.M[:, :], ch.rd + [ch.M], [psX], start=True, stop=False)
        P.mm(psX[:], ch.ZK[:, :], ch.UV[:, ch.UVc], [ch.ZK, ch.UV], [psX], start=False, stop=True)
    if PB < 0:
        return
    for i, ch in enumerate(chs):
        psX = bS_slots[i][0]
        P.copy("act", scb[i].Xs[:], psX[:], [psX], [scb[i].Xs])
    if PB < 3:
        return
    for i, ch in enumerate(chs):
        psU = bS_slots[i][1]
        P.mm(psU[:], ch.Tt[:], scb[i].Xs[:], [ch.Tt, scb[i].Xs], [psU])
    for i, ch in enumerate(chs):
        psU = bS_slots[i][1]
        P.copy("dve", ch.UV[0:64, ch.UVc], psU[:], [psU], [ch.UV])
    if PB < 5:
        return
    for i, ch in enumerate(chs):
        Mr = ch.M[ch.Mrows, :]
        P.mm(ch.yout, ch.RrT, ch.M[:, :], ch.rd + [ch.M], [ch.yout_b], start=True, stop=False)
        P.mm(ch.yout, ch.Zm[:, 64:128], ch.UV[:, ch.UVc], [ch.Zm, ch.UV], [ch.yout_b], start=False, stop=True)
    if PB < 6:
        return
    for (BK, BK_rd, UVb, psM) in groups:
        P.mm(psM[:], BK, UVb[:], BK_rd + [UVb], [psM])
    if PB < 7:
        return
    for i, ch in enumerate(chs):
        psM = groups[ch.gid][3]
        Mr = ch.M[ch.Mrows, :]
        if ch.Mrows == slice(0, 128):
            P.copy("act", scb[i].Mt[:], psM[:], [psM], [scb[i].Mt])
            P.stt("dve", Mr, Mr, ch.gC, scb[i].Mt[:], ALU.mult, ALU.add, [ch.M, scb[i].Mt] + ch.gC_rd, [ch.M])
            continue
        else:
            P.act(Mr, Mr, AF.Copy, [ch.M] + ch.gC_rd, [ch.M], scale=ch.gC)
        P.tt("dve", Mr, Mr, psM[ch.Mblk[0], ch.Mblk[1]], ALU.add, [ch.M, psM], [ch.M])


def build_r(nc, nblk, NTOK, stage=99):
    xT = nc.dram_tensor("xT", [D, NTOK], F32, kind="ExternalInput").ap()
    wr = nc.dram_tensor("wr", [D, 1024], F32, kind="ExternalInput").ap()
    lora_d = nc.dram_tensor("lora", [128, 1024], F32, kind="ExternalInput").ap()
    prm_d = nc.dram_tensor("prm", [128, 24], F32, kind="ExternalInput").ap()
    cst_d = nc.dram_tensor("cst", [128, NCST], F32, kind="ExternalInput").ap()
    orw = nc.dram_tensor("orw", [256, NTOK], F32, kind="ExternalOutput").ap()
    P = Prog(nc)
    fin = []
    cst, C = load_consts(P, cst_d)
    prm = P.sb([128, 24], name="prm")
    P.dma("sp", prm[:], prm_d, writes=[prm])
    lora = P.sb([128, 1024], name="lora")
    P.dma("sp", lora[:], lora_d, writes=[lora])
    om = P.sb([128, 2], name="om")
    for fg in range(2):
        P.ts("dve", om[:, fg:fg + 1], prm[:, fg * 10 + 6:fg * 10 + 7], -1.0, 1.0, ALU.mult, ALU.add, reads=[prm], writes=[om])

    stg = [P.sb([128, 4, 256], F32, name=f"stg{i}") for i in range(2)]
    wrb = P.sb([128, 32, 4, 256], BF16, name="wrb")
    wr_v = wr.rearrange("(c p) (a n) -> p c a n", p=128, a=4)
    for g in range(32):
        st = stg[g % 2]
        P.dma("sp" if g % 2 == 0 else "pool", st[:], wr_v[:, g], writes=[st])
        P.copy("act" if g % 2 == 0 else "dve", wrb[:, g], st[:], [st], [wrb])

    if stage == 0:
        tmp0 = P.sb([128, 256], F32, name="tmp0")
        P.copy("act", tmp0[:], wrb[:, 31, 3, :], [wrb], [tmp0])
        fin.append(P.dma("sp", orw[0:128, 0:256], tmp0[:], reads=[tmp0]).idx)
        P.emit(final_wait_ops=fin)
        return nc, P
    xb_t = [P.sb([128, 32, NB], BF16, name=f"xb{i}") for i in range(2)]
    xb_v = [[view(xb_t[p][:, g * 4:(g + 1) * 4, :], f"xb{p}_{g}") for g in range(8)] for p in range(2)]
    xT_v = xT.rearrange("(c p) t -> p c t", p=128)

    CG = [(0, 0, 128), (0, 128, 128), (1, 0, 128), (1, 128, 128), (2, 0, 128), (2, 128, 128), (3, 0, 128), (3, 128, 128)]
    MUCOL = [0, 10, 1, 11, 2, 12, 20, 21]
    hbuf = [P.sb([128, NB + 1], name=f"hbuf{c}") for c in range(8)]
    hs = [P.sb([128, NB], name=f"hs{c}") for c in range(8)]
    dtmp = [P.sb([128, NB], name=f"dtmp{c}") for c in range(2)]

    banks = [P.ps([128, 512], name=f"bank{i}") for i in range(8)]
    psA = [SubBuf(banks[0], banks[0][:, 0:256], "psA0"), SubBuf(banks[1], banks[1][:, 0:256], "psA1")]
    psL = [SubBuf(banks[7], banks[7][:, 0:256], "psL0"), SubBuf(banks[7], banks[7][:, 256:512], "psL1")]
    eng = Engine(P, 8, cst, C, banks[2:7])
    bS = banks[6]
    bT_ = banks[2]
    psT = [SubBuf(bT_, bT_[:, j * 128:(j + 1) * 128], f"psT{j}") for j in range(4)]
    bS_slots = []
    for h in range(2):
        bS_slots.append([SubBuf(bS, bS[0:64, (h * 2 + 0) * 64:(h * 2 + 1) * 64], f"psX{h}"),
                         SubBuf(bS, bS[0:64, (h * 2 + 1) * 64:(h * 2 + 2) * 64], f"psU{h}")])
    psMg = SubBuf(bS, bS[:, 256:384], "psMg")
    psYc = [SubBuf(bS, bS[0:64, 384 + 0:384 + 128], "psYc")]
    scb = scan_bufs(P, bS, 2, 64)

    def mk(shape, name, dt=F32):
        return P.sb(shape, dt, name=name)
    txw = mk([128, NB], "txw"); sxg = mk([128, NB], "sxg")
    lw = mk([128, NB], "lw"); aS = mk([128, NB], "aS"); gT = mk([128, NB], "gT")
    kkr = mk([128, NB], "kkr"); sq = mk([128, NB], "sq"); rinv = mk([128, NB], "rinv"); kk = mk([128, NB], "kk")
    t1 = mk([128, NB], "t1"); kp = mk([128, NB], "kp"); bT = mk([128, NB], "bT"); rk = mk([128, NB], "rk")
    bonus = mk([128, NB], "bonus"); G = mk([128, NB], "G"); Gm = mk([128, NB], "Gm")
    eG = mk([128, NB], "eG"); eGm = mk([128, NB], "eGm"); eNG = mk([128, NB], "eNG"); eRev = mk([128, NB], "eRev")
    LT2 = mk([128, NCH, 128], "LT2"); RT2 = mk([128, NCH, 128], "RT2"); BKT2 = mk([128, NCH, 128], "BKT2"); VT2 = mk([128, NCH, 128], "VT2")
    P.memset("pool", VT2[:], 0.0, [VT2])
    LTz = [mk([128, NCH, 128], f"LTz{h}") for h in range(2)]
    RTz = [mk([128, NCH, 128], f"RTz{h}") for h in range(2)]
    BKh = [mk([128, 128], f"BKh{c}") for c in range(NCH)]
    UV = [mk([128, 128], f"UV{c}") for c in range(NCH)]
    for c in range(NCH):
        P.memset("pool", UV[c][:], 0.0, [UV[c]])
    Mst = [mk([128, 64], f"Mst{fg}") for fg in range(2)]
    Ytm = mk([64, 128], "Ytm"); yc = mk([64, 128], "yc"); junk = mk([64, 128], "junk"); yn = mk([64, 128], "yn")
    msum = mk([64, 2], "msum"); nmean = mk([64, 2], "nmean"); vs = mk([64, 2], "vs"); rstd = mk([64, 2], "rstd")
    fin1 = mk([128, 64], "fin1"); fin2 = mk([128, 64], "fin2")
    oblk = [mk([128, NB], f"oblk{fg}") for fg in range(2)]

    def c3(ap):
        return ap.rearrange("p (c t) -> p c t", c=NCH)

    for blk in range(nblk):
        par = blk % 2
        tok0 = blk * NB
        seq_start = (tok0 % T) == 0
        for g in range(8):
            st = stg[g % 2]
            P.dma("sp" if g % 2 == 0 else "pool", st[:], xT_v[:, g * 4:(g + 1) * 4, tok0:tok0 + NB], writes=[st])
            P.copy("pool", xb_v[par][g][:], st[:], [st], [xb_v[par][g]])
        xb = xb_t[par]
        xbr = xb_v[par]
        if seq_start:
            for fg in range(2):
                P.memset("pool", Mst[fg][:], 0.0, [Mst[fg]])
        for cg in range(8):
            a_, off, M = CG[cg]
            ps = psA[cg % 2]
            for dc in range(32):
                P.mm(ps[0:M, :], wrb[:, dc, a_, off:off + M], xb[:, dc, :], [wrb, xbr[dc // 4]], [ps], start=(dc == 0), stop=(dc == 31))
            hb = hbuf[cg]
            if seq_start:
                P.memset("pool", hb[:, 0:1], 0.0, [hb])
            P.copy("act", hb[0:M, 1:NB + 1], ps[0:M, :], [ps], [hb])
            dt_ = dtmp[cg % 2]
            P.tt("dve", dt_[0:M, :], hb[0:M, 0:NB], hb[0:M, 1:NB + 1], ALU.subtract, [hb], [dt_])
            mc = MUCOL[cg]
            P.stt("dve", hs[cg][0:M, :], dt_[0:M, :], prm[0:M, mc:mc + 1], hb[0:M, 1:NB + 1], ALU.mult, ALU.add, [dt_, prm, hb], [hs[cg]])
            P.copy("pool", hb[0:M, 0:1], hb[0:M, NB:NB + 1], [hb], [hb])
        if stage == 1:
            fin.append(P.dma("sp", orw[0:128, tok0:tok0 + NB], hs[0][:], reads=[hs[0]]).idx)
            fin.append(P.dma("sp", orw[128:256, tok0:tok0 + NB], hs[3][:], reads=[hs[3]]).idx)
            continue
        P.act(txw[:], hs[6][:], AF.Tanh, [hs[6]], [txw])
        P.act(sxg[:], hs[7][:], AF.Sigmoid, [hs[7]], [sxg])
        for fg in range(2):
            pb = fg * 10
            rT = hs[fg]; kT = hs[2 + fg]; vT = hs[4 + fg]
            fc = slice(fg * 128, (fg + 1) * 128)
            pl = psL[0]
            P.mm(pl[:], lora[:, fc], txw[:], [lora, txw], [pl])
            P.act(lw[:], pl[:], AF.Sigmoid, [pl, prm], [lw], bias=prm[:, pb + 3:pb + 4])
            P.ts("pool", lw[:], lw[:], -0.6065306597126334, None, ALU.mult, reads=[lw], writes=[lw])
            pl = psL[1]
            P.mm(pl[:], lora[:, 256 + fg * 128:256 + (fg + 1) * 128], hs[6][:], [lora, hs[6]], [pl], start=True, stop=False)
            P.mm(pl[:], lora[:, 512 + fg * 128:512 + (fg + 1) * 128], hs[7][:], [lora, hs[7]], [pl], start=False, stop=True)
            P.act(aS[:], pl[:], AF.Sigmoid, [pl, prm], [aS], bias=prm[:, pb + 4:pb + 5])
            pl = psL[0]
            P.mm(pl[:], lora[:, 768 + fg * 128:768 + (fg + 1) * 128], sxg[:], [lora, sxg], [pl])
            P.copy("act", gT[:], pl[:], [pl], [gT])
            P.ts("pool", kkr[:], kT[:], prm[:, pb + 5:pb + 6], None, ALU.mult, reads=[kT, prm], writes=[kkr])
            P.tt("pool", sq[:], kkr[:], kkr[:], ALU.mult, [kkr], [sq])
            pl = psL[1]
            P.mm(pl[:], C["Eblk"], sq[:], [cst, sq], [pl])
            P.ts("dve", rinv[:], pl[:], 1e-6, None, ALU.add, reads=[pl], writes=[rinv])
            P.act(rinv[:], rinv[:], AF.Ln, [rinv], [rinv])
            P.act(rinv[:], rinv[:], AF.Exp, [rinv], [rinv], scale=-0.5)
            P.tt("pool", kk[:], kkr[:], rinv[:], ALU.mult, [kkr, rinv], [kk])
            P.ts("dve", t1[:], aS[:], prm[:, pb + 6:pb + 7], om[:, fg:fg + 1], ALU.mult, ALU.add, reads=[aS, prm, om], writes=[t1])
            P.tt("pool", kp[:], t1[:], kT[:], ALU.mult, [t1, kT], [kp])
            P.tt("pool", bT[:], kk[:], aS[:], ALU.mult, [kk, aS], [bT])
            P.stt("dve", rk[:], rT[:], prm[:, pb + 7:pb + 8], kp[:], ALU.mult, ALU.mult, [rT, prm, kp], [rk])
            pl = psL[0]
            P.mm(pl[:], C["Eblk"], rk[:], [cst, rk], [pl])
            P.tt("dve", bonus[:], pl[:], vT[:], ALU.mult, [pl, vT], [bonus])
            P.op("dve", lambda e: e.tensor_tensor_scan(out=G[:], data0=C["rmask"], data1=lw[:], initial=0.0, op0=ALU.mult, op1=ALU.add),
                 [cst, lw], [G])
            P.tt("pool", Gm[:], G[:], lw[:], ALU.subtract, [G, lw], [Gm])
            P.act(eG[:], G[:], AF.Exp, [G], [eG])
            P.act(eGm[:], Gm[:], AF.Exp, [Gm], [eGm])
            P.act(eNG[:], G[:], AF.Exp, [G], [eNG], scale=-1.0)
            for c in range(NCH):
                cs_ = slice(c * 64, (c + 1) * 64)
                P.act(eRev[:, cs_], G[:, cs_], AF.Exp, [G], [eRev], scale=-1.0, bias=G[:, c * 64 + 63:c * 64 + 64])
            if stage == 2:
                dbg = {0: lw, 1: aS}[fg]
                fin.append(P.dma("sp", orw[fc, tok0:tok0 + NB], dbg[:], reads=[dbg]).idx)
                continue
            P.tt("dve", LT2[:, :, 0:64], c3(bT[:]), c3(eNG[:]), ALU.mult, [bT, eNG], [LT2])
            P.tt("pool", LT2[:, :, 64:128], c3(kp[:]), c3(eNG[:]), ALU.mult, [kp, eNG], [LT2])
            P.stt("dve", RT2[:, :, 0:64], c3(kk[:]), -1.0, c3(eGm[:]), ALU.mult, ALU.mult, [kk, eGm], [RT2])
            P.tt("pool", RT2[:, :, 64:128], c3(rT[:]), c3(eG[:]), ALU.mult, [rT, eG], [RT2])
            P.tt("dve", BKT2[:, :, 0:64], c3(bT[:]), c3(eRev[:]), ALU.mult, [bT, eRev], [BKT2])
            P.tt("pool", BKT2[:, :, 64:128], c3(kp[:]), c3(eRev[:]), ALU.mult, [kp, eRev], [BKT2])
            P.copy("pool", VT2[:, :, 64:128], c3(vT[:]), [vT], [VT2])
            for h in range(2):
                hm = C["Eblk"][:, h * 64:h * 64 + 1]
                P.ts("pool", LTz[h][:], LT2[:], hm, None, ALU.mult, reads=[LT2, cst], writes=[LTz[h]])
                P.ts("pool", RTz[h][:], RT2[:], hm, None, ALU.mult, reads=[RT2, cst], writes=[RTz[h]])
            for c in range(NCH):
                pt = psT[c % 4]
                P.tr(pt[:], BKT2[:, c, :], C["ident"], [BKT2, cst], [pt])
                P.copy("act", BKh[c][:], pt[:], [pt], [BKh[c]])
            for c in range(NCH):
                pt = psT[c % 4]
                P.tr(pt[:], VT2[:, c, :], C["ident"], [VT2, cst], [pt])
                P.copy("act", UV[c][64:128, :], pt[64:128, :], [pt], [UV[c]])
            chs = []
            for c in range(NCH):
                for h in range(2):
                    ch = CH()
                    hp = slice(h * 64, (h + 1) * 64)
                    ch.LT = LTz[h][:, c, :]; ch.RT = RT2[:, c, :]; ch.rd = [LTz[h], RTz[h], RT2]
                    ch.Dm = C["MaskZ"]; ch.Dm_rd = [cst]
                    ch.AT = RTz[h][:, c, 0:64]; ch.RrT = RTz[h][:, c, 64:128]
                    ch.M = Mst[fg]; ch.Mrows = hp
                    ch.UV = UV[c]; ch.UVc = slice(h * 64, (h + 1) * 64)
                    ch.gid = 0; ch.Mblk = (hp, hp)
                    ch.gC = eG[hp, c * 64 + 63:c * 64 + 64]; ch.gC_rd = [eG]
                    ch.yout = psYc[0][:, h * 64:(h + 1) * 64]; ch.yout_b = psYc[0]
                    chs.append(ch)
            eng.phaseA(chs)
            if stage == 3:
                for c in range(NCH):
                    fin.append(P.dma("sp", orw[fg * 128:fg * 128 + 64, tok0 + c * 64:tok0 + (c + 1) * 64], chs[2 * c].Tt[:], reads=[chs[2 * c].Tt]).idx)
                    fin.append(P.dma("sp", orw[fg * 128 + 64:fg * 128 + 128, tok0 + c * 64:tok0 + (c + 1) * 64], chs[2 * c].Zm[0:64, 0:64], reads=[chs[2 * c].Zm]).idx)
                continue
            for c in range(NCH):
                phaseB(P, chs[2 * c:2 * c + 2], scb, bS_slots, [(BKh[c][:], [BKh[c]], UV[c], psMg)])
                if stage == 4 and PB < 5:
                    P.copy("act", Ytm[:, 0:64], scb[0].Xs[:], [scb[0].Xs], [Ytm])
                    P.copy("act", Ytm[:, 64:128], scb[1].Xs[:], [scb[1].Xs], [Ytm])
                else:
                    P.copy("act", Ytm[:], psYc[0][:], [psYc[0]], [Ytm])
                if stage == 4:
                    fin.append(P.dma("sp", orw[fg * 128:fg * 128 + 64, tok0 + c * 64:tok0 + c * 64 + 64], Ytm[:, 0:64], reads=[Ytm]).idx)
                    fin.append(P.dma("sp", orw[fg * 128 + 64:fg * 128 + 128, tok0 + c * 64:tok0 + c * 64 + 64], Ytm[:, 64:128], reads=[Ytm]).idx)
                    continue
                Y3 = Ytm[:].rearrange("p (h v) -> p h v", h=2)
                P.op("dve", lambda e, Y3=Y3: e.tensor_reduce(out=msum[:], in_=Y3, axis=AX.X, op=ALU.add), [Ytm], [msum])
                P.ts("dve", nmean[:], msum[:], -1.0 / 64, None, ALU.mult, reads=[msum], writes=[nmean])
                for h in range(2):
                    hc = slice(h * 64, (h + 1) * 64)
                    P.ts("dve", yc[:, hc], Ytm[:, hc], nmean[:, h:h + 1], None, ALU.add, reads=[Ytm, nmean], writes=[yc])
                for h in range(2):
                    hc = slice(h * 64, (h + 1) * 64)
                    P.op("act", lambda e, hc=hc, h=h: e.activation(out=junk[:, hc], in_=yc[:, hc], func=AF.Square, accum_out=vs[:, h:h + 1]),
                         [yc], [junk, vs])
                P.ts("dve", rstd[:], vs[:], 1.0 / 64, 64e-5, ALU.mult, ALU.add, reads=[vs], writes=[rstd])
                P.act(rstd[:], rstd[:], AF.Ln, [rstd], [rstd])
                P.act(rstd[:], rstd[:], AF.Exp, [rstd], [rstd], scale=-0.5)
                for h in range(2):
                    hc = slice(h * 64, (h + 1) * 64)
                    P.ts("dve", yn[:, hc], yc[:, hc], rstd[:, h:h + 1], None, ALU.mult, reads=[yc, rstd], writes=[yn])
                pt = psT[c % 4]
                P.tr(pt[:, 0:64], yn[:], C["ident"][0:64, 0:64], [yn, cst], [pt])
                cs_ = slice(c * 64, (c + 1) * 64)
                P.ts("dve", fin1[:], pt[:, 0:64], prm[:, pb + 8:pb + 9], prm[:, pb + 9:pb + 10], ALU.mult, ALU.add, reads=[pt, prm], writes=[fin1])
                P.tt("pool", fin2[:], fin1[:], bonus[:, cs_], ALU.add, [fin1, bonus], [fin2])
                P.tt("pool", oblk[fg][:, cs_], fin2[:], gT[:, cs_], ALU.mult, [fin2, gT], [oblk[fg]])
            if stage == 4:
                continue
            fin.append(P.dma("sp", orw[fc, tok0:tok0 + NB], oblk[fg][:], reads=[oblk[fg]]).idx)
    P.emit(final_wait_ops=fin)
    return nc, P


def r_inputs(inp, core, xT):
    GC = 8224
    w_in = inp["w_in"]
    f0 = core * 256
    cols = []
    for base in (0, 2048, 4096):
        cols.append(np.arange(GC + base + f0, GC + base + f0 + 256))
    cols.append(np.arange(GC + 6144, GC + 6400))
    cols = np.concatenate(cols)
    wr = np.ascontiguousarray(w_in[:, cols])
    lora = np.zeros((128, 1024), np.float32)
    lora[0:96, 0:256] = inp["rwkv_w2"][:, f0:f0 + 256]
    lora[96:128, 256:512] = inp["rwkv_a2"][0:32, f0:f0 + 256]
    lora[0:64, 512:768] = inp["rwkv_a2"][32:96, f0:f0 + 256]
    lora[64:128, 768:1024] = inp["rwkv_g2"][:, f0:f0 + 256]
    mu = inp["rwkv_mu"]
    prm = np.zeros((128, 24), np.float32)
    for fg in range(2):
        fs = slice(f0 + fg * 128, f0 + (fg + 1) * 128)
        pb = fg * 10
        prm[:, pb + 0] = mu[0:2048][fs]
        prm[:, pb + 1] = mu[2048:4096][fs]
        prm[:, pb + 2] = mu[4096:6144][fs]
        prm[:, pb + 3] = inp["rwkv_w0"][fs]
        prm[:, pb + 4] = inp["rwkv_a0"][fs]
        prm[:, pb + 5] = inp["rwkv_k_k"][fs]
        prm[:, pb + 6] = inp["rwkv_k_a"][fs]
        prm[:, pb + 7] = inp["rwkv_r_k"].reshape(-1)[fs]
        prm[:, pb + 8] = inp["rwkv_lnx_g"][fs]
        prm[:, pb + 9] = inp["rwkv_lnx_b"][fs]
    prm[:, 20] = mu[6144:6272]
    prm[:, 21] = mu[6272:6400]
    return {"xT": xT, "wr": wr, "lora": lora, "prm": prm, "cst": C_ARR}


def build_g(nc, nblk, NTOK, stage=99):
    xT = nc.dram_tensor("xT", [D, NTOK], F32, kind="ExternalInput").ap()
    wg = nc.dram_tensor("wg", [D, 1024], F32, kind="ExternalInput").ap()
    wab_d = nc.dram_tensor("wab", [128, 32, 4], F32, kind="ExternalInput").ap()
    cw_d = nc.dram_tensor("cw", [128, 24], F32, kind="ExternalInput").ap()
    prm_d = nc.dram_tensor("prm", [128, 8], F32, kind="ExternalInput").ap()
    cst_d = nc.dram_tensor("cst", [128, NCST], F32, kind="ExternalInput").ap()
    og = nc.dram_tensor("og", [256, NTOK], F32, kind="ExternalOutput").ap()
    P = Prog(nc)
    fin = []
    cst, C = load_consts(P, cst_d)
    cw = P.sb([128, 24], name="cw"); P.dma("sp", cw[:], cw_d, writes=[cw])
    prm = P.sb([128, 8], name="prm"); P.dma("sp", prm[:], prm_d, writes=[prm])
    wabf = P.sb([128, 32, 4], name="wabf"); P.dma("sp", wabf[:], wab_d, writes=[wabf])
    wab = P.sb([128, 32, 4], BF16, name="wab"); P.copy("dve", wab[:], wabf[:], [wabf], [wab])
    negA = P.sb([128, 2], name="negA")
    P.act(negA[:], prm[:, 0:2], AF.Exp, [prm], [negA])
    P.ts("dve", negA[:], negA[:], -1.0, None, ALU.mult, reads=[negA], writes=[negA])
    stg = [P.sb([128, 4, 256], F32, name=f"stg{i}") for i in range(2)]
    wgb = P.sb([128, 32, 4, 256], BF16, name="wgb")
    wg_v = wg.rearrange("(c p) (a n) -> p c a n", p=128, a=4)
    for g in range(32):
        st = stg[g % 2]
        P.dma("sp" if g % 2 == 0 else "pool", st[:], wg_v[:, g], writes=[st])
        P.copy("act" if g % 2 == 0 else "dve", wgb[:, g], st[:], [st], [wgb])
    xb_t = [P.sb([128, 32, NB], BF16, name=f"xb{i}") for i in range(2)]
    xb_v = [[view(xb_t[p][:, g * 4:(g + 1) * 4, :], f"xb{p}_{g}") for g in range(8)] for p in range(2)]
    xT_v = xT.rearrange("(c p) t -> p c t", p=128)

    banks = [P.ps([128, 512], name=f"bank{i}") for i in range(8)]
    psA = [SubBuf(banks[0], banks[0][:, 0:256], "psA0"), SubBuf(banks[1], banks[1][:, 0:256], "psA1")]
    psS = SubBuf(banks[1], banks[1][:, 256:512], "psS")
    eng = Engine(P, 8, cst, C, banks[2:7])
    bSA, bSB = banks[6], banks[7]
    psT = [SubBuf(banks[2], banks[2][:, j * 128:(j + 1) * 128], f"psT{j}") for j in range(4)]
    bS_slots = [[SubBuf(bSA, bSA[0:64, h * 128:(h + 1) * 128], f"psX{h}"), SubBuf(bSA, bSA[0:64, 256 + h * 128:256 + (h + 1) * 128], f"psU{h}")] for h in range(2)]
    psYo = [SubBuf(bSB, bSB[0:64, h * 128:(h + 1) * 128], f"psYo{h}") for h in range(2)]
    psMg = [SubBuf(bSB, bSB[:, 256 + h * 128:256 + (h + 1) * 128], f"psMg{h}") for h in range(2)]
    scb = scan_bufs(P, None, 2, 128)

    def mk(shape, name, dt=F32):
        return P.sb(shape, dt, name=name)
    hbuf = [mk([128, NB + 3], f"hbuf{c}") for c in range(6)]
    acc = [mk([128, NB], f"acc{c}") for c in range(2)]
    cs = [mk([128, NB], f"cs{c}") for c in range(6)]
    qk = [mk([128, NB], f"qk{c}") for c in range(4)]
    szT = [mk([128, NB], f"szT{c}") for c in range(2)]
    sq = mk([128, NB], "sq"); rn = mk([128, NB], "rn")
    ab = mk([128, 4], "ab"); vals = mk([128, 6], "vals"); g1 = mk([128, 2], "g1")
    st = [mk([128, 6], f"st{c}") for c in range(NCH)]
    gcb = [mk([128, 2], f"gcb{c}") for c in range(NCH)]
    eg = mk([128, 2], "eg"); tsc = mk([128, 2], "tsc")
    rs = [mk([128, 2], f"rs{c}") for c in range(NCH)]
    eGc = [mk([128, 2], f"eGc{c}") for c in range(NCH)]; eGm = mk([128, 2], "eGmc"); dG = mk([128, 2], "dG")
    eRv = mk([128, 2], "eRv"); bks = [mk([128, 2], f"bks{c}") for c in range(NCH)]; gCe = [mk([128, 2], f"gCe{c}") for c in range(NCH)]
    gB = mk([128, 128], "gB"); Dl = mk([128, 128], "Dl"); diagM = mk([128, 128], "diagM")
    Dm = [mk([128, 128], f"Dm{i}") for i in range(8)]
    LTg = [mk([128, 128], f"LTg{i}") for i in range(8)]
    RTg = [mk([128, 128], f"RTg{i}") for i in range(8)]
    RTu = [mk([128, 128], f"RTu{i}") for i in range(8)]
    VTg = mk([128, 128], "VTg"); P.memset("pool", VTg[:], 0.0, [VTg])
    BKh = [mk([128, 128], f"BKh{i}") for i in range(8)]
    UV = [mk([128, 128], f"UV{i}") for i in range(8)]
    for i in range(8):
        P.memset("pool", UV[i][:], 0.0, [UV[i]])
    Mst = [mk([128, 128], f"Mst{h}") for h in range(2)]
    o2 = mk([64, 128], "o2"); junk = mk([64, 128], "junk"); on = mk([64, 128], "on")
    ssq = mk([64, 1], "ssq"); rstd = mk([64, 1], "rstd"); f1 = mk([128, 64], "f1")
    oblk = [mk([128, NB], f"oblk{h}") for h in range(2)]

    for blk in range(nblk):
        par = blk % 2
        tok0 = blk * NB
        seq_start = (tok0 % T) == 0
        for g in range(8):
            s_ = stg[g % 2]
            P.dma("sp" if g % 2 == 0 else "pool", s_[:], xT_v[:, g * 4:(g + 1) * 4, tok0:tok0 + NB], writes=[s_])
            P.copy("pool", xb_v[par][g][:], s_[:], [s_], [xb_v[par][g]])
        xb = xb_t[par]; xbr = xb_v[par]
        if seq_start:
            for h in range(2):
                P.memset("pool", Mst[h][:], 0.0, [Mst[h]])
        for cg in range(8):
            ps = psA[cg % 2]
            for dc in range(32):
                P.mm(ps[:], wgb[:, dc, cg // 2, (cg % 2) * 128:(cg % 2 + 1) * 128], xb[:, dc, :], [wgb, xbr[dc // 4]], [ps], start=(dc == 0), stop=(dc == 31))
            if cg >= 6:
                P.act(szT[cg - 6][:], ps[:], AF.Silu, [ps], [szT[cg - 6]])
                continue
            hb = hbuf[cg]
            if seq_start:
                P.memset("pool", hb[:, 0:3], 0.0, [hb])
            P.copy("act", hb[:, 3:NB + 3], ps[:], [ps], [hb])
            a = acc[cg % 2]
            P.ts("dve", a[:], hb[:, 0:NB], cw[:, cg * 4:cg * 4 + 1], None, ALU.mult, reads=[hb, cw], writes=[a])
            for j in range(1, 4):
                P.stt("dve", a[:], hb[:, j:j + NB], cw[:, cg * 4 + j:cg * 4 + j + 1], a[:], ALU.mult, ALU.add, [hb, cw, a], [a])
            P.copy("pool", hb[:, 0:3], hb[:, NB:NB + 3], [hb], [hb])
            P.act(cs[cg][:], a[:], AF.Silu, [a], [cs[cg]])
        for cc in range(4):
            P.tt("pool", sq[:], cs[cc][:], cs[cc][:], ALU.mult, [cs[cc]], [sq])
            P.mm(psS[:], C["ones"], sq[:], [cst, sq], [psS])
            P.ts("dve", rn[:], psS[:], 1e-6, None, ALU.add, reads=[psS], writes=[rn])
            P.act(rn[:], rn[:], AF.Ln, [rn], [rn])
            P.act(rn[:], rn[:], AF.Exp, [rn], [rn], scale=-0.5)
            if cc < 2:
                P.stt("dve", qk[cc][:], cs[cc][:], float(128 ** -0.5), rn[:], ALU.mult, ALU.mult, [cs[cc], rn], [qk[cc]])
            else:
                P.tt("dve", qk[cc][:], cs[cc][:], rn[:], ALU.mult, [cs[cc], rn], [qk[cc]])
        if stage == 1:
            fin.append(P.dma("sp", og[0:128, tok0:tok0 + NB], qk[0][:], reads=[qk[0]]).idx)
            fin.append(P.dma("sp", og[128:256, tok0:tok0 + NB], qk[2][:], reads=[qk[2]]).idx)
            continue
        for s2 in range(NB // 128):
            tsl = slice(s2 * 128, (s2 + 1) * 128)
            pab = SubBuf(banks[1], banks[1][:, 256:260], "pab")
            for dc in range(32):
                P.mm(pab[:], xb[:, dc, tsl], wab[:, dc, :], [xbr[dc // 4], wab], [pab], start=(dc == 0), stop=(dc == 31))
            P.copy("dve", ab[:], pab[:], [pab], [ab])
            P.tt("dve", g1[:], ab[:, 0:2], prm[:, 2:4], ALU.add, [ab, prm], [g1])
            P.act(g1[:], g1[:], AF.Exp, [g1], [g1])
            P.ts("dve", g1[:], g1[:], 1.0, None, ALU.add, reads=[g1], writes=[g1])
            P.act(g1[:], g1[:], AF.Ln, [g1], [g1])
            P.tt("dve", vals[:, 0:2], g1[:], negA[:], ALU.mult, [g1, negA], [vals])
            P.act(vals[:, 4:6], ab[:, 2:4], AF.Sigmoid, [ab], [vals])
            pG = SubBuf(banks[1], banks[1][:, 264:266], "pG")
            P.mm(pG[:], C["UblkI"], vals[:, 0:2], [cst, vals], [pG])
            P.copy("dve", vals[:, 2:4], pG[:], [pG], [vals])
            for c2 in range(2):
                c = s2 * 2 + c2
                pst = SubBuf(banks[1], banks[1][:, 272:278], "pst")
                P.mm(pst[:], C["Dup" if c2 == 0 else "Dup1"], vals[:], [cst, vals], [pst])
                P.copy("dve", st[c][:], pst[:], [pst], [st[c]])
                pgc = SubBuf(banks[1], banks[1][:, 280:282], "pgc")
                P.mm(pgc[:], C["SelL0"], st[c][:, 2:4], [cst, st[c]], [pgc])
                P.copy("dve", gcb[c][:], pgc[:], [pgc], [gcb[c]])
                P.act(eg[:], st[c][:, 0:2], AF.Exp, [st[c]], [eg])
                P.ts("dve", tsc[:], eg[:], C["sgn"][:, 0:1], C["sgn"][:, 1:2], ALU.mult, ALU.add, reads=[eg, cst], writes=[tsc])
                P.tt("dve", rs[c][:], tsc[:], st[c][:, 4:6], ALU.mult, [tsc, st[c]], [rs[c]])
                P.act(eGc[c][:], st[c][:, 2:4], AF.Exp, [st[c]], [eGc[c]])
                P.tt("dve", dG[:], st[c][:, 2:4], st[c][:, 0:2], ALU.subtract, [st[c]], [dG])
                P.act(eGm[:], dG[:], AF.Exp, [dG], [eGm])
                P.tt("dve", dG[:], gcb[c][:], st[c][:, 2:4], ALU.subtract, [gcb[c], st[c]], [dG])
                P.act(eRv[:], dG[:], AF.Exp, [dG], [eRv])
                P.tt("dve", bks[c][:], rs[c][:], eRv[:], ALU.mult, [rs[c], eRv], [bks[c]])
                P.act(gCe[c][:], gcb[c][:], AF.Exp, [gcb[c]], [gCe[c]])
                for h in range(2):
                    i = c * 2 + h
                    csl = slice(c * 64, (c + 1) * 64)
                    qT = qk[h]; kT = qk[2 + h]; vT = cs[4 + h]
                    P.ts("pool", gB[:], C["ones"], st[c][:, h:h + 1], None, ALU.mult, reads=[cst, st[c]], writes=[gB])
                    pd = psT[i % 4]
                    P.mm(pd[:], gB[:], C["UU"], [gB, cst], [pd], start=True, stop=False)
                    P.mm(pd[:], C["negUU"], gB[:], [gB, cst], [pd], start=False, stop=True)
                    P.tt("dve", Dl[:], pd[:], C["NegM"], ALU.add, [pd, cst], [Dl])
                    P.act(Dl[:], Dl[:], AF.Exp, [Dl], [Dl])
                    P.ts("dve", Dm[i][:], Dl[:], rs[c][:, h:h + 1], None, ALU.mult, reads=[Dl, rs[c]], writes=[Dm[i]])
                    P.ts("dve", diagM[:, 0:64], C["Dup"][:, 0:64], eGm[:, h:h + 1], None, ALU.mult, reads=[cst, eGm], writes=[diagM])
                    P.ts("dve", diagM[:, 64:128], C["Dup"][:, 64:128], eGc[c][:, h:h + 1], None, ALU.mult, reads=[cst, eGc[c]], writes=[diagM])
                    pb_ = psT[(i + 1) % 4]
                    P.mm(pb_[:], C["ones"], diagM[:], [cst, diagM], [pb_])
                    P.tt("dve", RTg[i][:, 0:64], pb_[:, 0:64], kT[:, csl], ALU.mult, [pb_, kT], [RTg[i]])
                    P.tt("dve", RTg[i][:, 64:128], pb_[:, 64:128], qT[:, csl], ALU.mult, [pb_, qT], [RTg[i]])
                    P.copy("pool", LTg[i][:, 0:64], kT[:, csl], [kT], [LTg[i]])
                    P.copy("pool", LTg[i][:, 64:128], kT[:, csl], [kT], [LTg[i]])
                    P.copy("pool", RTu[i][:, 0:64], kT[:, csl], [kT], [RTu[i]])
                    P.copy("pool", RTu[i][:, 64:128], qT[:, csl], [qT], [RTu[i]])
                    pt = psT[(i + 2) % 4]
                    P.tr(pt[:], LTg[i][:], C["ident"], [LTg[i], cst], [pt])
                    P.ts("dve", BKh[i][:], pt[:], bks[c][:, h:h + 1], None, ALU.mult, reads=[pt, bks[c]], writes=[BKh[i]])
                    P.copy("pool", VTg[:, 64:128], vT[:, csl], [vT], [VTg])
                    pt = psT[(i + 3) % 4]
                    P.tr(pt[:], VTg[:], C["ident"], [VTg, cst], [pt])
                    P.copy("act", UV[i][64:128, :], pt[64:128, :], [pt], [UV[i]])
        if stage == 2:
            fin.append(P.dma("sp", og[0:128, tok0:tok0 + 128], BKh[7][:], reads=[BKh[7]]).idx)
            fin.append(P.dma("sp", og[128:256, tok0:tok0 + 128], Dm[7][:], reads=[Dm[7]]).idx)
            fin.append(P.dma("sp", og[0:128, tok0 + 128:tok0 + 256], UV[7][:], reads=[UV[7]]).idx)
            fin.append(P.dma("sp", og[128:256, tok0 + 128:tok0 + 256], RTg[7][:], reads=[RTg[7]]).idx)
            continue
        chs = []
        for c in range(NCH):
            for h in range(2):
                i = c * 2 + h
                ch = CH()
                ch.LT = LTg[i][:]; ch.RT = RTu[i][:]; ch.rd = [LTg[i], RTg[i], RTu[i]]
                ch.Dm = Dm[i][:]; ch.Dm_rd = [Dm[i]]
                ch.AT = RTg[i][:, 0:64]; ch.RrT = RTg[i][:, 64:128]
                ch.M = Mst[h]; ch.Mrows = slice(0, 128)
                ch.UV = UV[i]; ch.UVc = slice(0, 128)
                ch.gid = h; ch.Mblk = (slice(0, 128), slice(0, 128))
                ch.gC = gCe[c][:, h:h + 1]; ch.gC_rd = [gCe[c]]
                ch.yout = psYo[h][:]; ch.yout_b = psYo[h]
                chs.append(ch)
        eng.phaseA(chs)
        for c in range(NCH):
            cc_ = chs[2 * c:2 * c + 2]
            phaseB(P, cc_, scb, bS_slots, [(BKh[2 * c + h][:], [BKh[2 * c + h]], UV[2 * c + h], psMg[h]) for h in range(2)])
            csl = slice(c * 64, (c + 1) * 64)
            for h in range(2):
                P.copy("act", o2[:], psYo[h][:], [psYo[h]], [o2])
                P.op("act", lambda e: e.activation(out=junk[:], in_=o2[:], func=AF.Square, accum_out=ssq[:]), [o2], [junk, ssq])
                P.ts("dve", rstd[:], ssq[:], 1.0 / 128, 1e-6, ALU.mult, ALU.add, reads=[ssq], writes=[rstd])
                P.act(rstd[:], rstd[:], AF.Ln, [rstd], [rstd])
                P.act(rstd[:], rstd[:], AF.Exp, [rstd], [rstd], scale=-0.5)
                P.ts("dve", on[:], o2[:], rstd[:, 0:1], None, ALU.mult, reads=[o2, rstd], writes=[on])
                pt = psT[h]
                P.tr(pt[:, 0:64], on[:], C["ident"][0:64, 0:64], [on, cst], [pt])
                P.ts("dve", f1[:], pt[:, 0:64], prm[:, 4:5], None, ALU.mult, reads=[pt, prm], writes=[f1])
                P.tt("pool", oblk[h][:, csl], f1[:], szT[h][:, csl], ALU.mult, [f1, szT[h]], [oblk[h]])
        for h in range(2):
            fin.append(P.dma("sp", og[h * 128:(h + 1) * 128, tok0:tok0 + NB], oblk[h][:], reads=[oblk[h]]).idx)
    P.emit(final_wait_ops=fin)
    return nc, P


def g_inputs(inp, core, xT):
    w_in = inp["w_in"]
    h0, h1 = 2 * core, 2 * core + 1
    cols = []
    for base in (0, 2048, 4096, 6144):
        for h in (h0, h1):
            cols.append(np.arange(base + h * 128, base + (h + 1) * 128))
    wg = np.ascontiguousarray(w_in[:, np.concatenate(cols)])
    abc = np.array([8192 + h0, 8192 + h1, 8192 + 16 + h0, 8192 + 16 + h1])
    wab = np.ascontiguousarray(w_in[:, abc].reshape(32, 128, 4).transpose(1, 0, 2))
    cwl = inp["gdn_conv_w"]
    cw = np.zeros((128, 24), np.float32)
    cc = 0
    for base in (0, 2048, 4096):
        for h in (h0, h1):
            cw[:, cc * 4:(cc + 1) * 4] = cwl[:, base + h * 128: base + (h + 1) * 128].T
            cc += 1
    prm = np.zeros((128, 8), np.float32)
    prm[:, 0] = inp["gdn_a_log"][h0]; prm[:, 1] = inp["gdn_a_log"][h1]
    prm[:, 2] = inp["gdn_dt_bias"][h0]; prm[:, 3] = inp["gdn_dt_bias"][h1]
    prm[:, 4] = inp["gdn_norm_w"]
    return {"xT": xT, "wg": wg, "wab": wab, "cw": cw, "prm": prm, "cst": C_ARR}


DM = 4096
NE = 32
ALPHA = float((2 * 4) ** 0.25)
BIG = 10000.0


def f_consts():
    k = np.arange(128)[:, None]
    i = np.arange(128)[None, :]
    return np.ascontiguousarray(np.concatenate([(k == i), np.ones((128, 128))], axis=1).astype(np.float32))


CF_ARR = f_consts()


def build_f(nc, NTOKC=1024, NT=4, n_exp=NE, stage=99):
    TB = NT * 128
    nblk = NTOKC // TB
    oT = nc.dram_tensor("oT", [DM, NTOKC], F32, kind="ExternalInput").ap()
    xres = nc.dram_tensor("xres", [NTOKC, DM], F32, kind="ExternalInput").ap()
    pT = nc.dram_tensor("pT", [256, NTOKC], F32, kind="ExternalInput").ap()
    w_out = nc.dram_tensor("w_out", [DM, DM], F32, kind="ExternalInput").ap()
    lnp = nc.dram_tensor("lnp", [4, 128, DM], F32, kind="ExternalInput").ap()
    wrt = nc.dram_tensor("wrt", [DM, 36], F32, kind="ExternalInput").ap()
    brt = nc.dram_tensor("brt", [128, 36], F32, kind="ExternalInput").ap()
    wgate = nc.dram_tensor("wgate", [NE, DM, 256], F32, kind="ExternalInput").ap()
    wup = nc.dram_tensor("wup", [NE, DM, 256], F32, kind="ExternalInput").ap()
    wdown = nc.dram_tensor("wdown", [NE, 256, DM], F32, kind="ExternalInput").ap()
    pw_proj = nc.dram_tensor("pw_proj", [256, DM], F32, kind="ExternalInput").ap()
    pw_gd = nc.dram_tensor("pw_gd", [DM, 256], F32, kind="ExternalInput").ap()
    pw_gu = nc.dram_tensor("pw_gu", [256, DM], F32, kind="ExternalInput").ap()
    cst_d = nc.dram_tensor("cst", [128, 256], F32, kind="ExternalInput").ap()
    xo = nc.dram_tensor("xo", [NTOKC, DM], F32, kind="ExternalOutput").ap()
    P = Prog(nc)
    fin = []
    Y1 = [P.dram(f"Y1_{t}", [128, DM]) for t in range(NT)]
    X1 = [P.dram(f"X1_{t}", [128, DM]) for t in range(NT)]
    Y2 = [P.dram(f"Y2_{t}", [128, DM]) for t in range(NT)]

    cst = P.sb([128, 256], name="cst"); P.dma("sp", cst[:], cst_d, writes=[cst])
    ident = cst[:, 0:128]; ones = cst[:, 128:256]
    wr32 = P.sb([128, 32, 36], name="wr32")
    P.dma("sp", wr32[:], wrt.rearrange("(c p) n -> p c n", p=128), writes=[wr32])
    bias = P.sb([128, 36], name="bias"); P.dma("sp", bias[:], brt, writes=[bias])

    def mk(shape, name, dt=F32):
        return P.sb(shape, dt, name=name)
    actT = mk([128, 32, TB], "actT", BF16)
    hact = mk([128, 2 * NE, TB], "hact", BF16)
    wa = mk([128, DM], "wa")
    NWS = 3
    wst = [mk([128, 1024], f"wst{i}") for i in range(NWS)]
    wbf = [mk([128, 1024], f"wbf{i}", BF16) for i in range(NWS)]
    wcnt = [0]

    def wload(src, a):
        j = wcnt[0] % NWS
        wcnt[0] += 1
        st, bf = wst[j], wbf[j]
        q = "sp" if wcnt[0] % 2 else "pool"
        dst = st[:] if a == 1 else st[:].rearrange("p (a n) -> p a n", a=a)
        P.dma(q, dst, src, writes=[st])
        eng = ("act", "dve", "pool")[wcnt[0] % 3]
        P.copy(eng, bf[:], st[:], [st], [bf])
        return bf

    banks = [P.ps([128, 512], name=f"bank{i}") for i in range(8)]
    accA = banks[0:4]
    accB = banks[4:8]
    xs = [mk([128, 512], f"xs{i}") for i in range(2)]
    ev = [mk([128, 512], f"ev{i}") for i in range(2)]
    yv = [mk([128, 512], f"yv{i}") for i in range(2)]
    gpc = [mk([128, 512], f"gpc{i}") for i in range(2)]
    bpc = [mk([128, 512], f"bpc{i}") for i in range(2)]
    xf = [mk([128, 128], f"xf{i}") for i in range(4)]
    junk = mk([128, 512], "junk")
    s1 = mk([128, 1], "s1"); nmean = mk([128, 1], "nmean"); ssq8 = mk([128, 8], "ssq8"); ssq = mk([128, 1], "ssq"); rstd = mk([128, 1], "rstd")
    combT = mk([128, TB], "combT"); P.memset("pool", combT[:], 0.0, [combT])
    sel = [mk([128, 128], f"sel{i}") for i in range(2)]
    cb = mk([128, TB], "cb")
    sil = [mk([128, TB], f"sil{i}") for i in range(2)]
    tmu = [mk([128, TB], f"tmu{i}") for i in range(2)]
    pst = mk([128, 2, TB], "pst"); pTb = mk([128, 2, TB], "pTb", BF16); gdT = mk([128, 2, TB], "gdT", BF16)
    pr = [mk([128, 512], f"pr{i}") for i in range(2)]; sg = [mk([128, 512], f"sg{i}") for i in range(2)]
    lg = mk([128, 36], "lg"); gmax = mk([128, 1], "gmax"); ngmax = mk([128, 1], "ngmax"); gmask = mk([128, 4], "gmask")
    eg4 = mk([128, 4], "eg4"); gsum = mk([128, 1], "gsum"); pg = mk([128, 1], "pg"); pen = mk([128, 4], "pen")
    ml = mk([128, 32], "ml"); v1 = mk([128, 1], "v1"); m1 = mk([128, 32], "m1"); ml2 = mk([128, 32], "ml2")
    v2 = mk([128, 1], "v2"); m2 = mk([128, 32], "m2"); dd = mk([128, 1], "dd"); w1 = mk([128, 1], "w1"); w2 = mk([128, 1], "w2")
    g1 = mk([128, 1], "g1"); g2 = mk([128, 1], "g2"); comb = mk([128, 32], "comb"); comb2 = mk([128, 32], "comb2")

    oT_v = oT.rearrange("(c p) t -> p c t", p=128)
    w_out_v = w_out.rearrange("(c p) n -> p c n", p=128)
    pT_v = pT.rearrange("(c p) t -> p c t", p=128)

    def layer_norm_tile(src, which, t, tok0):
        P.dma("sp", wa[:], src[:], reads=[src], writes=[wa])
        P.op("dve", lambda e: e.tensor_reduce(out=s1[:], in_=wa[:], axis=AX.X, op=ALU.add), [wa], [s1])
        P.ts("dve", nmean[:], s1[:], -1.0 / DM, None, ALU.mult, reads=[s1], writes=[nmean])
        for ng in range(8):
            P.op("act", lambda e, ng=ng: e.activation(out=junk[:], in_=wa[:, ng * 512:(ng + 1) * 512], func=AF.Square,
                                                      bias=nmean[:, 0:1], accum_out=ssq8[:, ng:ng + 1]), [wa, nmean], [junk, ssq8])
        P.op("dve", lambda e: e.tensor_reduce(out=ssq[:], in_=ssq8[:], axis=AX.X, op=ALU.add), [ssq8], [ssq])
        P.ts("dve", rstd[:], ssq[:], 1.0 / DM, 1e-5, ALU.mult, ALU.add, reads=[ssq], writes=[rstd])
        P.act(rstd[:], rstd[:], AF.Ln, [rstd], [rstd])
        P.act(rstd[:], rstd[:], AF.Exp, [rstd], [rstd], scale=-0.5)
        psR = banks[5]
        for ng in range(8):
            gs = slice(ng * 512, (ng + 1) * 512)
            gp = gpc[ng % 2]; bp = bpc[ng % 2]; y = yv[ng % 2]; e_ = ev[ng % 2]
            P.dma("pool", gp[:], lnp[2 * which, :, gs], writes=[gp])
            P.dma("pool", bp[:], lnp[2 * which + 1, :, gs], writes=[bp])
            P.ts("dve", e_[:], wa[:, gs], nmean[:, 0:1], rstd[:, 0:1], ALU.add, ALU.mult, reads=[wa, nmean, rstd], writes=[e_])
            P.tt("pool", y[:], e_[:], gp[:], ALU.mult, [e_, gp], [y])
            P.tt("pool", y[:], y[:], bp[:], ALU.add, [y, bp], [y])
            if which == 1:
                fin.append(P.dma("sp", xo[tok0 + t * 128:tok0 + (t + 1) * 128, gs], y[:], reads=[y]).idx)
                continue
            P.dma("sp", X1[t][:, gs], y[:], reads=[y], writes=[X1[t]])
            for j in range(4):
                kc = ng * 4 + j
                pt = banks[4] if kc % 2 == 0 else banks[7]
                P.tr(pt[:, 0:128], y[:, j * 128:(j + 1) * 128], ident, [y, cst], [pt])
                f = xf[kc % 4]
                P.copy("act", f[:], pt[:, 0:128], [pt], [f])
                P.copy("pool", actT[:, kc, t * 128:(t + 1) * 128], f[:], [f], [actT])
                P.mm(psR[:, 0:36], f[:], wr32[:, kc, :], [f, wr32], [psR], start=(kc == 0), stop=(kc == 31))
        if which == 1:
            return
        P.tt("dve", lg[:], psR[:, 0:36], bias[:], ALU.add, [psR, bias], [lg])
        P.op("dve", lambda e: e.tensor_reduce(out=gmax[:], in_=lg[:, 0:4], axis=AX.X, op=ALU.max), [lg], [gmax])
        P.ts("dve", gmask[:], lg[:, 0:4], gmax[:, 0:1], None, ALU.is_ge, reads=[lg, gmax], writes=[gmask])
        P.ts("dve", ngmax[:], gmax[:], -1.0, None, ALU.mult, reads=[gmax], writes=[ngmax])
        P.op("act", lambda e: e.activation(out=eg4[:], in_=lg[:, 0:4], func=AF.Exp, bias=ngmax[:, 0:1], accum_out=gsum[:]), [lg, ngmax], [eg4, gsum])
        P.op("dve", lambda e: e.reciprocal(out=pg[:], in_=gsum[:]), [gsum], [pg])
        P.ts("dve", pen[:], gmask[:], BIG, -BIG, ALU.mult, ALU.add, reads=[gmask], writes=[pen])
        for g in range(4):
            P.ts("dve", ml[:, g * 8:(g + 1) * 8], lg[:, 4 + g * 8:4 + (g + 1) * 8], pen[:, g:g + 1], None, ALU.add, reads=[lg, pen], writes=[ml])
        P.op("dve", lambda e: e.tensor_reduce(out=v1[:], in_=ml[:], axis=AX.X, op=ALU.max), [ml], [v1])
        P.ts("dve", m1[:], ml[:], v1[:, 0:1], None, ALU.is_ge, reads=[ml, v1], writes=[m1])
        P.stt("dve", ml2[:], m1[:], -BIG, ml[:], ALU.mult, ALU.add, [m1, ml], [ml2])
        P.op("dve", lambda e: e.tensor_reduce(out=v2[:], in_=ml2[:], axis=AX.X, op=ALU.max), [ml2], [v2])
        P.ts("dve", m2[:], ml2[:], v2[:, 0:1], None, ALU.is_ge, reads=[ml2, v2], writes=[m2])
        P.tt("dve", dd[:], v1[:], v2[:], ALU.subtract, [v1, v2], [dd])
        P.act(w1[:], dd[:], AF.Sigmoid, [dd], [w1])
        P.ts("dve", w2[:], w1[:], -1.0, 1.0, ALU.mult, ALU.add, reads=[w1], writes=[w2])
        P.tt("dve", g1[:], w1[:], pg[:], ALU.mult, [w1, pg], [g1])
        P.tt("dve", g2[:], w2[:], pg[:], ALU.mult, [w2, pg], [g2])
        P.ts("dve", comb[:], m1[:], g1[:, 0:1], None, ALU.mult, reads=[m1, g1], writes=[comb])
        P.ts("dve", comb2[:], m2[:], g2[:, 0:1], None, ALU.mult, reads=[m2, g2], writes=[comb2])
        P.tt("dve", comb[:], comb[:], comb2[:], ALU.add, [comb, comb2], [comb])
        pt = banks[4]
        P.tr(pt[0:32, 0:128], comb[:], ident, [comb, cst], [pt])
        P.copy("act", combT[0:32, t * 128:(t + 1) * 128], pt[0:32, 0:128], [pt], [combT])

    for blk in range(nblk):
        tok0 = blk * TB
        for g in range(16):
            st = wst[g % NWS]
            sv = st[:, 0:2 * TB].rearrange("p (a n) -> p a n", a=2)
            P.dma("sp" if g % 2 == 0 else "pool", sv, oT_v[:, g * 2:(g + 1) * 2, tok0:tok0 + TB], writes=[st])
            P.copy("pool" if g % 2 == 0 else "act", actT[:, g * 2:(g + 1) * 2, :], sv, [st], [actT])
        P.dma("sp", pst[:], pT_v[:, :, tok0:tok0 + TB], writes=[pst])
        P.copy("dve", pTb[:], pst[:], [pst], [pTb])
        for ng in range(8):
            gs = slice(ng * 512, (ng + 1) * 512)
            for k2 in range(16):
                bf = wload(w_out_v[:, k2 * 2:(k2 + 1) * 2, gs], 2)
                for j in range(2):
                    kc = k2 * 2 + j
                    for t in range(NT):
                        P.mm(accA[t][:], actT[:, kc, t * 128:(t + 1) * 128], bf[:, j * 512:(j + 1) * 512], [actT, bf], [accA[t]],
                             start=(kc == 0), stop=(kc == 31))
            for t in range(NT):
                x_ = xs[t % 2]; e_ = ev[t % 2]; y = yv[t % 2]
                P.dma("pool", x_[:], xres[tok0 + t * 128:tok0 + (t + 1) * 128, gs], writes=[x_])
                P.copy("act", e_[:], accA[t][:], [accA[t]], [e_])
                P.stt("dve", y[:], x_[:], ALPHA, e_[:], ALU.mult, ALU.add, [x_, e_], [y])
                P.dma("sp", Y1[t][:, gs], y[:], reads=[y], writes=[Y1[t]])
        if stage == 1:
            for t in range(NT):
                P.dma("sp", wa[:], Y1[t][:], reads=[Y1[t]], writes=[wa])
                fin.append(P.dma("sp", xo[tok0 + t * 128:tok0 + (t + 1) * 128, :], wa[:], reads=[wa]).idx)
            continue
        for t in range(NT):
            layer_norm_tile(Y1[t], 0, t, tok0)
        if stage == 2:
            for t in range(NT):
                P.dma("sp", wa[:], X1[t][:], reads=[X1[t]], writes=[wa])
                fin.append(P.dma("sp", xo[tok0 + t * 128:tok0 + (t + 1) * 128, :], wa[:], reads=[wa]).idx)
            continue
        for e in range(n_exp + 1):
            mats = [(wgate[e], 0), (wup[e], 2)] if e < n_exp else [(pw_gd, 0)]
            for (wm, b0) in mats:
                wm_v = wm.rearrange("(c p) n -> p c n", p=128)
                for k4 in range(8):
                    bf = wload(wm_v[:, k4 * 4:(k4 + 1) * 4, :], 4)
                    for j in range(4):
                        kc = k4 * 4 + j
                        for fc in range(2):
                            P.mm(accA[b0 + fc][:, 0:TB], bf[:, j * 256 + fc * 128:j * 256 + (fc + 1) * 128], actT[:, kc, :], [bf, actT], [accA[b0 + fc]],
                                 start=(kc == 0), stop=(kc == 31))
            if e == n_exp:
                for fc in range(2):
                    P.copy("act", gdT[:, fc, :], accA[fc][:, 0:TB], [accA[fc]], [gdT])
                continue
            sl = sel[e % 2]
            P.ts("pool", sl[:], ones, ident[:, e:e + 1], None, ALU.mult, reads=[cst], writes=[sl])
            pc = banks[6]
            P.mm(pc[:, 0:TB], sl[:], combT[:], [sl, combT], [pc])
            P.copy("act", cb[:], pc[:, 0:TB], [pc], [cb])
            for fc in range(2):
                s_ = sil[fc]; t_ = tmu[fc]
                P.act(s_[:], accA[fc][:, 0:TB], AF.Silu, [accA[fc]], [s_])
                P.tt("dve", t_[:], accA[2 + fc][:, 0:TB], s_[:], ALU.mult, [accA[2 + fc], s_], [t_])
                P.tt("pool", hact[:, e * 2 + fc, :], t_[:], cb[:], ALU.mult, [t_, cb], [hact])
        for dg in range(8):
            gs = slice(dg * 512, (dg + 1) * 512)
            for e in range(n_exp):
                bf = wload(wdown[e].rearrange("(c p) n -> p c n", p=128)[:, :, gs], 2)
                for fc in range(2):
                    for t in range(NT):
                        P.mm(accA[t][:], hact[:, e * 2 + fc, t * 128:(t + 1) * 128], bf[:, fc * 512:(fc + 1) * 512], [hact, bf], [accA[t]],
                             start=(e == 0 and fc == 0), stop=(e == n_exp - 1 and fc == 1))
            bfp = wload(pw_proj.rearrange("(c p) n -> p c n", p=128)[:, :, gs], 2)
            for t in range(NT):
                for fc in range(2):
                    P.mm(accB[t][:], pTb[:, fc, t * 128:(t + 1) * 128], bfp[:, fc * 512:(fc + 1) * 512], [pTb, bfp], [accB[t]], start=(fc == 0), stop=(fc == 1))
            bfg = wload(pw_gu.rearrange("(c p) n -> p c n", p=128)[:, :, gs], 2)
            for t in range(NT):
                p_ = pr[t % 2]; s_ = sg[t % 2]; x_ = xs[t % 2]; e_ = ev[t % 2]; y = yv[t % 2]
                P.copy("act", p_[:], accB[t][:], [accB[t]], [p_])
                for fc in range(2):
                    P.mm(accB[t][:], gdT[:, fc, t * 128:(t + 1) * 128], bfg[:, fc * 512:(fc + 1) * 512], [gdT, bfg], [accB[t]], start=(fc == 0), stop=(fc == 1))
                P.act(s_[:], accB[t][:], AF.Sigmoid, [accB[t]], [s_])
                P.tt("pool", p_[:], p_[:], s_[:], ALU.mult, [p_, s_], [p_])
                P.dma("pool", x_[:], X1[t][:, gs], reads=[X1[t]], writes=[x_])
                P.copy("act", e_[:], accA[t][:], [accA[t]], [e_])
                P.stt("dve", y[:], x_[:], ALPHA, e_[:], ALU.mult, ALU.add, [x_, e_], [y])
                P.tt("pool", y[:], y[:], p_[:], ALU.add, [y, p_], [y])
                P.dma("sp", Y2[t][:, gs], y[:], reads=[y], writes=[Y2[t]])
        for t in range(NT):
            layer_norm_tile(Y2[t], 1, t, tok0)
    P.emit(final_wait_ops=fin)
    return nc, P


def f_inputs(inp, oT_c, xres_c, pT_c):
    lnp = np.stack([np.broadcast_to(inp[k][None, :], (128, DM)) for k in ("ln1_g", "ln1_b", "ln2_g", "ln2_b")]).astype(np.float32)
    wrt = np.ascontiguousarray(np.concatenate([inp["moe_w_grp"], inp["moe_w_rt"]], axis=1))
    brt = np.ascontiguousarray(np.broadcast_to(np.concatenate([inp["moe_b_grp"], inp["moe_b_rt"]])[None, :], (128, 36))).astype(np.float32)
    return {"oT": oT_c, "xres": xres_c, "pT": pT_c, "w_out": inp["w_out"], "lnp": np.ascontiguousarray(lnp), "wrt": wrt, "brt": brt,
            "wgate": inp["moe_w_gate"], "wup": inp["moe_w_up"], "wdown": inp["moe_w_down"],
            "pw_proj": inp["ple_w_proj"], "pw_gd": inp["ple_w_gate_down"], "pw_gu": inp["ple_w_gate_up"], "cst": CF_ARR}


NCORES = 8
_PROGS = {}


def _prog(kind):
    if kind not in _PROGS:
        nc = bass.Bass("TRN2", target_bir_lowering=False)
        if kind == "g":
            nc, _ = build_g(nc, nblk=8192 // NB, NTOK=8192)
        elif kind == "r":
            nc, _ = build_r(nc, nblk=8192 // NB, NTOK=8192)
        else:
            nc, _ = build_f(nc, NTOKC=1024, NT=4)
        _PROGS[kind] = nc
    return _PROGS[kind]


def kernel(**inputs):
    x = np.asarray(inputs["x"], dtype=np.float32)
    p = np.asarray(inputs["p"], dtype=np.float32)
    B, S, Dm = x.shape
    X = np.ascontiguousarray(x.reshape(B * S, Dm))
    cores = list(range(NCORES))
    for layer in range(4):
        inpL = {k: np.asarray(v[layer], dtype=np.float32) for k, v in inputs.items() if k not in ("x", "p")}
        xT = np.ascontiguousarray(X.T)
        res = run_bass_kernel_spmd(_prog("g"), [g_inputs(inpL, c, xT) for c in cores], core_ids=cores)
        og = [np.asarray(r["og"]) for r in res.results]
        res = run_bass_kernel_spmd(_prog("r"), [r_inputs(inpL, c, xT) for c in cores], core_ids=cores)
        orw = [np.asarray(r["orw"]) for r in res.results]
        oT = np.concatenate(og + orw, axis=0)
        del og, orw, xT
        pTl = p[layer].reshape(B * S, -1).T
        maps = []
        for c in cores:
            ts_ = slice(c * 1024, (c + 1) * 1024)
            maps.append(f_inputs(inpL, np.ascontiguousarray(oT[:, ts_]), np.ascontiguousarray(X[ts_]), np.ascontiguousarray(pTl[:, ts_])))
        res = run_bass_kernel_spmd(_prog("f"), maps, core_ids=cores)
        X = np.concatenate([np.asarray(r["xo"]) for r in res.results], axis=0)
        del maps, oT
    return np.ascontiguousarray(X.reshape(B, S, Dm)).astype(np.float32)
```

```python
import os

import contextlib
import numpy as np
import concourse.bass as bass
import concourse.mybir as mybir
from concourse.bass_utils import run_bass_kernel_spmd

F32 = mybir.dt.float32
BF16 = mybir.dt.bfloat16
AF = mybir.ActivationFunctionType
ALU = mybir.AluOpType
AX = mybir.AxisListType


class Buf:
    __slots__ = ("t", "last_w", "readers", "name")

    def __init__(self, t, name):
        self.t = t
        self.name = name
        self.last_w = None
        self.readers = []

    def __getitem__(self, idx):
        return self.t[idx]


class Op:
    __slots__ = ("eng", "fn", "deps", "sig", "semkey", "semval", "is_dma", "idx")


class Prog:
    NDMASEM = 6

    def __init__(self, nc):
        self.nc = nc
        self.ops = []
        self.stack = contextlib.ExitStack()
        self.dma_count = {}
        self.dma_ops = {}
        self.nbuf = 0

    def sb(self, shape, dtype=F32, name=None):
        self.nbuf += 1
        name = "s_" + (name or f"sb{self.nbuf}")
        t = self.stack.enter_context(self.nc.sbuf_tensor(name, list(shape), dtype))
        return Buf(t, name)

    def ps(self, shape, dtype=F32, name=None):
        self.nbuf += 1
        name = "p_" + (name or f"ps{self.nbuf}")
        t = self.stack.enter_context(self.nc.psum_tensor(name, list(shape), dtype))
        return Buf(t, name)

    def dram(self, name, shape, dtype=F32, kind="Internal"):
        t = self.nc.dram_tensor(name, list(shape), dtype, kind=kind)
        return Buf(t, name)

    def op(self, eng, fn, reads=(), writes=(), dma=False, ser=False):
        o = Op()
        o.eng = eng
        o.fn = fn
        o.is_dma = dma
        o.sig = False
        o.idx = len(self.ops)
        deps = set()
        for b in reads:
            if b is None:
                continue
            if b.last_w is not None:
                deps.add(b.last_w)
        for b in writes:
            if b.last_w is not None:
                deps.add(b.last_w)
            for r in b.readers:
                deps.add(r)
        if dma:
            n = self.dma_count.get(eng, 0)
            self.dma_count[eng] = n + 1
            lst = self.dma_ops.setdefault(eng, [])
            if n >= self.NDMASEM:
                deps.add(lst[n - self.NDMASEM])
            lst.append(o.idx)
            o.semkey = ("dma", eng, n % self.NDMASEM)
            o.semval = 16 * (n // self.NDMASEM + 1)
        else:
            o.semkey = ("eng", eng)
            o.semval = None
        pruned = set()
        for d in deps:
            od = self.ops[d]
            if (not od.is_dma) and od.eng == eng and eng == "pe" and not dma:
                continue
            pruned.add(d)
        if ser and getattr(self, "last_pe", None) is not None:
            pruned.add(self.last_pe)
        if eng == "pe" and not dma:
            self.last_pe = o.idx
        o.deps = pruned
        for b in reads:
            if b is not None:
                b.readers.append(o.idx)
        for b in writes:
            b.last_w = o.idx
            b.readers = []
        self.ops.append(o)
        return o

    def dma(self, q, out, in_, reads=(), writes=(), **kw):
        return self.op(q, lambda e: e.dma_start(out=out, in_=in_, **kw), reads, writes, dma=True)

    def mm(self, out_ap, lhsT, rhs, reads, writes, start=True, stop=True, ser=False, **kw):
        return self.op("pe", lambda e: e.matmul(out_ap, lhsT, rhs, start=start, stop=stop, **kw), reads, writes, ser=ser)

    def tr(self, out_ap, in_ap, ident_ap, reads, writes):
        return self.op("pe", lambda e: e.transpose(out_ap, in_ap, ident_ap), reads, writes)

    def act(self, out, in_, func, reads, writes, eng="act", **kw):
        return self.op(eng, lambda e: e.activation(out=out, in_=in_, func=func, **kw), reads, writes)

    def tt(self, eng, out, in0, in1, op, reads, writes):
        return self.op(eng, lambda e: e.tensor_tensor(out=out, in0=in0, in1=in1, op=op), reads, writes)

    def ts(self, eng, out, in0, s1, s2, op0, op1=None, reads=(), writes=(), **kw):
        if op1 is None:
            return self.op(eng, lambda e: e.tensor_scalar(out=out, in0=in0, scalar1=s1, scalar2=None, op0=op0, **kw), reads, writes)
        return self.op(eng, lambda e: e.tensor_scalar(out=out, in0=in0, scalar1=s1, scalar2=s2, op0=op0, op1=op1, **kw), reads, writes)

    def stt(self, eng, out, in0, scalar, in1, op0, op1, reads, writes):
        return self.op(eng, lambda e: e.scalar_tensor_tensor(out=out, in0=in0, scalar=scalar, in1=in1, op0=op0, op1=op1), reads, writes)

    def copy(self, eng, out, in_, reads, writes):
        if eng == "act":
            return self.op(eng, lambda e: e.activation(out=out, in_=in_, func=AF.Copy), reads, writes)
        return self.op(eng, lambda e: e.tensor_copy(out=out, in_=in_), reads, writes)

    def memset(self, eng, ap, val, writes):
        return self.op(eng, lambda e: e.memset(ap, val), (), writes)

    def emit(self, final_wait_ops=()):
        nc = self.nc
        ops = self.ops
        for o in ops:
            for d in o.deps:
                ops[d].sig = True
        for i in final_wait_ops:
            ops[i].sig = True
        cnt = {}
        for o in ops:
            if not o.is_dma and o.sig:
                cnt[o.eng] = cnt.get(o.eng, 0) + 1
                o.semval = cnt[o.eng]
        semkeys = sorted({o.semkey for o in ops if o.sig or o.is_dma}, key=str)
        sems = {}
        for k in semkeys:
            sems[k] = self.stack.enter_context(nc.semaphore("s_" + "_".join(str(x) for x in k)))
        waited = {}
        per_eng = {}
        for o in ops:
            need = {}
            for d in o.deps:
                od = ops[d]
                k = od.semkey
                if od.semval > need.get(k, 0):
                    need[k] = od.semval
            wl = []
            for k, v in need.items():
                if waited.get((o.eng, k), 0) >= v:
                    continue
                waited[(o.eng, k)] = v
                wl.append((k, v))
            per_eng.setdefault(o.eng, []).append((o, wl))
        fin = {}
        for i in final_wait_ops:
            od = ops[i]
            fin[od.semkey] = max(fin.get(od.semkey, 0), od.semval)
        self.stats = {e: len(l) for e, l in per_eng.items()}

        engmap = {"pe": "tensor", "act": "scalar", "dve": "vector", "pool": "gpsimd", "sp": "sync"}
        with nc.Block() as block:
            for ename, lst in per_eng.items():
                def body(eng, lst=lst, ename=ename):
                    for o, wl in lst:
                        for k, v in wl:
                            eng.wait_ge(sems[k], v)
                        ins = o.fn(eng)
                        if o.is_dma:
                            ins.then_inc(sems[o.semkey], 16)
                        elif o.sig:
                            ins.then_inc(sems[o.semkey], 1)
                    if ename == "sp":
                        for k, v in fin.items():
                            eng.wait_ge(sems[k], v)
                getattr(block, engmap[ename])(body)
            if "sp" not in per_eng and fin:
                def body2(eng):
                    for k, v in fin.items():
                        eng.wait_ge(sems[k], v)
                block.sync(body2)
        self.stack.close()


def view(ap, name="v"):
    return Buf(ap, name)


class SubBuf:
    __slots__ = ("p", "t", "name")

    def __init__(self, parent, ap, name="sub"):
        self.p = parent
        self.t = ap
        self.name = name

    @property
    def last_w(self):
        return self.p.last_w

    @last_w.setter
    def last_w(self, v):
        self.p.last_w = v

    @property
    def readers(self):
        return self.p.readers

    @readers.setter
    def readers(self, v):
        self.p.readers = v

    def __getitem__(self, idx):
        return self.t[idx]


D = 4096
T = 4096
NB = 256
NCH = NB // 64


def consts():
    k = np.arange(128)[:, None]
    i = np.arange(128)[None, :]
    c = {}
    c["ident"] = (k == i)
    c["Eblk"] = (k // 64) == (i // 64)
    s = k % 64
    t = i % 64
    c["MaskZ"] = np.where(i < 64, s < t, s <= t)
    c["ones"] = np.ones((128, 128))
    c["UU"] = np.where(i < 64, k < t, k <= t) & (k < 64)
    c["negUU"] = -1.0 * ((k <= t) & (k < 64))
    c["NegM"] = np.where(c["MaskZ"], 0.0, -30000.0)
    c["Dup"] = (k == t) & (k < 64)
    c["Urev"] = (k > i) & (k < 64) & (i < 64)
    c["Uinc"] = (k <= i) & (k < 64) & (i < 64)
    c["Dup1"] = (k == 64 + t)
    c["UblkI"] = ((k // 64) == (i // 64)) & (k <= i)
    c["SelL0"] = (k == 63) & (i >= 0)
    c["SelL1"] = (k == 127) & (i >= 0)
    c["sgn"] = np.where(i == 0, np.where(k < 64, -1.0, 0.0), np.where(k < 64, 0.0, 1.0))
    names = list(c.keys())
    arr = np.concatenate([np.asarray(c[n], dtype=np.float32) for n in names], axis=1)
    rmask = np.ones((128, NB), np.float32)
    rmask[:, ::64] = 0.0
    arr = np.concatenate([arr, rmask], axis=1)
    return names, np.ascontiguousarray(arr)


C_NAMES, C_ARR = consts()
NCST = C_ARR.shape[1]


def load_consts(P, cst_d):
    cst = P.sb([128, NCST], name="cst")
    P.dma("sp", cst[:], cst_d, writes=[cst])
    C = {n: cst[:, i * 128:(i + 1) * 128] for i, n in enumerate(C_NAMES)}
    C["rmask"] = cst[:, len(C_NAMES) * 128: len(C_NAMES) * 128 + NB]
    return cst, C


class CH:
    pass


class Engine:
    def __init__(self, P, n, cst, C, banks):
        self.P = P
        self.n = n
        self.cst = cst
        self.C = C
        bZ, bP, bQ, bY, bS = banks
        self.psZ = [SubBuf(bZ, bZ[:, j * 128:(j + 1) * 128], f"psZ{j}") for j in range(4)]
        self.psP = [SubBuf(bP, bP[0:64, j * 64:(j + 1) * 64], f"psP{j}") for j in range(8)]
        self.psQ = [SubBuf(bQ, bQ[0:64, j * 64:(j + 1) * 64], f"psQ{j}") for j in range(8)]
        self.psY = [SubBuf(bY, bY[0:64, j * 64:(j + 1) * 64], f"psY{j}") for j in range(8)]
        self.bS = bS
        self.Zm = [P.sb([128, 128], name=f"eZm{i}") for i in range(n)]
        self.Pm = [[P.sb([64, 64], name=f"eP{i}_{j}") for j in range(2)] for i in range(n)]
        self.Qm = [[P.sb([64, 64], name=f"eQ{i}_{j}") for j in range(2)] for i in range(n)]
        self.Ym = [[P.sb([64, 64], name=f"eY{i}_{j}") for j in range(2)] for i in range(n)]
        self.ZK = [P.sb([128, 64], name=f"eZK{i}") for i in range(n)]
        for i in range(n):
            P.memset("pool", self.ZK[i][:], 0.0, [self.ZK[i]])

    def phaseA(self, chs):
        P, C, cst = self.P, self.C, self.cst
        assert len(chs) <= self.n
        for i, ch in enumerate(chs):
            pz = self.psZ[i % 4]
            ch.Zm = self.Zm[i]
            P.mm(pz[:], ch.LT, ch.RT, ch.rd, [pz])
            P.tt("dve", ch.Zm[:], pz[:], ch.Dm, ALU.mult, [pz] + ch.Dm_rd, [ch.Zm])
            ch.ZK = self.ZK[i]
            P.copy("pool", ch.ZK[64:128, :], ch.Zm[64:128, 0:64], [ch.Zm], [ch.ZK])
        for i, ch in enumerate(chs):
            P.tr(self.psQ[i][:], ch.Zm[0:64, 0:64], C["ident"][0:64, 0:64], [ch.Zm, cst], [self.psQ[i]])
        for i, ch in enumerate(chs):
            P.copy("act", self.Qm[i][0][:], self.psQ[i][:], [self.psQ[i]], [self.Qm[i][0]])
            P.tt("pool", self.Ym[i][0][:], ch.Zm[0:64, 0:64], C["ident"][0:64, 0:64], ALU.add, [ch.Zm, cst], [self.Ym[i][0]])
        for k in range(1, 6):
            a, b = (k - 1) % 2, k % 2
            for i, ch in enumerate(chs):
                Pp = ch.Zm[0:64, 0:64] if k == 1 else self.Pm[i][a][:]
                Pb = ch.Zm if k == 1 else self.Pm[i][a]
                Qp = self.Qm[i][a]
                if k < 5:
                    P.mm(self.psP[i][:], Qp[:], Pp, [Qp, Pb], [self.psP[i]])
                P.mm(self.psQ[i][:], Pp, Qp[:], [Qp, Pb], [self.psQ[i]])
            for i, ch in enumerate(chs):
                P.copy("act", self.Qm[i][b][:], self.psQ[i][:], [self.psQ[i]], [self.Qm[i][b]])
                if k < 5:
                    P.copy("dve", self.Pm[i][b][:], self.psP[i][:], [self.psP[i]], [self.Pm[i][b]])
            for i, ch in enumerate(chs):
                P.mm(self.psY[i][:], self.Qm[i][b][:], self.Ym[i][a][:], [self.Qm[i][b], self.Ym[i][a]], [self.psY[i]])
            for i, ch in enumerate(chs):
                P.tt("dve", self.Ym[i][b][:], self.psY[i][:], self.Ym[i][a][:], ALU.add, [self.psY[i], self.Ym[i][a]], [self.Ym[i][b]])
        for i, ch in enumerate(chs):
            ch.Tt = self.Ym[i][1]


def scan_bufs(P, bS, nh, Nv):
    sb = []
    for h in range(nh):
        o = CH()
        o.Xs = P.sb([64, Nv], name=f"sXs{h}")
        o.Mt = P.sb([128, 128], name=f"sMt{h}")
        sb.append(o)
    return sb


PB = 99


def phaseB(P, chs, scb, bS_slots, groups):
    if PB < -1:
        return
    for i, ch in enumerate(chs):
        psX = bS_slots[i][0]
        Mr = ch.M[ch.Mrows, :]
        P.mm(psX[:], ch.AT, ch.M[:, :], ch.rd + [ch.M], [psX], start=True, stop=False)
        P.mm(psX[:], ch.ZK[:, :], ch.UV[:, ch.UVc], [ch.ZK, ch.UV], [psX], start=False, stop=True)
    if PB < 0:
        return
    for i, ch in enumerate(chs):
        psX = bS_slots[i][0]
        P.copy("act", scb[i].Xs[:], psX[:], [psX], [scb[i].Xs])
    if PB < 3:
        return
    for i, ch in enumerate(chs):
        psU = bS_slots[i][1]
        P.mm(psU[:], ch.Tt[:], scb[i].Xs[:], [ch.Tt, scb[i].Xs], [psU])
    for i, ch in enumerate(chs):
        psU = bS_slots[i][1]
        P.copy("dve", ch.UV[0:64, ch.UVc], psU[:], [psU], [ch.UV])
    if PB < 5:
        return
    for i, ch in enumerate(chs):
        Mr = ch.M[ch.Mrows, :]
        P.mm(ch.yout, ch.RrT, ch.M[:, :], ch.rd + [ch.M], [ch.yout_b], start=True, stop=False)
        P.mm(ch.yout, ch.Zm[:, 64:128], ch.UV[:, ch.UVc], [ch.Zm, ch.UV], [ch.yout_b], start=False, stop=True)
    if PB < 6:
        return
    for (BK, BK_rd, UVb, psM) in groups:
        P.mm(psM[:], BK, UVb[:], BK_rd + [UVb], [psM])
    if PB < 7:
        return
    for i, ch in enumerate(chs):
        psM = groups[ch.gid][3]
        Mr = ch.M[ch.Mrows, :]
        if ch.Mrows == slice(0, 128):
            P.copy("act", scb[i].Mt[:], psM[:], [psM], [scb[i].Mt])
            P.stt("dve", Mr, Mr, ch.gC, scb[i].Mt[:], ALU.mult, ALU.add, [ch.M, scb[i].Mt] + ch.gC_rd, [ch.M])
            continue
        else:
            P.act(Mr, Mr, AF.Copy, [ch.M] + ch.gC_rd, [ch.M], scale=ch.gC)
        P.tt("dve", Mr, Mr, psM[ch.Mblk[0], ch.Mblk[1]], ALU.add, [ch.M, psM], [ch.M])


def build_r(nc, nblk, NTOK, stage=99):
    xT = nc.dram_tensor("xT", [D, NTOK], F32, kind="ExternalInput").ap()
    wr = nc.dram_tensor("wr", [D, 1024], F32, kind="ExternalInput").ap()
    lora_d = nc.dram_tensor("lora", [128, 1024], F32, kind="ExternalInput").ap()
    prm_d = nc.dram_tensor("prm", [128, 24], F32, kind="ExternalInput").ap()
    cst_d = nc.dram_tensor("cst", [128, NCST], F32, kind="ExternalInput").ap()
    orw = nc.dram_tensor("orw", [256, NTOK], F32, kind="ExternalOutput").ap()
    P = Prog(nc)
    fin = []
    cst, C = load_consts(P, cst_d)
    prm = P.sb([128, 24], name="prm")
    P.dma("sp", prm[:], prm_d, writes=[prm])
    lora = P.sb([128, 1024], name="lora")
    P.dma("sp", lora[:], lora_d, writes=[lora])
    om = P.sb([128, 2], name="om")
    for fg in range(2):
        P.ts("dve", om[:, fg:fg + 1], prm[:, fg * 10 + 6:fg * 10 + 7], -1.0, 1.0, ALU.mult, ALU.add, reads=[prm], writes=[om])

    stg = [P.sb([128, 4, 256], F32, name=f"stg{i}") for i in range(2)]
    wrb = P.sb([128, 32, 4, 256], BF16, name="wrb")
    wr_v = wr.rearrange("(c p) (a n) -> p c a n", p=128, a=4)
    for g in range(32):
        st = stg[g % 2]
        P.dma("sp" if g % 2 == 0 else "pool", st[:], wr_v[:, g], writes=[st])
        P.copy("act" if g % 2 == 0 else "dve", wrb[:, g], st[:], [st], [wrb])

    if stage == 0:
        tmp0 = P.sb([128, 256], F32, name="tmp0")
        P.copy("act", tmp0[:], wrb[:, 31, 3, :], [wrb], [tmp0])
        fin.append(P.dma("sp", orw[0:128, 0:256], tmp0[:], reads=[tmp0]).idx)
        P.emit(final_wait_ops=fin)
        return nc, P
    xb_t = [P.sb([128, 32, NB], BF16, name=f"xb{i}") for i in range(2)]
    xb_v = [[view(xb_t[p][:, g * 4:(g + 1) * 4, :], f"xb{p}_{g}") for g in range(8)] for p in range(2)]
    xT_v = xT.rearrange("(c p) t -> p c t", p=128)

    CG = [(0, 0, 128), (0, 128, 128), (1, 0, 128), (1, 128, 128), (2, 0, 128), (2, 128, 128), (3, 0, 128), (3, 128, 128)]
    MUCOL = [0, 10, 1, 11, 2, 12, 20, 21]
    hbuf = [P.sb([128, NB + 1], name=f"hbuf{c}") for c in range(8)]
    hs = [P.sb([128, NB], name=f"hs{c}") for c in range(8)]
    dtmp = [P.sb([128, NB], name=f"dtmp{c}") for c in range(2)]

    banks = [P.ps([128, 512], name=f"bank{i}") for i in range(8)]
    psA = [SubBuf(banks[0], banks[0][:, 0:256], "psA0"), SubBuf(banks[1], banks[1][:, 0:256], "psA1")]
    psL = [SubBuf(banks[7], banks[7][:, 0:256], "psL0"), SubBuf(banks[7], banks[7][:, 256:512], "psL1")]
    eng = Engine(P, 8, cst, C, banks[2:7])
    bS = banks[6]
    bT_ = banks[2]
    psT = [SubBuf(bT_, bT_[:, j * 128:(j + 1) * 128], f"psT{j}") for j in range(4)]
    bS_slots = []
    for h in range(2):
        bS_slots.append([SubBuf(bS, bS[0:64, (h * 2 + 0) * 64:(h * 2 + 1) * 64], f"psX{h}"),
                         SubBuf(bS, bS[0:64, (h * 2 + 1) * 64:(h * 2 + 2) * 64], f"psU{h}")])
    psMg = SubBuf(bS, bS[:, 256:384], "psMg")
    psYc = [SubBuf(bS, bS[0:64, 384 + 0:384 + 128], "psYc")]
    scb = scan_bufs(P, bS, 2, 64)

    def mk(shape, name, dt=F32):
        return P.sb(shape, dt, name=name)
    txw = mk([128, NB], "txw"); sxg = mk([128, NB], "sxg")
    lw = mk([128, NB], "lw"); aS = mk([128, NB], "aS"); gT = mk([128, NB], "gT")
    kkr = mk([128, NB], "kkr"); sq = mk([128, NB], "sq"); rinv = mk([128, NB], "rinv"); kk = mk([128, NB], "kk")
    t1 = mk([128, NB], "t1"); kp = mk([128, NB], "kp"); bT = mk([128, NB], "bT"); rk = mk([128, NB], "rk")
    bonus = mk([128, NB], "bonus"); G = mk([128, NB], "G"); Gm = mk([128, NB], "Gm")
    eG = mk([128, NB], "eG"); eGm = mk([128, NB], "eGm"); eNG = mk([128, NB], "eNG"); eRev = mk([128, NB], "eRev")
    LT2 = mk([128, NCH, 128], "LT2"); RT2 = mk([128, NCH, 128], "RT2"); BKT2 = mk([128, NCH, 128], "BKT2"); VT2 = mk([128, NCH, 128], "VT2")
    P.memset("pool", VT2[:], 0.0, [VT2])
    LTz = [mk([128, NCH, 128], f"LTz{h}") for h in range(2)]
    RTz = [mk([128, NCH, 128], f"RTz{h}") for h in range(2)]
    BKh = [mk([128, 128], f"BKh{c}") for c in range(NCH)]
    UV = [mk([128, 128], f"UV{c}") for c in range(NCH)]
    for c in range(NCH):
        P.memset("pool", UV[c][:], 0.0, [UV[c]])
    Mst = [mk([128, 64], f"Mst{fg}") for fg in range(2)]
    Ytm = [mk([64, 128], f"Ytm{c}") for c in range(NCH)]; yc = [mk([64, 128], f"yc{c}") for c in range(NCH)]; yn = [mk([64, 128], f"yn{c}") for c in range(NCH)]
    msum = [mk([64, 2], f"msum{c}") for c in range(NCH)]; nmean = [mk([64, 2], f"nmean{c}") for c in range(NCH)]
    vs = [mk([64, 2], f"vs{c}") for c in range(NCH)]; rstd = [mk([64, 2], f"rstd{c}") for c in range(NCH)]
    fin1 = [mk([128, 64], f"fin1{c}") for c in range(NCH)]
    oblk = [mk([128, NB], f"oblk{fg}") for fg in range(2)]

    def c3(ap):
        return ap.rearrange("p (c t) -> p c t", c=NCH)

    for blk in range(nblk):
        par = blk % 2
        tok0 = blk * NB
        seq_start = (tok0 % T) == 0
        for g in range(8):
            st = stg[g % 2]
            P.dma("sp" if g % 2 == 0 else "pool", st[:], xT_v[:, g * 4:(g + 1) * 4, tok0:tok0 + NB], writes=[st])
            P.copy("pool", xb_v[par][g][:], st[:], [st], [xb_v[par][g]])
        xb = xb_t[par]
        xbr = xb_v[par]
        if seq_start:
            for fg in range(2):
                P.memset("pool", Mst[fg][:], 0.0, [Mst[fg]])
        for cg in range(8):
            a_, off, M = CG[cg]
            ps = psA[cg % 2]
            for dc in range(32):
                P.mm(ps[0:M, :], wrb[:, dc, a_, off:off + M], xb[:, dc, :], [wrb, xbr[dc // 4]], [ps], start=(dc == 0), stop=(dc == 31))
            hb = hbuf[cg]
            if seq_start:
                P.memset("pool", hb[:, 0:1], 0.0, [hb])
            P.copy("act", hb[0:M, 1:NB + 1], ps[0:M, :], [ps], [hb])
            dt_ = dtmp[cg % 2]
            P.tt("dve", dt_[0:M, :], hb[0:M, 0:NB], hb[0:M, 1:NB + 1], ALU.subtract, [hb], [dt_])
            mc = MUCOL[cg]
            P.stt("dve", hs[cg][0:M, :], dt_[0:M, :], prm[0:M, mc:mc + 1], hb[0:M, 1:NB + 1], ALU.mult, ALU.add, [dt_, prm, hb], [hs[cg]])
            P.copy("pool", hb[0:M, 0:1], hb[0:M, NB:NB + 1], [hb], [hb])
        if stage == 1:
            fin.append(P.dma("sp", orw[0:128, tok0:tok0 + NB], hs[0][:], reads=[hs[0]]).idx)
            fin.append(P.dma("sp", orw[128:256, tok0:tok0 + NB], hs[3][:], reads=[hs[3]]).idx)
            continue
        P.act(txw[:], hs[6][:], AF.Tanh, [hs[6]], [txw])
        P.act(sxg[:], hs[7][:], AF.Sigmoid, [hs[7]], [sxg])
        for fg in range(2):
            pb = fg * 10
            rT = hs[fg]; kT = hs[2 + fg]; vT = hs[4 + fg]
            fc = slice(fg * 128, (fg + 1) * 128)
            pl = psL[0]
            P.mm(pl[:], lora[:, fc], txw[:], [lora, txw], [pl])
            P.act(lw[:], pl[:], AF.Sigmoid, [pl, prm], [lw], bias=prm[:, pb + 3:pb + 4])
            P.ts("pool", lw[:], lw[:], -0.6065306597126334, None, ALU.mult, reads=[lw], writes=[lw])
            pl = psL[1]
            P.mm(pl[:], lora[:, 256 + fg * 128:256 + (fg + 1) * 128], hs[6][:], [lora, hs[6]], [pl], start=True, stop=False)
            P.mm(pl[:], lora[:, 512 + fg * 128:512 + (fg + 1) * 128], hs[7][:], [lora, hs[7]], [pl], start=False, stop=True)
            P.act(aS[:], pl[:], AF.Sigmoid, [pl, prm], [aS], bias=prm[:, pb + 4:pb + 5])
            pl = psL[0]
            P.mm(pl[:], lora[:, 768 + fg * 128:768 + (fg + 1) * 128], sxg[:], [lora, sxg], [pl])
            P.copy("act", gT[:], pl[:], [pl], [gT])
            P.ts("pool", kkr[:], kT[:], prm[:, pb + 5:pb + 6], None, ALU.mult, reads=[kT, prm], writes=[kkr])
            P.tt("pool", sq[:], kkr[:], kkr[:], ALU.mult, [kkr], [sq])
            pl = psL[1]
            P.mm(pl[:], C["Eblk"], sq[:], [cst, sq], [pl])
            P.ts("dve", rinv[:], pl[:], 1e-6, None, ALU.add, reads=[pl], writes=[rinv])
            P.act(rinv[:], rinv[:], AF.Ln, [rinv], [rinv])
            P.act(rinv[:], rinv[:], AF.Exp, [rinv], [rinv], scale=-0.5)
            P.tt("pool", kk[:], kkr[:], rinv[:], ALU.mult, [kkr, rinv], [kk])
            P.ts("dve", t1[:], aS[:], prm[:, pb + 6:pb + 7], om[:, fg:fg + 1], ALU.mult, ALU.add, reads=[aS, prm, om], writes=[t1])
            P.tt("pool", kp[:], t1[:], kT[:], ALU.mult, [t1, kT], [kp])
            P.tt("pool", bT[:], kk[:], aS[:], ALU.mult, [kk, aS], [bT])
            P.stt("dve", rk[:], rT[:], prm[:, pb + 7:pb + 8], kp[:], ALU.mult, ALU.mult, [rT, prm, kp], [rk])
            pl = psL[0]
            P.mm(pl[:], C["Eblk"], rk[:], [cst, rk], [pl])
            P.tt("dve", bonus[:], pl[:], vT[:], ALU.mult, [pl, vT], [bonus])
            P.op("dve", lambda e: e.tensor_tensor_scan(out=G[:], data0=C["rmask"], data1=lw[:], initial=0.0, op0=ALU.mult, op1=ALU.add),
                 [cst, lw], [G])
            P.tt("pool", Gm[:], G[:], lw[:], ALU.subtract, [G, lw], [Gm])
            P.act(eG[:], G[:], AF.Exp, [G], [eG])
            P.act(eGm[:], Gm[:], AF.Exp, [Gm], [eGm])
            P.act(eNG[:], G[:], AF.Exp, [G], [eNG], scale=-1.0)
            for c in range(NCH):
                cs_ = slice(c * 64, (c + 1) * 64)
                P.act(eRev[:, cs_], G[:, cs_], AF.Exp, [G], [eRev], scale=-1.0, bias=G[:, c * 64 + 63:c * 64 + 64])
            if stage == 2:
                dbg = {0: lw, 1: aS}[fg]
                fin.append(P.dma("sp", orw[fc, tok0:tok0 + NB], dbg[:], reads=[dbg]).idx)
                continue
            P.tt("dve", LT2[:, :, 0:64], c3(bT[:]), c3(eNG[:]), ALU.mult, [bT, eNG], [LT2])
            P.tt("pool", LT2[:, :, 64:128], c3(kp[:]), c3(eNG[:]), ALU.mult, [kp, eNG], [LT2])
            P.stt("dve", RT2[:, :, 0:64], c3(kk[:]), -1.0, c3(eGm[:]), ALU.mult, ALU.mult, [kk, eGm], [RT2])
            P.tt("pool", RT2[:, :, 64:128], c3(rT[:]), c3(eG[:]), ALU.mult, [rT, eG], [RT2])
            P.tt("dve", BKT2[:, :, 0:64], c3(bT[:]), c3(eRev[:]), ALU.mult, [bT, eRev], [BKT2])
            P.tt("pool", BKT2[:, :, 64:128], c3(kp[:]), c3(eRev[:]), ALU.mult, [kp, eRev], [BKT2])
            P.copy("pool", VT2[:, :, 64:128], c3(vT[:]), [vT], [VT2])
            for h in range(2):
                hm = C["Eblk"][:, h * 64:h * 64 + 1]
                P.ts("pool", LTz[h][:], LT2[:], hm, None, ALU.mult, reads=[LT2, cst], writes=[LTz[h]])
                P.ts("pool", RTz[h][:], RT2[:], hm, None, ALU.mult, reads=[RT2, cst], writes=[RTz[h]])
            for c in range(NCH):
                pt = psT[c % 4]
                P.tr(pt[:], BKT2[:, c, :], C["ident"], [BKT2, cst], [pt])
                P.copy("act", BKh[c][:], pt[:], [pt], [BKh[c]])
            for c in range(NCH):
                pt = psT[c % 4]
                P.tr(pt[:], VT2[:, c, :], C["ident"], [VT2, cst], [pt])
                P.copy("act", UV[c][64:128, :], pt[64:128, :], [pt], [UV[c]])
            chs = []
            for c in range(NCH):
                for h in range(2):
                    ch = CH()
                    hp = slice(h * 64, (h + 1) * 64)
                    ch.LT = LTz[h][:, c, :]; ch.RT = RT2[:, c, :]; ch.rd = [LTz[h], RTz[h], RT2]
                    ch.Dm = C["MaskZ"]; ch.Dm_rd = [cst]
                    ch.AT = RTz[h][:, c, 0:64]; ch.RrT = RTz[h][:, c, 64:128]
                    ch.M = Mst[fg]; ch.Mrows = hp
                    ch.UV = UV[c]; ch.UVc = slice(h * 64, (h + 1) * 64)
                    ch.gid = 0; ch.Mblk = (hp, hp)
                    ch.gC = eG[hp, c * 64 + 63:c * 64 + 64]; ch.gC_rd = [eG]
                    ch.yout = psYc[0][:, h * 64:(h + 1) * 64]; ch.yout_b = psYc[0]
                    chs.append(ch)
            eng.phaseA(chs)
            if stage == 3:
                for c in range(NCH):
                    fin.append(P.dma("sp", orw[fg * 128:fg * 128 + 64, tok0 + c * 64:tok0 + (c + 1) * 64], chs[2 * c].Tt[:], reads=[chs[2 * c].Tt]).idx)
                    fin.append(P.dma("sp", orw[fg * 128 + 64:fg * 128 + 128, tok0 + c * 64:tok0 + (c + 1) * 64], chs[2 * c].Zm[0:64, 0:64], reads=[chs[2 * c].Zm]).idx)
                continue
            for c in range(NCH):
                phaseB(P, chs[2 * c:2 * c + 2], scb, bS_slots, [(BKh[c][:], [BKh[c]], UV[c], psMg)])
                P.copy("act", Ytm[c][:], psYc[0][:], [psYc[0]], [Ytm[c]])
            R4 = range(NCH)
            for c in R4:
                Y3 = Ytm[c][:].rearrange("p (h v) -> p h v", h=2)
                P.op("dve", lambda e, Y3=Y3, c=c: e.tensor_reduce(out=msum[c][:], in_=Y3, axis=AX.X, op=ALU.add), [Ytm[c]], [msum[c]])
            for c in R4:
                P.ts("dve", nmean[c][:], msum[c][:], -1.0 / 64, None, ALU.mult, reads=[msum[c]], writes=[nmean[c]])
            for c in R4:
                for h in range(2):
                    hc = slice(h * 64, (h + 1) * 64)
                    P.ts("dve", yc[c][:, hc], Ytm[c][:, hc], nmean[c][:, h:h + 1], None, ALU.add, reads=[Ytm[c], nmean[c]], writes=[yc[c]])
            for c in R4:
                for h in range(2):
                    hc = slice(h * 64, (h + 1) * 64)
                    P.op("act", lambda e, hc=hc, h=h, c=c: e.activation(out=yn[c][:, hc], in_=yc[c][:, hc], func=AF.Square, accum_out=vs[c][:, h:h + 1]),
                         [yc[c]], [yn[c], vs[c]])
            for c in R4:
                P.ts("dve", rstd[c][:], vs[c][:], 1.0 / 64, 64e-5, ALU.mult, ALU.add, reads=[vs[c]], writes=[rstd[c]])
            for c in R4:
                P.act(rstd[c][:], rstd[c][:], AF.Ln, [rstd[c]], [rstd[c]])
            for c in R4:
                P.act(rstd[c][:], rstd[c][:], AF.Exp, [rstd[c]], [rstd[c]], scale=-0.5)
            for c in R4:
                for h in range(2):
                    hc = slice(h * 64, (h + 1) * 64)
                    P.ts("dve", yn[c][:, hc], yc[c][:, hc], rstd[c][:, h:h + 1], None, ALU.mult, reads=[yc[c], rstd[c]], writes=[yn[c]])
            for c in R4:
                P.tr(psT[c % 4][:, 0:64], yn[c][:], C["ident"][0:64, 0:64], [yn[c], cst], [psT[c % 4]])
            for c in R4:
                P.ts("dve", fin1[c][:], psT[c % 4][:, 0:64], prm[:, pb + 8:pb + 9], prm[:, pb + 9:pb + 10], ALU.mult, ALU.add, reads=[psT[c % 4], prm], writes=[fin1[c]])
            for c in R4:
                cs_ = slice(c * 64, (c + 1) * 64)
                P.tt("pool", fin1[c][:], fin1[c][:], bonus[:, cs_], ALU.add, [fin1[c], bonus], [fin1[c]])
            for c in R4:
                cs_ = slice(c * 64, (c + 1) * 64)
                P.tt("pool", oblk[fg][:, cs_], fin1[c][:], gT[:, cs_], ALU.mult, [fin1[c], gT], [oblk[fg]])
            fin.append(P.dma("sp", orw[fc, tok0:tok0 + NB], oblk[fg][:], reads=[oblk[fg]]).idx)
    P.emit(final_wait_ops=fin)
    return nc, P


def r_inputs(inp, core, xT):
    GC = 8224
    w_in = inp["w_in"]
    f0 = core * 256
    cols = []
    for base in (0, 2048, 4096):
        cols.append(np.arange(GC + base + f0, GC + base + f0 + 256))
    cols.append(np.arange(GC + 6144, GC + 6400))
    cols = np.concatenate(cols)
    wr = np.ascontiguousarray(w_in[:, cols])
    lora = np.zeros((128, 1024), np.float32)
    lora[0:96, 0:256] = inp["rwkv_w2"][:, f0:f0 + 256]
    lora[96:128, 256:512] = inp["rwkv_a2"][0:32, f0:f0 + 256]
    lora[0:64, 512:768] = inp["rwkv_a2"][32:96, f0:f0 + 256]
    lora[64:128, 768:1024] = inp["rwkv_g2"][:, f0:f0 + 256]
    mu = inp["rwkv_mu"]
    prm = np.zeros((128, 24), np.float32)
    for fg in range(2):
        fs = slice(f0 + fg * 128, f0 + (fg + 1) * 128)
        pb = fg * 10
        prm[:, pb + 0] = mu[0:2048][fs]
        prm[:, pb + 1] = mu[2048:4096][fs]
        prm[:, pb + 2] = mu[4096:6144][fs]
        prm[:, pb + 3] = inp["rwkv_w0"][fs]
        prm[:, pb + 4] = inp["rwkv_a0"][fs]
        prm[:, pb + 5] = inp["rwkv_k_k"][fs]
        prm[:, pb + 6] = inp["rwkv_k_a"][fs]
        prm[:, pb + 7] = inp["rwkv_r_k"].reshape(-1)[fs]
        prm[:, pb + 8] = inp["rwkv_lnx_g"][fs]
        prm[:, pb + 9] = inp["rwkv_lnx_b"][fs]
    prm[:, 20] = mu[6144:6272]
    prm[:, 21] = mu[6272:6400]
    return {"xT": xT, "wr": wr, "lora": lora, "prm": prm, "cst": C_ARR}


def build_g(nc, nblk, NTOK, stage=99):
    xT = nc.dram_tensor("xT", [D, NTOK], F32, kind="ExternalInput").ap()
    wg = nc.dram_tensor("wg", [D, 1024], F32, kind="ExternalInput").ap()
    wab_d = nc.dram_tensor("wab", [128, 32, 4], F32, kind="ExternalInput").ap()
    cw_d = nc.dram_tensor("cw", [128, 24], F32, kind="ExternalInput").ap()
    prm_d = nc.dram_tensor("prm", [128, 8], F32, kind="ExternalInput").ap()
    cst_d = nc.dram_tensor("cst", [128, NCST], F32, kind="ExternalInput").ap()
    og = nc.dram_tensor("og", [256, NTOK], F32, kind="ExternalOutput").ap()
    P = Prog(nc)
    fin = []
    cst, C = load_consts(P, cst_d)
    cw = P.sb([128, 24], name="cw"); P.dma("sp", cw[:], cw_d, writes=[cw])
    prm = P.sb([128, 8], name="prm"); P.dma("sp", prm[:], prm_d, writes=[prm])
    wabf = P.sb([128, 32, 4], name="wabf"); P.dma("sp", wabf[:], wab_d, writes=[wabf])
    wab = P.sb([128, 32, 4], BF16, name="wab"); P.copy("dve", wab[:], wabf[:], [wabf], [wab])
    negA = P.sb([128, 2], name="negA")
    P.act(negA[:], prm[:, 0:2], AF.Exp, [prm], [negA])
    P.ts("dve", negA[:], negA[:], -1.0, None, ALU.mult, reads=[negA], writes=[negA])
    stg = [P.sb([128, 4, 256], F32, name=f"stg{i}") for i in range(2)]
    wgb = P.sb([128, 32, 4, 256], BF16, name="wgb")
    wg_v = wg.rearrange("(c p) (a n) -> p c a n", p=128, a=4)
    for g in range(32):
        st = stg[g % 2]
        P.dma("sp" if g % 2 == 0 else "pool", st[:], wg_v[:, g], writes=[st])
        P.copy("act" if g % 2 == 0 else "dve", wgb[:, g], st[:], [st], [wgb])
    xb_t = [P.sb([128, 32, NB], BF16, name=f"xb{i}") for i in range(2)]
    xb_v = [[view(xb_t[p][:, g * 4:(g + 1) * 4, :], f"xb{p}_{g}") for g in range(8)] for p in range(2)]
    xT_v = xT.rearrange("(c p) t -> p c t", p=128)

    banks = [P.ps([128, 512], name=f"bank{i}") for i in range(8)]
    psA = [SubBuf(banks[0], banks[0][:, 0:256], "psA0"), SubBuf(banks[1], banks[1][:, 0:256], "psA1")]
    psS = SubBuf(banks[1], banks[1][:, 256:512], "psS")
    eng = Engine(P, 8, cst, C, banks[2:7])
    bSA, bSB = banks[6], banks[7]
    psT = [SubBuf(banks[2], banks[2][:, j * 128:(j + 1) * 128], f"psT{j}") for j in range(4)]
    bS_slots = [[SubBuf(bSA, bSA[0:64, h * 128:(h + 1) * 128], f"psX{h}"), SubBuf(bSA, bSA[0:64, 256 + h * 128:256 + (h + 1) * 128], f"psU{h}")] for h in range(2)]
    psYo = [SubBuf(bSB, bSB[0:64, h * 128:(h + 1) * 128], f"psYo{h}") for h in range(2)]
    psMg = [SubBuf(bSB, bSB[:, 256 + h * 128:256 + (h + 1) * 128], f"psMg{h}") for h in range(2)]
    scb = scan_bufs(P, None, 2, 128)

    def mk(shape, name, dt=F32):
        return P.sb(shape, dt, name=name)
    hbuf = [mk([128, NB + 3], f"hbuf{c}") for c in range(6)]
    acc = [mk([128, NB], f"acc{c}") for c in range(2)]
    cs = [mk([128, NB], f"cs{c}") for c in range(6)]
    qk = [mk([128, NB], f"qk{c}") for c in range(4)]
    szT = [mk([128, NB], f"szT{c}") for c in range(2)]
    sq = mk([128, NB], "sq"); rn = mk([128, NB], "rn")
    ab = mk([128, 4], "ab"); vals = mk([128, 6], "vals"); g1 = mk([128, 2], "g1")
    st = [mk([128, 6], f"st{c}") for c in range(NCH)]
    gcb = [mk([128, 2], f"gcb{c}") for c in range(NCH)]
    eg = mk([128, 2], "eg"); tsc = mk([128, 2], "tsc")
    rs = [mk([128, 2], f"rs{c}") for c in range(NCH)]
    eGc = [mk([128, 2], f"eGc{c}") for c in range(NCH)]; eGm = mk([128, 2], "eGmc"); dG = mk([128, 2], "dG")
    eRv = mk([128, 2], "eRv"); bks = [mk([128, 2], f"bks{c}") for c in range(NCH)]; gCe = [mk([128, 2], f"gCe{c}") for c in range(NCH)]
    gB = mk([128, 128], "gB"); Dl = mk([128, 128], "Dl"); diagM = mk([128, 128], "diagM")
    Dm = [mk([128, 128], f"Dm{i}") for i in range(8)]
    LTg = [mk([128, 128], f"LTg{i}") for i in range(8)]
    RTg = [mk([128, 128], f"RTg{i}") for i in range(8)]
    RTu = [mk([128, 128], f"RTu{i}") for i in range(8)]
    VTg = mk([128, 128], "VTg"); P.memset("pool", VTg[:], 0.0, [VTg])
    BKh = [mk([128, 128], f"BKh{i}") for i in range(8)]
    UV = [mk([128, 128], f"UV{i}") for i in range(8)]
    for i in range(8):
        P.memset("pool", UV[i][:], 0.0, [UV[i]])
    Mst = [mk([128, 128], f"Mst{h}") for h in range(2)]
    o2 = [mk([64, 128], f"o2_{i}") for i in range(8)]; on = [mk([64, 128], f"on_{i}") for i in range(8)]
    ssq = [mk([64, 1], f"ssq{i}") for i in range(8)]; rstd = [mk([64, 1], f"rstd{i}") for i in range(8)]; f1 = [mk([128, 64], f"f1_{i}") for i in range(8)]
    oblk = [mk([128, NB], f"oblk{h}") for h in range(2)]

    for blk in range(nblk):
        par = blk % 2
        tok0 = blk * NB
        seq_start = (tok0 % T) == 0
        for g in range(8):
            s_ = stg[g % 2]
            P.dma("sp" if g % 2 == 0 else "pool", s_[:], xT_v[:, g * 4:(g + 1) * 4, tok0:tok0 + NB], writes=[s_])
            P.copy("pool", xb_v[par][g][:], s_[:], [s_], [xb_v[par][g]])
        xb = xb_t[par]; xbr = xb_v[par]
        if seq_start:
            for h in range(2):
                P.memset("pool", Mst[h][:], 0.0, [Mst[h]])
        for cg in range(8):
            ps = psA[cg % 2]
            for dc in range(32):
                P.mm(ps[:], wgb[:, dc, cg // 2, (cg % 2) * 128:(cg % 2 + 1) * 128], xb[:, dc, :], [wgb, xbr[dc // 4]], [ps], start=(dc == 0), stop=(dc == 31))
            if cg >= 6:
                P.act(szT[cg - 6][:], ps[:], AF.Silu, [ps], [szT[cg - 6]])
                continue
            hb = hbuf[cg]
            if seq_start:
                P.memset("pool", hb[:, 0:3], 0.0, [hb])
            P.copy("act", hb[:, 3:NB + 3], ps[:], [ps], [hb])
            a = acc[cg % 2]
            P.ts("dve", a[:], hb[:, 0:NB], cw[:, cg * 4:cg * 4 + 1], None, ALU.mult, reads=[hb, cw], writes=[a])
            for j in range(1, 4):
                P.stt("dve", a[:], hb[:, j:j + NB], cw[:, cg * 4 + j:cg * 4 + j + 1], a[:], ALU.mult, ALU.add, [hb, cw, a], [a])
            P.copy("pool", hb[:, 0:3], hb[:, NB:NB + 3], [hb], [hb])
            P.act(cs[cg][:], a[:], AF.Silu, [a], [cs[cg]])
        for cc in range(4):
            P.tt("pool", sq[:], cs[cc][:], cs[cc][:], ALU.mult, [cs[cc]], [sq])
            P.mm(psS[:], C["ones"], sq[:], [cst, sq], [psS])
            P.ts("dve", rn[:], psS[:], 1e-6, None, ALU.add, reads=[psS], writes=[rn])
            P.act(rn[:], rn[:], AF.Ln, [rn], [rn])
            P.act(rn[:], rn[:], AF.Exp, [rn], [rn], scale=-0.5)
            if cc < 2:
                P.stt("dve", qk[cc][:], cs[cc][:], float(128 ** -0.5), rn[:], ALU.mult, ALU.mult, [cs[cc], rn], [qk[cc]])
            else:
                P.tt("dve", qk[cc][:], cs[cc][:], rn[:], ALU.mult, [cs[cc], rn], [qk[cc]])
        if stage == 1:
            fin.append(P.dma("sp", og[0:128, tok0:tok0 + NB], qk[0][:], reads=[qk[0]]).idx)
            fin.append(P.dma("sp", og[128:256, tok0:tok0 + NB], qk[2][:], reads=[qk[2]]).idx)
            continue
        for s2 in range(NB // 128):
            tsl = slice(s2 * 128, (s2 + 1) * 128)
            pab = SubBuf(banks[1], banks[1][:, 256:260], "pab")
            for dc in range(32):
                P.mm(pab[:], xb[:, dc, tsl], wab[:, dc, :], [xbr[dc // 4], wab], [pab], start=(dc == 0), stop=(dc == 31))
            P.copy("dve", ab[:], pab[:], [pab], [ab])
            P.tt("dve", g1[:], ab[:, 0:2], prm[:, 2:4], ALU.add, [ab, prm], [g1])
            P.act(g1[:], g1[:], AF.Exp, [g1], [g1])
            P.ts("dve", g1[:], g1[:], 1.0, None, ALU.add, reads=[g1], writes=[g1])
            P.act(g1[:], g1[:], AF.Ln, [g1], [g1])
            P.tt("dve", vals[:, 0:2], g1[:], negA[:], ALU.mult, [g1, negA], [vals])
            P.act(vals[:, 4:6], ab[:, 2:4], AF.Sigmoid, [ab], [vals])
            pG = SubBuf(banks[1], banks[1][:, 264:266], "pG")
            P.mm(pG[:], C["UblkI"], vals[:, 0:2], [cst, vals], [pG])
            P.copy("dve", vals[:, 2:4], pG[:], [pG], [vals])
            for c2 in range(2):
                c = s2 * 2 + c2
                pst = SubBuf(banks[1], banks[1][:, 272:278], "pst")
                P.mm(pst[:], C["Dup" if c2 == 0 else "Dup1"], vals[:], [cst, vals], [pst])
                P.copy("dve", st[c][:], pst[:], [pst], [st[c]])
                pgc = SubBuf(banks[1], banks[1][:, 280:282], "pgc")
                P.mm(pgc[:], C["SelL0"], st[c][:, 2:4], [cst, st[c]], [pgc])
                P.copy("dve", gcb[c][:], pgc[:], [pgc], [gcb[c]])
                P.act(eg[:], st[c][:, 0:2], AF.Exp, [st[c]], [eg])
                P.ts("dve", tsc[:], eg[:], C["sgn"][:, 0:1], C["sgn"][:, 1:2], ALU.mult, ALU.add, reads=[eg, cst], writes=[tsc])
                P.tt("dve", rs[c][:], tsc[:], st[c][:, 4:6], ALU.mult, [tsc, st[c]], [rs[c]])
                P.act(eGc[c][:], st[c][:, 2:4], AF.Exp, [st[c]], [eGc[c]])
                P.tt("dve", dG[:], st[c][:, 2:4], st[c][:, 0:2], ALU.subtract, [st[c]], [dG])
                P.act(eGm[:], dG[:], AF.Exp, [dG], [eGm])
                P.tt("dve", dG[:], gcb[c][:], st[c][:, 2:4], ALU.subtract, [gcb[c], st[c]], [dG])
                P.act(eRv[:], dG[:], AF.Exp, [dG], [eRv])
                P.tt("dve", bks[c][:], rs[c][:], eRv[:], ALU.mult, [rs[c], eRv], [bks[c]])
                P.act(gCe[c][:], gcb[c][:], AF.Exp, [gcb[c]], [gCe[c]])
                for h in range(2):
                    i = c * 2 + h
                    csl = slice(c * 64, (c + 1) * 64)
                    qT = qk[h]; kT = qk[2 + h]; vT = cs[4 + h]
                    P.ts("pool", gB[:], C["ones"], st[c][:, h:h + 1], None, ALU.mult, reads=[cst, st[c]], writes=[gB])
                    pd = psT[i % 4]
                    P.mm(pd[:], gB[:], C["UU"], [gB, cst], [pd], start=True, stop=False)
                    P.mm(pd[:], C["negUU"], gB[:], [gB, cst], [pd], start=False, stop=True)
                    P.tt("dve", Dl[:], pd[:], C["NegM"], ALU.add, [pd, cst], [Dl])
                    P.act(Dl[:], Dl[:], AF.Exp, [Dl], [Dl])
                    P.ts("dve", Dm[i][:], Dl[:], rs[c][:, h:h + 1], None, ALU.mult, reads=[Dl, rs[c]], writes=[Dm[i]])
                    P.ts("dve", diagM[:, 0:64], C["Dup"][:, 0:64], eGm[:, h:h + 1], None, ALU.mult, reads=[cst, eGm], writes=[diagM])
                    P.ts("dve", diagM[:, 64:128], C["Dup"][:, 64:128], eGc[c][:, h:h + 1], None, ALU.mult, reads=[cst, eGc[c]], writes=[diagM])
                    pb_ = psT[(i + 1) % 4]
                    P.mm(pb_[:], C["ones"], diagM[:], [cst, diagM], [pb_])
                    P.tt("dve", RTg[i][:, 0:64], pb_[:, 0:64], kT[:, csl], ALU.mult, [pb_, kT], [RTg[i]])
                    P.tt("dve", RTg[i][:, 64:128], pb_[:, 64:128], qT[:, csl], ALU.mult, [pb_, qT], [RTg[i]])
                    P.copy("pool", LTg[i][:, 0:64], kT[:, csl], [kT], [LTg[i]])
                    P.copy("pool", LTg[i][:, 64:128], kT[:, csl], [kT], [LTg[i]])
                    P.copy("pool", RTu[i][:, 0:64], kT[:, csl], [kT], [RTu[i]])
                    P.copy("pool", RTu[i][:, 64:128], qT[:, csl], [qT], [RTu[i]])
                    pt = psT[(i + 2) % 4]
                    P.tr(pt[:], LTg[i][:], C["ident"], [LTg[i], cst], [pt])
                    P.ts("dve", BKh[i][:], pt[:], bks[c][:, h:h + 1], None, ALU.mult, reads=[pt, bks[c]], writes=[BKh[i]])
                    P.copy("pool", VTg[:, 64:128], vT[:, csl], [vT], [VTg])
                    pt = psT[(i + 3) % 4]
                    P.tr(pt[:], VTg[:], C["ident"], [VTg, cst], [pt])
                    P.copy("act", UV[i][64:128, :], pt[64:128, :], [pt], [UV[i]])
        if stage == 2:
            fin.append(P.dma("sp", og[0:128, tok0:tok0 + 128], BKh[7][:], reads=[BKh[7]]).idx)
            fin.append(P.dma("sp", og[128:256, tok0:tok0 + 128], Dm[7][:], reads=[Dm[7]]).idx)
            fin.append(P.dma("sp", og[0:128, tok0 + 128:tok0 + 256], UV[7][:], reads=[UV[7]]).idx)
            fin.append(P.dma("sp", og[128:256, tok0 + 128:tok0 + 256], RTg[7][:], reads=[RTg[7]]).idx)
            continue
        chs = []
        for c in range(NCH):
            for h in range(2):
                i = c * 2 + h
                ch = CH()
                ch.LT = LTg[i][:]; ch.RT = RTu[i][:]; ch.rd = [LTg[i], RTg[i], RTu[i]]
                ch.Dm = Dm[i][:]; ch.Dm_rd = [Dm[i]]
                ch.AT = RTg[i][:, 0:64]; ch.RrT = RTg[i][:, 64:128]
                ch.M = Mst[h]; ch.Mrows = slice(0, 128)
                ch.UV = UV[i]; ch.UVc = slice(0, 128)
                ch.gid = h; ch.Mblk = (slice(0, 128), slice(0, 128))
                ch.gC = gCe[c][:, h:h + 1]; ch.gC_rd = [gCe[c]]
                ch.yout = psYo[h][:]; ch.yout_b = psYo[h]
                chs.append(ch)
        eng.phaseA(chs)
        for c in range(NCH):
            cc_ = chs[2 * c:2 * c + 2]
            phaseB(P, cc_, scb, bS_slots, [(BKh[2 * c + h][:], [BKh[2 * c + h]], UV[2 * c + h], psMg[h]) for h in range(2)])
            for h in range(2):
                P.copy("act", o2[2 * c + h][:], psYo[h][:], [psYo[h]], [o2[2 * c + h]])
        R8 = range(2 * NCH)
        for i in R8:
            P.op("act", lambda e, i=i: e.activation(out=on[i][:], in_=o2[i][:], func=AF.Square, accum_out=ssq[i][:]), [o2[i]], [on[i], ssq[i]])
        for i in R8:
            P.ts("dve", rstd[i][:], ssq[i][:], 1.0 / 128, 1e-6, ALU.mult, ALU.add, reads=[ssq[i]], writes=[rstd[i]])
        for i in R8:
            P.act(rstd[i][:], rstd[i][:], AF.Ln, [rstd[i]], [rstd[i]])
        for i in R8:
            P.act(rstd[i][:], rstd[i][:], AF.Exp, [rstd[i]], [rstd[i]], scale=-0.5)
        for i in R8:
            P.ts("dve", on[i][:], o2[i][:], rstd[i][:, 0:1], None, ALU.mult, reads=[o2[i], rstd[i]], writes=[on[i]])
        for i in R8:
            P.tr(psT[i % 4][:, 0:64], on[i][:], C["ident"][0:64, 0:64], [on[i], cst], [psT[i % 4]])
            P.ts("dve", f1[i][:], psT[i % 4][:, 0:64], prm[:, 4:5], None, ALU.mult, reads=[psT[i % 4], prm], writes=[f1[i]])
        for i in R8:
            c, h = i // 2, i % 2
            csl = slice(c * 64, (c + 1) * 64)
            P.tt("pool", oblk[h][:, csl], f1[i][:], szT[h][:, csl], ALU.mult, [f1[i], szT[h]], [oblk[h]])
        for h in range(2):
            fin.append(P.dma("sp", og[h * 128:(h + 1) * 128, tok0:tok0 + NB], oblk[h][:], reads=[oblk[h]]).idx)
    P.emit(final_wait_ops=fin)
    return nc, P


def g_inputs(inp, core, xT):
    w_in = inp["w_in"]
    h0, h1 = 2 * core, 2 * core + 1
    cols = []
    for base in (0, 2048, 4096, 6144):
        for h in (h0, h1):
            cols.append(np.arange(base + h * 128, base + (h + 1) * 128))
    wg = np.ascontiguousarray(w_in[:, np.concatenate(cols)])
    abc = np.array([8192 + h0, 8192 + h1, 8192 + 16 + h0, 8192 + 16 + h1])
    wab = np.ascontiguousarray(w_in[:, abc].reshape(32, 128, 4).transpose(1, 0, 2))
    cwl = inp["gdn_conv_w"]
    cw = np.zeros((128, 24), np.float32)
    cc = 0
    for base in (0, 2048, 4096):
        for h in (h0, h1):
            cw[:, cc * 4:(cc + 1) * 4] = cwl[:, base + h * 128: base + (h + 1) * 128].T
            cc += 1
    prm = np.zeros((128, 8), np.float32)
    prm[:, 0] = inp["gdn_a_log"][h0]; prm[:, 1] = inp["gdn_a_log"][h1]
    prm[:, 2] = inp["gdn_dt_bias"][h0]; prm[:, 3] = inp["gdn_dt_bias"][h1]
    prm[:, 4] = inp["gdn_norm_w"]
    return {"xT": xT, "wg": wg, "wab": wab, "cw": cw, "prm": prm, "cst": C_ARR}


DM = 4096
NE = 32
ALPHA = float((2 * 4) ** 0.25)
BIG = 10000.0


def f_consts():
    k = np.arange(128)[:, None]
    i = np.arange(128)[None, :]
    return np.ascontiguousarray(np.concatenate([(k == i), np.ones((128, 128))], axis=1).astype(np.float32))


CF_ARR = f_consts()


def build_f(nc, NTOKC=1024, NT=4, n_exp=NE, stage=99):
    TB = NT * 128
    nblk = NTOKC // TB
    oT = nc.dram_tensor("oT", [DM, NTOKC], F32, kind="ExternalInput").ap()
    xres = nc.dram_tensor("xres", [NTOKC, DM], F32, kind="ExternalInput").ap()
    pT = nc.dram_tensor("pT", [256, NTOKC], F32, kind="ExternalInput").ap()
    w_out = nc.dram_tensor("w_out", [DM, DM], F32, kind="ExternalInput").ap()
    lnp = nc.dram_tensor("lnp", [4, 128, DM], F32, kind="ExternalInput").ap()
    wrt = nc.dram_tensor("wrt", [DM, 36], F32, kind="ExternalInput").ap()
    brt = nc.dram_tensor("brt", [128, 36], F32, kind="ExternalInput").ap()
    wgate = nc.dram_tensor("wgate", [NE, DM, 256], F32, kind="ExternalInput").ap()
    wup = nc.dram_tensor("wup", [NE, DM, 256], F32, kind="ExternalInput").ap()
    wdown = nc.dram_tensor("wdown", [NE, 256, DM], F32, kind="ExternalInput").ap()
    pw_proj = nc.dram_tensor("pw_proj", [256, DM], F32, kind="ExternalInput").ap()
    pw_gd = nc.dram_tensor("pw_gd", [DM, 256], F32, kind="ExternalInput").ap()
    pw_gu = nc.dram_tensor("pw_gu", [256, DM], F32, kind="ExternalInput").ap()
    cst_d = nc.dram_tensor("cst", [128, 256], F32, kind="ExternalInput").ap()
    xo = nc.dram_tensor("xo", [NTOKC, DM], F32, kind="ExternalOutput").ap()
    P = Prog(nc)
    fin = []
    Y1 = [P.dram(f"Y1_{t}", [128, DM]) for t in range(NT)]
    X1 = [P.dram(f"X1_{t}", [128, DM]) for t in range(NT)]
    Y2 = [P.dram(f"Y2_{t}", [128, DM]) for t in range(NT)]

    cst = P.sb([128, 256], name="cst"); P.dma("sp", cst[:], cst_d, writes=[cst])
    ident = cst[:, 0:128]; ones = cst[:, 128:256]
    wr32 = P.sb([128, 32, 36], name="wr32")
    P.dma("sp", wr32[:], wrt.rearrange("(c p) n -> p c n", p=128), writes=[wr32])
    bias = P.sb([128, 36], name="bias"); P.dma("sp", bias[:], brt, writes=[bias])

    def mk(shape, name, dt=F32):
        return P.sb(shape, dt, name=name)
    actT = mk([128, 32, TB], "actT", BF16)
    hact = mk([128, 2 * NE, TB], "hact", BF16)
    wa = mk([128, DM], "wa")
    NWS = 3
    wst = [mk([128, 1024], f"wst{i}") for i in range(NWS)]
    wbf = [mk([128, 1024], f"wbf{i}", BF16) for i in range(NWS)]
    wcnt = [0]

    def wload(src, a):
        j = wcnt[0] % NWS
        wcnt[0] += 1
        st, bf = wst[j], wbf[j]
        q = "sp" if wcnt[0] % 2 else "pool"
        dst = st[:] if a == 1 else st[:].rearrange("p (a n) -> p a n", a=a)
        P.dma(q, dst, src, writes=[st])
        eng = ("act", "dve", "pool")[wcnt[0] % 3]
        P.copy(eng, bf[:], st[:], [st], [bf])
        return bf

    banks = [P.ps([128, 512], name=f"bank{i}") for i in range(8)]
    accA = banks[0:4]
    accB = banks[4:8]
    xs = [mk([128, 512], f"xs{i}") for i in range(2)]
    ev = [mk([128, 512], f"ev{i}") for i in range(2)]
    yv = [mk([128, 512], f"yv{i}") for i in range(2)]
    gpc = [mk([128, 512], f"gpc{i}") for i in range(2)]
    bpc = [mk([128, 512], f"bpc{i}") for i in range(2)]
    xf = [mk([128, 128], f"xf{i}") for i in range(4)]
    junk = mk([128, 512], "junk")
    s1 = mk([128, 1], "s1"); nmean = mk([128, 1], "nmean"); ssq8 = mk([128, 8], "ssq8"); ssq = mk([128, 1], "ssq"); rstd = mk([128, 1], "rstd")
    combT = mk([128, TB], "combT"); P.memset("pool", combT[:], 0.0, [combT])
    sel = [mk([128, 128], f"sel{i}") for i in range(2)]
    cb = mk([128, TB], "cb")
    sil = [mk([128, TB], f"sil{i}") for i in range(2)]
    tmu = [mk([128, TB], f"tmu{i}") for i in range(2)]
    pst = mk([128, 2, TB], "pst"); pTb = mk([128, 2, TB], "pTb", BF16); gdT = mk([128, 2, TB], "gdT", BF16)
    pr = [mk([128, 512], f"pr{i}") for i in range(2)]; sg = [mk([128, 512], f"sg{i}") for i in range(2)]
    lg = mk([128, 36], "lg"); gmax = mk([128, 1], "gmax"); ngmax = mk([128, 1], "ngmax"); gmask = mk([128, 4], "gmask")
    eg4 = mk([128, 4], "eg4"); gsum = mk([128, 1], "gsum"); pg = mk([128, 1], "pg"); pen = mk([128, 4], "pen")
    ml = mk([128, 32], "ml"); v1 = mk([128, 1], "v1"); m1 = mk([128, 32], "m1"); ml2 = mk([128, 32], "ml2")
    v2 = mk([128, 1], "v2"); m2 = mk([128, 32], "m2"); dd = mk([128, 1], "dd"); w1 = mk([128, 1], "w1"); w2 = mk([128, 1], "w2")
    g1 = mk([128, 1], "g1"); g2 = mk([128, 1], "g2"); comb = mk([128, 32], "comb"); comb2 = mk([128, 32], "comb2")

    oT_v = oT.rearrange("(c p) t -> p c t", p=128)
    w_out_v = w_out.rearrange("(c p) n -> p c n", p=128)
    pT_v = pT.rearrange("(c p) t -> p c t", p=128)

    def layer_norm_tile(src, which, t, tok0):
        P.dma("sp", wa[:], src[:], reads=[src], writes=[wa])
        P.op("dve", lambda e: e.tensor_reduce(out=s1[:], in_=wa[:], axis=AX.X, op=ALU.add), [wa], [s1])
        P.ts("dve", nmean[:], s1[:], -1.0 / DM, None, ALU.mult, reads=[s1], writes=[nmean])
        for ng in range(8):
            P.op("act", lambda e, ng=ng: e.activation(out=junk[:], in_=wa[:, ng * 512:(ng + 1) * 512], func=AF.Square,
                                                      bias=nmean[:, 0:1], accum_out=ssq8[:, ng:ng + 1]), [wa, nmean], [junk, ssq8])
        P.op("dve", lambda e: e.tensor_reduce(out=ssq[:], in_=ssq8[:], axis=AX.X, op=ALU.add), [ssq8], [ssq])
        P.ts("dve", rstd[:], ssq[:], 1.0 / DM, 1e-5, ALU.mult, ALU.add, reads=[ssq], writes=[rstd])
        P.act(rstd[:], rstd[:], AF.Ln, [rstd], [rstd])
        P.act(rstd[:], rstd[:], AF.Exp, [rstd], [rstd], scale=-0.5)
        psR = banks[5]
        for ng in range(8):
            gs = slice(ng * 512, (ng + 1) * 512)
            gp = gpc[ng % 2]; bp = bpc[ng % 2]; y = yv[ng % 2]; e_ = ev[ng % 2]
            P.dma("pool", gp[:], lnp[2 * which, :, gs], writes=[gp])
            P.dma("pool", bp[:], lnp[2 * which + 1, :, gs], writes=[bp])
            P.ts("dve", e_[:], wa[:, gs], nmean[:, 0:1], rstd[:, 0:1], ALU.add, ALU.mult, reads=[wa, nmean, rstd], writes=[e_])
            P.tt("pool", y[:], e_[:], gp[:], ALU.mult, [e_, gp], [y])
            P.tt("pool", y[:], y[:], bp[:], ALU.add, [y, bp], [y])
            if which == 1:
                fin.append(P.dma("sp", xo[tok0 + t * 128:tok0 + (t + 1) * 128, gs], y[:], reads=[y]).idx)
                continue
            P.dma("sp", X1[t][:, gs], y[:], reads=[y], writes=[X1[t]])
            for j in range(4):
                kc = ng * 4 + j
                pt = banks[4] if kc % 2 == 0 else banks[7]
                P.tr(pt[:, 0:128], y[:, j * 128:(j + 1) * 128], ident, [y, cst], [pt])
                f = xf[kc % 4]
                P.copy("act", f[:], pt[:, 0:128], [pt], [f])
                P.copy("pool", actT[:, kc, t * 128:(t + 1) * 128], f[:], [f], [actT])
                P.mm(psR[:, 0:36], f[:], wr32[:, kc, :], [f, wr32], [psR], start=(kc == 0), stop=(kc == 31))
        if which == 1:
            return
        P.tt("dve", lg[:], psR[:, 0:36], bias[:], ALU.add, [psR, bias], [lg])
        P.op("dve", lambda e: e.tensor_reduce(out=gmax[:], in_=lg[:, 0:4], axis=AX.X, op=ALU.max), [lg], [gmax])
        P.ts("dve", gmask[:], lg[:, 0:4], gmax[:, 0:1], None, ALU.is_ge, reads=[lg, gmax], writes=[gmask])
        P.ts("dve", ngmax[:], gmax[:], -1.0, None, ALU.mult, reads=[gmax], writes=[ngmax])
        P.op("act", lambda e: e.activation(out=eg4[:], in_=lg[:, 0:4], func=AF.Exp, bias=ngmax[:, 0:1], accum_out=gsum[:]), [lg, ngmax], [eg4, gsum])
        P.op("dve", lambda e: e.reciprocal(out=pg[:], in_=gsum[:]), [gsum], [pg])
        P.ts("dve", pen[:], gmask[:], BIG, -BIG, ALU.mult, ALU.add, reads=[gmask], writes=[pen])
        for g in range(4):
            P.ts("dve", ml[:, g * 8:(g + 1) * 8], lg[:, 4 + g * 8:4 + (g + 1) * 8], pen[:, g:g + 1], None, ALU.add, reads=[lg, pen], writes=[ml])
        P.op("dve", lambda e: e.tensor_reduce(out=v1[:], in_=ml[:], axis=AX.X, op=ALU.max), [ml], [v1])
        P.ts("dve", m1[:], ml[:], v1[:, 0:1], None, ALU.is_ge, reads=[ml, v1], writes=[m1])
        P.stt("dve", ml2[:], m1[:], -BIG, ml[:], ALU.mult, ALU.add, [m1, ml], [ml2])
        P.op("dve", lambda e: e.tensor_reduce(out=v2[:], in_=ml2[:], axis=AX.X, op=ALU.max), [ml2], [v2])
        P.ts("dve", m2[:], ml2[:], v2[:, 0:1], None, ALU.is_ge, reads=[ml2, v2], writes=[m2])
        P.tt("dve", dd[:], v1[:], v2[:], ALU.subtract, [v1, v2], [dd])
        P.act(w1[:], dd[:], AF.Sigmoid, [dd], [w1])
        P.ts("dve", w2[:], w1[:], -1.0, 1.0, ALU.mult, ALU.add, reads=[w1], writes=[w2])
        P.tt("dve", g1[:], w1[:], pg[:], ALU.mult, [w1, pg], [g1])
        P.tt("dve", g2[:], w2[:], pg[:], ALU.mult, [w2, pg], [g2])
        P.ts("dve", comb[:], m1[:], g1[:, 0:1], None, ALU.mult, reads=[m1, g1], writes=[comb])
        P.ts("dve", comb2[:], m2[:], g2[:, 0:1], None, ALU.mult, reads=[m2, g2], writes=[comb2])
        P.tt("dve", comb[:], comb[:], comb2[:], ALU.add, [comb, comb2], [comb])
        pt = banks[4]
        P.tr(pt[0:32, 0:128], comb[:], ident, [comb, cst], [pt])
        P.copy("act", combT[0:32, t * 128:(t + 1) * 128], pt[0:32, 0:128], [pt], [combT])

    for blk in range(nblk):
        tok0 = blk * TB
        for g in range(16):
            st = wst[g % NWS]
            sv = st[:, 0:2 * TB].rearrange("p (a n) -> p a n", a=2)
            P.dma("sp" if g % 2 == 0 else "pool", sv, oT_v[:, g * 2:(g + 1) * 2, tok0:tok0 + TB], writes=[st])
            P.copy("pool" if g % 2 == 0 else "act", actT[:, g * 2:(g + 1) * 2, :], sv, [st], [actT])
        P.dma("sp", pst[:], pT_v[:, :, tok0:tok0 + TB], writes=[pst])
        P.copy("dve", pTb[:], pst[:], [pst], [pTb])
        for ng in range(8):
            gs = slice(ng * 512, (ng + 1) * 512)
            for k2 in range(16):
                bf = wload(w_out_v[:, k2 * 2:(k2 + 1) * 2, gs], 2)
                for j in range(2):
                    kc = k2 * 2 + j
                    for t in range(NT):
                        P.mm(accA[t][:], actT[:, kc, t * 128:(t + 1) * 128], bf[:, j * 512:(j + 1) * 512], [actT, bf], [accA[t]],
                             start=(kc == 0), stop=(kc == 31))
            for t in range(NT):
                x_ = xs[t % 2]; e_ = ev[t % 2]; y = yv[t % 2]
                P.dma("pool", x_[:], xres[tok0 + t * 128:tok0 + (t + 1) * 128, gs], writes=[x_])
                P.copy("act", e_[:], accA[t][:], [accA[t]], [e_])
                P.stt("dve", y[:], x_[:], ALPHA, e_[:], ALU.mult, ALU.add, [x_, e_], [y])
                P.dma("sp", Y1[t][:, gs], y[:], reads=[y], writes=[Y1[t]])
        if stage == 1:
            for t in range(NT):
                P.dma("sp", wa[:], Y1[t][:], reads=[Y1[t]], writes=[wa])
                fin.append(P.dma("sp", xo[tok0 + t * 128:tok0 + (t + 1) * 128, :], wa[:], reads=[wa]).idx)
            continue
        for t in range(NT):
            layer_norm_tile(Y1[t], 0, t, tok0)
        if stage == 2:
            for t in range(NT):
                P.dma("sp", wa[:], X1[t][:], reads=[X1[t]], writes=[wa])
                fin.append(P.dma("sp", xo[tok0 + t * 128:tok0 + (t + 1) * 128, :], wa[:], reads=[wa]).idx)
            continue
        for e in range(n_exp + 1):
            mats = [(wgate[e], 0), (wup[e], 2)] if e < n_exp else [(pw_gd, 0)]
            for (wm, b0) in mats:
                wm_v = wm.rearrange("(c p) n -> p c n", p=128)
                for k4 in range(8):
                    bf = wload(wm_v[:, k4 * 4:(k4 + 1) * 4, :], 4)
                    for j in range(4):
                        kc = k4 * 4 + j
                        for fc in range(2):
                            P.mm(accA[b0 + fc][:, 0:TB], bf[:, j * 256 + fc * 128:j * 256 + (fc + 1) * 128], actT[:, kc, :], [bf, actT], [accA[b0 + fc]],
                                 start=(kc == 0), stop=(kc == 31))
            if e == n_exp:
                for fc in range(2):
                    P.copy("act", gdT[:, fc, :], accA[fc][:, 0:TB], [accA[fc]], [gdT])
                continue
            sl = sel[e % 2]
            P.ts("pool", sl[:], ones, ident[:, e:e + 1], None, ALU.mult, reads=[cst], writes=[sl])
            pc = banks[6]
            P.mm(pc[:, 0:TB], sl[:], combT[:], [sl, combT], [pc])
            P.copy("act", cb[:], pc[:, 0:TB], [pc], [cb])
            for fc in range(2):
                s_ = sil[fc]; t_ = tmu[fc]
                P.act(s_[:], accA[fc][:, 0:TB], AF.Silu, [accA[fc]], [s_])
                P.tt("dve", t_[:], accA[2 + fc][:, 0:TB], s_[:], ALU.mult, [accA[2 + fc], s_], [t_])
                P.tt("pool", hact[:, e * 2 + fc, :], t_[:], cb[:], ALU.mult, [t_, cb], [hact])
        for dg in range(8):
            gs = slice(dg * 512, (dg + 1) * 512)
            for e in range(n_exp):
                bf = wload(wdown[e].rearrange("(c p) n -> p c n", p=128)[:, :, gs], 2)
                for fc in range(2):
                    for t in range(NT):
                        P.mm(accA[t][:], hact[:, e * 2 + fc, t * 128:(t + 1) * 128], bf[:, fc * 512:(fc + 1) * 512], [hact, bf], [accA[t]],
                             start=(e == 0 and fc == 0), stop=(e == n_exp - 1 and fc == 1))
            bfp = wload(pw_proj.rearrange("(c p) n -> p c n", p=128)[:, :, gs], 2)
            for t in range(NT):
                for fc in range(2):
                    P.mm(accB[t][:], pTb[:, fc, t * 128:(t + 1) * 128], bfp[:, fc * 512:(fc + 1) * 512], [pTb, bfp], [accB[t]], start=(fc == 0), stop=(fc == 1))
            bfg = wload(pw_gu.rearrange("(c p) n -> p c n", p=128)[:, :, gs], 2)
            for t in range(NT):
                p_ = pr[t % 2]; s_ = sg[t % 2]; x_ = xs[t % 2]; e_ = ev[t % 2]; y = yv[t % 2]
                P.copy("act", p_[:], accB[t][:], [accB[t]], [p_])
                for fc in range(2):
                    P.mm(accB[t][:], gdT[:, fc, t * 128:(t + 1) * 128], bfg[:, fc * 512:(fc + 1) * 512], [gdT, bfg], [accB[t]], start=(fc == 0), stop=(fc == 1))
                P.act(s_[:], accB[t][:], AF.Sigmoid, [accB[t]], [s_])
                P.tt("pool", p_[:], p_[:], s_[:], ALU.mult, [p_, s_], [p_])
                P.dma("pool", x_[:], X1[t][:, gs], reads=[X1[t]], writes=[x_])
                P.copy("act", e_[:], accA[t][:], [accA[t]], [e_])
                P.stt("dve", y[:], x_[:], ALPHA, e_[:], ALU.mult, ALU.add, [x_, e_], [y])
                P.tt("pool", y[:], y[:], p_[:], ALU.add, [y, p_], [y])
                P.dma("sp", Y2[t][:, gs], y[:], reads=[y], writes=[Y2[t]])
        for t in range(NT):
            layer_norm_tile(Y2[t], 1, t, tok0)
    P.emit(final_wait_ops=fin)
    return nc, P


def f_inputs(inp, oT_c, xres_c, pT_c):
    lnp = np.stack([np.broadcast_to(inp[k][None, :], (128, DM)) for k in ("ln1_g", "ln1_b", "ln2_g", "ln2_b")]).astype(np.float32)
    wrt = np.ascontiguousarray(np.concatenate([inp["moe_w_grp"], inp["moe_w_rt"]], axis=1))
    brt = np.ascontiguousarray(np.broadcast_to(np.concatenate([inp["moe_b_grp"], inp["moe_b_rt"]])[None, :], (128, 36))).astype(np.float32)
    return {"oT": oT_c, "xres": xres_c, "pT": pT_c, "w_out": inp["w_out"], "lnp": np.ascontiguousarray(lnp), "wrt": wrt, "brt": brt,
            "wgate": inp["moe_w_gate"], "wup": inp["moe_w_up"], "wdown": inp["moe_w_down"],
            "pw_proj": inp["ple_w_proj"], "pw_gd": inp["ple_w_gate_down"], "pw_gu": inp["ple_w_gate_up"], "cst": CF_ARR}


NCORES = 8
_PROGS = {}


def _prog(kind):
    if kind not in _PROGS:
        nc = bass.Bass("TRN2", target_bir_lowering=False)
        if kind == "g":
            nc, _ = build_g(nc, nblk=8192 // NB, NTOK=8192)
        elif kind == "r":
            nc, _ = build_r(nc, nblk=8192 // NB, NTOK=8192)
        else:
            nc, _ = build_f(nc, NTOKC=1024, NT=4)
        _PROGS[kind] = nc
    return _PROGS[kind]


def kernel(**inputs):
    x = np.asarray(inputs["x"], dtype=np.float32)
    p = np.asarray(inputs["p"], dtype=np.float32)
    B, S, Dm = x.shape
    X = np.ascontiguousarray(x.reshape(B * S, Dm))
    cores = list(range(NCORES))
    for layer in range(4):
        inpL = {k: np.asarray(v[layer], dtype=np.float32) for k, v in inputs.items() if k not in ("x", "p")}
        xT = np.ascontiguousarray(X.T)
        res = run_bass_kernel_spmd(_prog("g"), [g_inputs(inpL, c, xT) for c in cores], core_ids=cores)
        og = [np.asarray(r["og"]) for r in res.results]
        res = run_bass_kernel_spmd(_prog("r"), [r_inputs(inpL, c, xT) for c in cores], core_ids=cores)
        orw = [np.asarray(r["orw"]) for r in res.results]
        oT = np.concatenate(og + orw, axis=0)
        del og, orw, xT
        pTl = p[layer].reshape(B * S, -1).T
        maps = []
        for c in cores:
            ts_ = slice(c * 1024, (c + 1) * 1024)
            maps.append(f_inputs(inpL, np.ascontiguousarray(oT[:, ts_]), np.ascontiguousarray(X[ts_]), np.ascontiguousarray(pTl[:, ts_])))
        res = run_bass_kernel_spmd(_prog("f"), maps, core_ids=cores)
        X = np.concatenate([np.asarray(r["xo"]) for r in res.results], axis=0)
        del maps, oT
    return np.ascontiguousarray(X.reshape(B, S, Dm)).astype(np.float32)
```

```python
import os

import contextlib
import numpy as np
import concourse.bass as bass
import concourse.mybir as mybir
from concourse.bass_utils import run_bass_kernel_spmd

F32 = mybir.dt.float32
BF16 = mybir.dt.bfloat16
AF = mybir.ActivationFunctionType
ALU = mybir.AluOpType
AX = mybir.AxisListType


class Buf:
    __slots__ = ("t", "last_w", "readers", "name")

    def __init__(self, t, name):
        self.t = t
        self.name = name
        self.last_w = None
        self.readers = []

    def __getitem__(self, idx):
        return self.t[idx]


class Op:
    __slots__ = ("eng", "fn", "deps", "sig", "semkey", "semval", "is_dma", "idx")


class Prog:
    NDMASEM = 6

    def __init__(self, nc):
        self.nc = nc
        self.ops = []
        self.stack = contextlib.ExitStack()
        self.dma_count = {}
        self.dma_ops = {}
        self.nbuf = 0

    def sb(self, shape, dtype=F32, name=None):
        self.nbuf += 1
        name = "s_" + (name or f"sb{self.nbuf}")
        t = self.stack.enter_context(self.nc.sbuf_tensor(name, list(shape), dtype))
        return Buf(t, name)

    def ps(self, shape, dtype=F32, name=None):
        self.nbuf += 1
        name = "p_" + (name or f"ps{self.nbuf}")
        t = self.stack.enter_context(self.nc.psum_tensor(name, list(shape), dtype))
        return Buf(t, name)

    def dram(self, name, shape, dtype=F32, kind="Internal"):
        t = self.nc.dram_tensor(name, list(shape), dtype, kind=kind)
        return Buf(t, name)

    def op(self, eng, fn, reads=(), writes=(), dma=False, ser=False):
        o = Op()
        o.eng = eng
        o.fn = fn
        o.is_dma = dma
        o.sig = False
        o.idx = len(self.ops)
        deps = set()
        for b in reads:
            if b is None:
                continue
            if b.last_w is not None:
                deps.add(b.last_w)
        for b in writes:
            if b.last_w is not None:
                deps.add(b.last_w)
            for r in b.readers:
                deps.add(r)
        if dma:
            n = self.dma_count.get(eng, 0)
            self.dma_count[eng] = n + 1
            lst = self.dma_ops.setdefault(eng, [])
            if n >= self.NDMASEM:
                deps.add(lst[n - self.NDMASEM])
            lst.append(o.idx)
            o.semkey = ("dma", eng, n % self.NDMASEM)
            o.semval = 16 * (n // self.NDMASEM + 1)
        else:
            o.semkey = ("eng", eng)
            o.semval = None
        pruned = set()
        for d in deps:
            od = self.ops[d]
            if (not od.is_dma) and od.eng == eng and eng == "pe" and not dma:
                continue
            pruned.add(d)
        if ser and getattr(self, "last_pe", None) is not None:
            pruned.add(self.last_pe)
        if eng == "pe" and not dma:
            self.last_pe = o.idx
        o.deps = pruned
        for b in reads:
            if b is not None:
                b.readers.append(o.idx)
        for b in writes:
            b.last_w = o.idx
            b.readers = []
        self.ops.append(o)
        return o

    def dma(self, q, out, in_, reads=(), writes=(), **kw):
        return self.op(q, lambda e: e.dma_start(out=out, in_=in_, **kw), reads, writes, dma=True)

    def mm(self, out_ap, lhsT, rhs, reads, writes, start=True, stop=True, ser=False, **kw):
        return self.op("pe", lambda e: e.matmul(out_ap, lhsT, rhs, start=start, stop=stop, **kw), reads, writes, ser=ser)

    def tr(self, out_ap, in_ap, ident_ap, reads, writes):
        return self.op("pe", lambda e: e.transpose(out_ap, in_ap, ident_ap), reads, writes)

    def act(self, out, in_, func, reads, writes, eng="act", **kw):
        return self.op(eng, lambda e: e.activation(out=out, in_=in_, func=func, **kw), reads, writes)

    def tt(self, eng, out, in0, in1, op, reads, writes):
        return self.op(eng, lambda e: e.tensor_tensor(out=out, in0=in0, in1=in1, op=op), reads, writes)

    def ts(self, eng, out, in0, s1, s2, op0, op1=None, reads=(), writes=(), **kw):
        if op1 is None:
            return self.op(eng, lambda e: e.tensor_scalar(out=out, in0=in0, scalar1=s1, scalar2=None, op0=op0, **kw), reads, writes)
        return self.op(eng, lambda e: e.tensor_scalar(out=out, in0=in0, scalar1=s1, scalar2=s2, op0=op0, op1=op1, **kw), reads, writes)

    def stt(self, eng, out, in0, scalar, in1, op0, op1, reads, writes):
        return self.op(eng, lambda e: e.scalar_tensor_tensor(out=out, in0=in0, scalar=scalar, in1=in1, op0=op0, op1=op1), reads, writes)

    def copy(self, eng, out, in_, reads, writes):
        if eng == "act":
            return self.op(eng, lambda e: e.activation(out=out, in_=in_, func=AF.Copy), reads, writes)
        return self.op(eng, lambda e: e.tensor_copy(out=out, in_=in_), reads, writes)

    def memset(self, eng, ap, val, writes):
        return self.op(eng, lambda e: e.memset(ap, val), (), writes)

    def emit(self, final_wait_ops=()):
        nc = self.nc
        ops = self.ops
        for o in ops:
            for d in o.deps:
                ops[d].sig = True
        for i in final_wait_ops:
            ops[i].sig = True
        cnt = {}
        for o in ops:
            if not o.is_dma and o.sig:
                cnt[o.eng] = cnt.get(o.eng, 0) + 1
                o.semval = cnt[o.eng]
        semkeys = sorted({o.semkey for o in ops if o.sig or o.is_dma}, key=str)
        sems = {}
        for k in semkeys:
            sems[k] = self.stack.enter_context(nc.semaphore("s_" + "_".join(str(x) for x in k)))
        waited = {}
        per_eng = {}
        for o in ops:
            need = {}
            for d in o.deps:
                od = ops[d]
                k = od.semkey
                if od.semval > need.get(k, 0):
                    need[k] = od.semval
            wl = []
            for k, v in need.items():
                if waited.get((o.eng, k), 0) >= v:
                    continue
                waited[(o.eng, k)] = v
                wl.append((k, v))
            per_eng.setdefault(o.eng, []).append((o, wl))
        fin = {}
        for i in final_wait_ops:
            od = ops[i]
            fin[od.semkey] = max(fin.get(od.semkey, 0), od.semval)
        self.stats = {e: len(l) for e, l in per_eng.items()}

        engmap = {"pe": "tensor", "act": "scalar", "dve": "vector", "pool": "gpsimd", "sp": "sync"}
        with nc.Block() as block:
            for ename, lst in per_eng.items():
                def body(eng, lst=lst, ename=ename):
                    for o, wl in lst:
                        for k, v in wl:
                            eng.wait_ge(sems[k], v)
                        ins = o.fn(eng)
                        if o.is_dma:
                            ins.then_inc(sems[o.semkey], 16)
                        elif o.sig:
                            ins.then_inc(sems[o.semkey], 1)
                    if ename == "sp":
                        for k, v in fin.items():
                            eng.wait_ge(sems[k], v)
                getattr(block, engmap[ename])(body)
            if "sp" not in per_eng and fin:
                def body2(eng):
                    for k, v in fin.items():
                        eng.wait_ge(sems[k], v)
                block.sync(body2)
        self.stack.close()


def view(ap, name="v"):
    return Buf(ap, name)


class SubBuf:
    __slots__ = ("p", "t", "name")

    def __init__(self, parent, ap, name="sub"):
        self.p = parent
        self.t = ap
        self.name = name

    @property
    def last_w(self):
        return self.p.last_w

    @last_w.setter
    def last_w(self, v):
        self.p.last_w = v

    @property
    def readers(self):
        return self.p.readers

    @readers.setter
    def readers(self, v):
        self.p.readers = v

    def __getitem__(self, idx):
        return self.t[idx]


D = 4096
T = 4096
NB = 256
NCH = NB // 64


def consts():
    k = np.arange(128)[:, None]
    i = np.arange(128)[None, :]
    c = {}
    c["ident"] = (k == i)
    c["Eblk"] = (k // 64) == (i // 64)
    s = k % 64
    t = i % 64
    c["MaskZ"] = np.where(i < 64, s < t, s <= t)
    c["ones"] = np.ones((128, 128))
    c["UU"] = np.where(i < 64, k < t, k <= t) & (k < 64)
    c["negUU"] = -1.0 * ((k <= t) & (k < 64))
    c["NegM"] = np.where(c["MaskZ"], 0.0, -30000.0)
    c["Dup"] = (k == t) & (k < 64)
    c["Urev"] = (k > i) & (k < 64) & (i < 64)
    c["Uinc"] = (k <= i) & (k < 64) & (i < 64)
    c["Dup1"] = (k == 64 + t)
    c["UblkI"] = ((k // 64) == (i // 64)) & (k <= i)
    c["SelL0"] = (k == 63) & (i >= 0)
    c["SelL1"] = (k == 127) & (i >= 0)
    c["sgn"] = np.where(i == 0, np.where(k < 64, -1.0, 0.0), np.where(k < 64, 0.0, 1.0))
    names = list(c.keys())
    arr = np.concatenate([np.asarray(c[n], dtype=np.float32) for n in names], axis=1)
    rmask = np.ones((128, NB), np.float32)
    rmask[:, ::64] = 0.0
    arr = np.concatenate([arr, rmask], axis=1)
    return names, np.ascontiguousarray(arr)


C_NAMES, C_ARR = consts()
NCST = C_ARR.shape[1]


def load_consts(P, cst_d):
    cst = P.sb([128, NCST], name="cst")
    P.dma("sp", cst[:], cst_d, writes=[cst])
    C = {n: cst[:, i * 128:(i + 1) * 128] for i, n in enumerate(C_NAMES)}
    C["rmask"] = cst[:, len(C_NAMES) * 128: len(C_NAMES) * 128 + NB]
    return cst, C


class CH:
    pass


class Engine:
    def __init__(self, P, n, cst, C, banks):
        self.P = P
        self.n = n
        self.cst = cst
        self.C = C
        bZ, bP, bQ, bY, bS = banks
        self.psZ = [SubBuf(bZ, bZ[:, j * 128:(j + 1) * 128], f"psZ{j}") for j in range(4)]
        self.psP = [SubBuf(bP, bP[0:64, j * 64:(j + 1) * 64], f"psP{j}") for j in range(8)]
        self.psQ = [SubBuf(bQ, bQ[0:64, j * 64:(j + 1) * 64], f"psQ{j}") for j in range(8)]
        self.psY = [SubBuf(bY, bY[0:64, j * 64:(j + 1) * 64], f"psY{j}") for j in range(8)]
        self.bS = bS
        self.Zm = [P.sb([128, 128], name=f"eZm{i}") for i in range(n)]
        self.Pm = [[P.sb([64, 64], name=f"eP{i}_{j}") for j in range(2)] for i in range(n)]
        self.Qm = [[P.sb([64, 64], name=f"eQ{i}_{j}") for j in range(2)] for i in range(n)]
        self.Ym = [[P.sb([64, 64], name=f"eY{i}_{j}") for j in range(2)] for i in range(n)]
        self.ZK = [P.sb([128, 64], name=f"eZK{i}") for i in range(n)]
        for i in range(n):
            P.memset("pool", self.ZK[i][:], 0.0, [self.ZK[i]])

    def phaseA(self, chs):
        P, C, cst = self.P, self.C, self.cst
        assert len(chs) <= self.n
        for i, ch in enumerate(chs):
            pz = self.psZ[i % 4]
            ch.Zm = self.Zm[i]
            P.mm(pz[:], ch.LT, ch.RT, ch.rd, [pz])
            P.tt("dve", ch.Zm[:], pz[:], ch.Dm, ALU.mult, [pz] + ch.Dm_rd, [ch.Zm])
            ch.ZK = self.ZK[i]
            P.copy("pool", ch.ZK[64:128, :], ch.Zm[64:128, 0:64], [ch.Zm], [ch.ZK])
        for i, ch in enumerate(chs):
            P.tr(self.psQ[i][:], ch.Zm[0:64, 0:64], C["ident"][0:64, 0:64], [ch.Zm, cst], [self.psQ[i]])
        for i, ch in enumerate(chs):
            P.copy("act", self.Qm[i][0][:], self.psQ[i][:], [self.psQ[i]], [self.Qm[i][0]])
            P.tt("pool", self.Ym[i][0][:], ch.Zm[0:64, 0:64], C["ident"][0:64, 0:64], ALU.add, [ch.Zm, cst], [self.Ym[i][0]])
        for k in range(1, 6):
            a, b = (k - 1) % 2, k % 2
            for i, ch in enumerate(chs):
                Pp = ch.Zm[0:64, 0:64] if k == 1 else self.Pm[i][a][:]
                Pb = ch.Zm if k == 1 else self.Pm[i][a]
                Qp = self.Qm[i][a]
                if k < 5:
                    P.mm(self.psP[i][:], Qp[:], Pp, [Qp, Pb], [self.psP[i]])
                P.mm(self.psQ[i][:], Pp, Qp[:], [Qp, Pb], [self.psQ[i]])
            for i, ch in enumerate(chs):
                P.copy("act", self.Qm[i][b][:], self.psQ[i][:], [self.psQ[i]], [self.Qm[i][b]])
                if k < 5:
                    P.copy("dve", self.Pm[i][b][:], self.psP[i][:], [self.psP[i]], [self.Pm[i][b]])
            for i, ch in enumerate(chs):
                P.mm(self.psY[i][:], self.Qm[i][b][:], self.Ym[i][a][:], [self.Qm[i][b], self.Ym[i][a]], [self.psY[i]])
            for i, ch in enumerate(chs):
                P.tt("dve", self.Ym[i][b][:], self.psY[i][:], self.Ym[i][a][:], ALU.add, [self.psY[i], self.Ym[i][a]], [self.Ym[i][b]])
        for i, ch in enumerate(chs):
            ch.Tt = self.Ym[i][1]


def scan_bufs(P, bS, nh, Nv):
    sb = []
    for h in range(nh):
        o = CH()
        o.Xs = P.sb([64, Nv], name=f"sXs{h}")
        o.Mt = P.sb([128, 128], name=f"sMt{h}")
        sb.append(o)
    return sb


PB = 99


def phaseB(P, chs, scb, bS_slots, groups):
    if PB < -1:
        return
    for i, ch in enumerate(chs):
        psX = bS_slots[i][0]
        Mr = ch.M[ch.Mrows, :]
        P.mm(psX[:], ch.AT, ch.M[:, :], ch.rd + [ch.M], [psX], start=True, stop=False)
        P.mm(psX[:], ch.ZK[:, :], ch.UV[:, ch.UVc], [ch.ZK, ch.UV], [psX], start=False, stop=True)
    if PB < 0:
        return
    for i, ch in enumerate(chs):
        psX = bS_slots[i][0]
        P.copy("act", scb[i].Xs[:], psX[:], [psX], [scb[i].Xs])
    if PB < 3:
        return
    for i, ch in enumerate(chs):
        psU = bS_slots[i][1]
        P.mm(psU[:], ch.Tt[:], scb[i].Xs[:], [ch.Tt, scb[i].Xs], [psU])
    for i, ch in enumerate(chs):
        psU = bS_slots[i][1]
        P.copy("dve", ch.UV[0:64, ch.UVc], psU[:], [psU], [ch.UV])
    if PB < 5:
        return
    for i, ch in enumerate(chs):
        Mr = ch.M[ch.Mrows, :]
        P.mm(ch.yout, ch.RrT, ch.M[:, :], ch.rd + [ch.M], [ch.yout_b], start=True, stop=False)
        P.mm(ch.yout, ch.Zm[:, 64:128], ch.UV[:, ch.UVc], [ch.Zm, ch.UV], [ch.yout_b], start=False, stop=True)
    if PB < 6:
        return
    for (BK, BK_rd, UVb, psM) in groups:
        P.mm(psM[:], BK, UVb[:], BK_rd + [UVb], [psM])
    if PB < 7:
        return
    for i, ch in enumerate(chs):
        psM = groups[ch.gid][3]
        Mr = ch.M[ch.Mrows, :]
        if ch.Mrows == slice(0, 128):
            P.copy("act", scb[i].Mt[:], psM[:], [psM], [scb[i].Mt])
            P.stt("dve", Mr, Mr, ch.gC, scb[i].Mt[:], ALU.mult, ALU.add, [ch.M, scb[i].Mt] + ch.gC_rd, [ch.M])
            continue
        else:
            P.act(Mr, Mr, AF.Copy, [ch.M] + ch.gC_rd, [ch.M], scale=ch.gC)
        P.tt("dve", Mr, Mr, psM[ch.Mblk[0], ch.Mblk[1]], ALU.add, [ch.M, psM], [ch.M])


def build_r(nc, nblk, NTOK, stage=99):
    xT = nc.dram_tensor("xT", [D, NTOK], F32, kind="ExternalInput").ap()
    wr = nc.dram_tensor("wr", [D, 1024], F32, kind="ExternalInput").ap()
    lora_d = nc.dram_tensor("lora", [128, 1024], F32, kind="ExternalInput").ap()
    prm_d = nc.dram_tensor("prm", [128, 24], F32, kind="ExternalInput").ap()
    cst_d = nc.dram_tensor("cst", [128, NCST], F32, kind="ExternalInput").ap()
    orw = nc.dram_tensor("orw", [256, NTOK], F32, kind="ExternalOutput").ap()
    P = Prog(nc)
    fin = []
    cst, C = load_consts(P, cst_d)
    prm = P.sb([128, 24], name="prm")
    P.dma("sp", prm[:], prm_d, writes=[prm])
    lora = P.sb([128, 1024], name="lora")
    P.dma("sp", lora[:], lora_d, writes=[lora])
    om = P.sb([128, 2], name="om")
    for fg in range(2):
        P.ts("dve", om[:, fg:fg + 1], prm[:, fg * 10 + 6:fg * 10 + 7], -1.0, 1.0, ALU.mult, ALU.add, reads=[prm], writes=[om])

    stg = [P.sb([128, 4, 256], F32, name=f"stg{i}") for i in range(2)]
    wrb = P.sb([128, 32, 4, 256], BF16, name="wrb")
    wr_v = wr.rearrange("(c p) (a n) -> p c a n", p=128, a=4)
    for g in range(32):
        st = stg[g % 2]
        P.dma("sp" if g % 2 == 0 else "act", st[:], wr_v[:, g], writes=[st])
        P.copy("act" if g % 2 == 0 else "dve", wrb[:, g], st[:], [st], [wrb])

    if stage == 0:
        tmp0 = P.sb([128, 256], F32, name="tmp0")
        P.copy("act", tmp0[:], wrb[:, 31, 3, :], [wrb], [tmp0])
        fin.append(P.dma("sp", orw[0:128, 0:256], tmp0[:], reads=[tmp0]).idx)
        P.emit(final_wait_ops=fin)
        return nc, P
    xb_t = [P.sb([128, 32, NB], BF16, name=f"xb{i}") for i in range(2)]
    xb_v = [[view(xb_t[p][:, g * 4:(g + 1) * 4, :], f"xb{p}_{g}") for g in range(8)] for p in range(2)]
    xT_v = xT.rearrange("(c p) t -> p c t", p=128)

    CG = [(0, 0, 128), (0, 128, 128), (1, 0, 128), (1, 128, 128), (2, 0, 128), (2, 128, 128), (3, 0, 128), (3, 128, 128)]
    MUCOL = [0, 10, 1, 11, 2, 12, 20, 21]
    hbuf = [P.sb([128, NB + 1], name=f"hbuf{c}") for c in range(8)]
    hs = [P.sb([128, NB], name=f"hs{c}") for c in range(8)]
    dtmp = [P.sb([128, NB], name=f"dtmp{c}") for c in range(2)]

    banks = [P.ps([128, 512], name=f"bank{i}") for i in range(8)]
    psA = [SubBuf(banks[0], banks[0][:, 0:256], "psA0"), SubBuf(banks[1], banks[1][:, 0:256], "psA1")]
    psL = [SubBuf(banks[7], banks[7][:, 0:256], "psL0"), SubBuf(banks[7], banks[7][:, 256:512], "psL1")]
    eng = Engine(P, 8, cst, C, banks[2:7])
    bS = banks[6]
    bT_ = banks[2]
    psT = [SubBuf(bT_, bT_[:, j * 128:(j + 1) * 128], f"psT{j}") for j in range(4)]
    bS_slots = []
    for h in range(2):
        bS_slots.append([SubBuf(bS, bS[0:64, (h * 2 + 0) * 64:(h * 2 + 1) * 64], f"psX{h}"),
                         SubBuf(bS, bS[0:64, (h * 2 + 1) * 64:(h * 2 + 2) * 64], f"psU{h}")])
    psMg = SubBuf(bS, bS[:, 256:384], "psMg")
    psYc = [SubBuf(bS, bS[0:64, 384 + 0:384 + 128], "psYc")]
    scb = scan_bufs(P, bS, 2, 64)

    def mk(shape, name, dt=F32):
        return P.sb(shape, dt, name=name)
    txw = mk([128, NB], "txw"); sxg = mk([128, NB], "sxg")
    lw = mk([128, NB], "lw"); aS = mk([128, NB], "aS"); gT = mk([128, NB], "gT")
    kkr = mk([128, NB], "kkr"); sq = mk([128, NB], "sq"); rinv = mk([128, NB], "rinv"); kk = mk([128, NB], "kk")
    t1 = mk([128, NB], "t1"); kp = mk([128, NB], "kp"); bT = mk([128, NB], "bT"); rk = mk([128, NB], "rk")
    bonus = mk([128, NB], "bonus"); G = mk([128, NB], "G"); Gm = mk([128, NB], "Gm")
    eG = mk([128, NB], "eG"); eGm = mk([128, NB], "eGm"); eNG = mk([128, NB], "eNG"); eRev = mk([128, NB], "eRev")
    LT2 = mk([128, NCH, 128], "LT2"); RT2 = mk([128, NCH, 128], "RT2"); BKT2 = mk([128, NCH, 128], "BKT2"); VT2 = mk([128, NCH, 128], "VT2")
    P.memset("pool", VT2[:], 0.0, [VT2])
    LTz = [mk([128, NCH, 128], f"LTz{h}") for h in range(2)]
    RTz = [mk([128, NCH, 128], f"RTz{h}") for h in range(2)]
    BKh = [mk([128, 128], f"BKh{c}") for c in range(NCH)]
    UV = [mk([128, 128], f"UV{c}") for c in range(NCH)]
    for c in range(NCH):
        P.memset("pool", UV[c][:], 0.0, [UV[c]])
    Mst = [mk([128, 64], f"Mst{fg}") for fg in range(2)]
    Ytm = [mk([64, 128], f"Ytm{c}") for c in range(NCH)]; yc = [mk([64, 128], f"yc{c}") for c in range(NCH)]; yn = [mk([64, 128], f"yn{c}") for c in range(NCH)]
    msum = [mk([64, 2], f"msum{c}") for c in range(NCH)]; nmean = [mk([64, 2], f"nmean{c}") for c in range(NCH)]
    vs = [mk([64, 2], f"vs{c}") for c in range(NCH)]; rstd = [mk([64, 2], f"rstd{c}") for c in range(NCH)]
    fin1 = [mk([128, 64], f"fin1{c}") for c in range(NCH)]
    oblk = [mk([128, NB], f"oblk{fg}") for fg in range(2)]

    def c3(ap):
        return ap.rearrange("p (c t) -> p c t", c=NCH)

    for blk in range(nblk):
        par = blk % 2
        tok0 = blk * NB
        seq_start = (tok0 % T) == 0
        for g in range(8):
            st = stg[g % 2]
            P.dma("sp" if g % 2 == 0 else "act", st[:], xT_v[:, g * 4:(g + 1) * 4, tok0:tok0 + NB], writes=[st])
            P.copy("pool", xb_v[par][g][:], st[:], [st], [xb_v[par][g]])
        xb = xb_t[par]
        xbr = xb_v[par]
        if seq_start:
            for fg in range(2):
                P.memset("pool", Mst[fg][:], 0.0, [Mst[fg]])
        for cg in range(8):
            a_, off, M = CG[cg]
            ps = psA[cg % 2]
            for dc in range(32):
                P.mm(ps[0:M, :], wrb[:, dc, a_, off:off + M], xb[:, dc, :], [wrb, xbr[dc // 4]], [ps], start=(dc == 0), stop=(dc == 31))
            hb = hbuf[cg]
            if seq_start:
                P.memset("pool", hb[:, 0:1], 0.0, [hb])
            P.copy("act", hb[0:M, 1:NB + 1], ps[0:M, :], [ps], [hb])
            dt_ = dtmp[cg % 2]
            P.tt("dve", dt_[0:M, :], hb[0:M, 0:NB], hb[0:M, 1:NB + 1], ALU.subtract, [hb], [dt_])
            mc = MUCOL[cg]
            P.stt("dve", hs[cg][0:M, :], dt_[0:M, :], prm[0:M, mc:mc + 1], hb[0:M, 1:NB + 1], ALU.mult, ALU.add, [dt_, prm, hb], [hs[cg]])
            P.copy("pool", hb[0:M, 0:1], hb[0:M, NB:NB + 1], [hb], [hb])
        if stage == 1:
            fin.append(P.dma("sp", orw[0:128, tok0:tok0 + NB], hs[0][:], reads=[hs[0]]).idx)
            fin.append(P.dma("sp", orw[128:256, tok0:tok0 + NB], hs[3][:], reads=[hs[3]]).idx)
            continue
        P.act(txw[:], hs[6][:], AF.Tanh, [hs[6]], [txw])
        P.act(sxg[:], hs[7][:], AF.Sigmoid, [hs[7]], [sxg])
        for fg in range(2):
            pb = fg * 10
            rT = hs[fg]; kT = hs[2 + fg]; vT = hs[4 + fg]
            fc = slice(fg * 128, (fg + 1) * 128)
            pl = psL[0]
            P.mm(pl[:], lora[:, fc], txw[:], [lora, txw], [pl])
            P.act(lw[:], pl[:], AF.Sigmoid, [pl, prm], [lw], bias=prm[:, pb + 3:pb + 4])
            P.ts("pool", lw[:], lw[:], -0.6065306597126334, None, ALU.mult, reads=[lw], writes=[lw])
            pl = psL[1]
            P.mm(pl[:], lora[:, 256 + fg * 128:256 + (fg + 1) * 128], hs[6][:], [lora, hs[6]], [pl], start=True, stop=False)
            P.mm(pl[:], lora[:, 512 + fg * 128:512 + (fg + 1) * 128], hs[7][:], [lora, hs[7]], [pl], start=False, stop=True)
            P.act(aS[:], pl[:], AF.Sigmoid, [pl, prm], [aS], bias=prm[:, pb + 4:pb + 5])
            pl = psL[0]
            P.mm(pl[:], lora[:, 768 + fg * 128:768 + (fg + 1) * 128], sxg[:], [lora, sxg], [pl])
            P.copy("act", gT[:], pl[:], [pl], [gT])
            P.ts("pool", kkr[:], kT[:], prm[:, pb + 5:pb + 6], None, ALU.mult, reads=[kT, prm], writes=[kkr])
            P.tt("pool", sq[:], kkr[:], kkr[:], ALU.mult, [kkr], [sq])
            pl = psL[1]
            P.mm(pl[:], C["Eblk"], sq[:], [cst, sq], [pl])
            P.ts("dve", rinv[:], pl[:], 1e-6, None, ALU.add, reads=[pl], writes=[rinv])
            P.act(rinv[:], rinv[:], AF.Ln, [rinv], [rinv])
            P.act(rinv[:], rinv[:], AF.Exp, [rinv], [rinv], scale=-0.5)
            P.tt("pool", kk[:], kkr[:], rinv[:], ALU.mult, [kkr, rinv], [kk])
            P.ts("dve", t1[:], aS[:], prm[:, pb + 6:pb + 7], om[:, fg:fg + 1], ALU.mult, ALU.add, reads=[aS, prm, om], writes=[t1])
            P.tt("pool", kp[:], t1[:], kT[:], ALU.mult, [t1, kT], [kp])
            P.tt("pool", bT[:], kk[:], aS[:], ALU.mult, [kk, aS], [bT])
            P.stt("dve", rk[:], rT[:], prm[:, pb + 7:pb + 8], kp[:], ALU.mult, ALU.mult, [rT, prm, kp], [rk])
            pl = psL[0]
            P.mm(pl[:], C["Eblk"], rk[:], [cst, rk], [pl])
            P.tt("dve", bonus[:], pl[:], vT[:], ALU.mult, [pl, vT], [bonus])
            P.op("dve", lambda e: e.tensor_tensor_scan(out=G[:], data0=C["rmask"], data1=lw[:], initial=0.0, op0=ALU.mult, op1=ALU.add),
                 [cst, lw], [G])
            P.tt("pool", Gm[:], G[:], lw[:], ALU.subtract, [G, lw], [Gm])
            P.act(eG[:], G[:], AF.Exp, [G], [eG])
            P.act(eGm[:], Gm[:], AF.Exp, [Gm], [eGm])
            P.act(eNG[:], G[:], AF.Exp, [G], [eNG], scale=-1.0)
            for c in range(NCH):
                cs_ = slice(c * 64, (c + 1) * 64)
                P.act(eRev[:, cs_], G[:, cs_], AF.Exp, [G], [eRev], scale=-1.0, bias=G[:, c * 64 + 63:c * 64 + 64])
            if stage == 2:
                dbg = {0: lw, 1: aS}[fg]
                fin.append(P.dma("sp", orw[fc, tok0:tok0 + NB], dbg[:], reads=[dbg]).idx)
                continue
            P.tt("dve", LT2[:, :, 0:64], c3(bT[:]), c3(eNG[:]), ALU.mult, [bT, eNG], [LT2])
            P.tt("pool", LT2[:, :, 64:128], c3(kp[:]), c3(eNG[:]), ALU.mult, [kp, eNG], [LT2])
            P.stt("dve", RT2[:, :, 0:64], c3(kk[:]), -1.0, c3(eGm[:]), ALU.mult, ALU.mult, [kk, eGm], [RT2])
            P.tt("pool", RT2[:, :, 64:128], c3(rT[:]), c3(eG[:]), ALU.mult, [rT, eG], [RT2])
            P.tt("dve", BKT2[:, :, 0:64], c3(bT[:]), c3(eRev[:]), ALU.mult, [bT, eRev], [BKT2])
            P.tt("pool", BKT2[:, :, 64:128], c3(kp[:]), c3(eRev[:]), ALU.mult, [kp, eRev], [BKT2])
            P.copy("pool", VT2[:, :, 64:128], c3(vT[:]), [vT], [VT2])
            for h in range(2):
                hm = C["Eblk"][:, h * 64:h * 64 + 1]
                P.ts("pool", LTz[h][:], LT2[:], hm, None, ALU.mult, reads=[LT2, cst], writes=[LTz[h]])
                P.ts("pool", RTz[h][:], RT2[:], hm, None, ALU.mult, reads=[RT2, cst], writes=[RTz[h]])
            for c in range(NCH):
                pt = psT[c % 4]
                P.tr(pt[:], BKT2[:, c, :], C["ident"], [BKT2, cst], [pt])
                P.copy("act", BKh[c][:], pt[:], [pt], [BKh[c]])
            for c in range(NCH):
                pt = psT[c % 4]
                P.tr(pt[:], VT2[:, c, :], C["ident"], [VT2, cst], [pt])
                P.copy("act", UV[c][64:128, :], pt[64:128, :], [pt], [UV[c]])
            chs = []
            for c in range(NCH):
                for h in range(2):
                    ch = CH()
                    hp = slice(h * 64, (h + 1) * 64)
                    ch.LT = LTz[h][:, c, :]; ch.RT = RT2[:, c, :]; ch.rd = [LTz[h], RTz[h], RT2]
                    ch.Dm = C["MaskZ"]; ch.Dm_rd = [cst]
                    ch.AT = RTz[h][:, c, 0:64]; ch.RrT = RTz[h][:, c, 64:128]
                    ch.M = Mst[fg]; ch.Mrows = hp
                    ch.UV = UV[c]; ch.UVc = slice(h * 64, (h + 1) * 64)
                    ch.gid = 0; ch.Mblk = (hp, hp)
                    ch.gC = eG[hp, c * 64 + 63:c * 64 + 64]; ch.gC_rd = [eG]
                    ch.yout = psYc[0][:, h * 64:(h + 1) * 64]; ch.yout_b = psYc[0]
                    chs.append(ch)
            eng.phaseA(chs)
            if stage == 3:
                for c in range(NCH):
                    fin.append(P.dma("sp", orw[fg * 128:fg * 128 + 64, tok0 + c * 64:tok0 + (c + 1) * 64], chs[2 * c].Tt[:], reads=[chs[2 * c].Tt]).idx)
                    fin.append(P.dma("sp", orw[fg * 128 + 64:fg * 128 + 128, tok0 + c * 64:tok0 + (c + 1) * 64], chs[2 * c].Zm[0:64, 0:64], reads=[chs[2 * c].Zm]).idx)
                continue
            for c in range(NCH):
                phaseB(P, chs[2 * c:2 * c + 2], scb, bS_slots, [(BKh[c][:], [BKh[c]], UV[c], psMg)])
                P.copy("act", Ytm[c][:], psYc[0][:], [psYc[0]], [Ytm[c]])
            R4 = range(NCH)
            for c in R4:
                Y3 = Ytm[c][:].rearrange("p (h v) -> p h v", h=2)
                P.op("dve", lambda e, Y3=Y3, c=c: e.tensor_reduce(out=msum[c][:], in_=Y3, axis=AX.X, op=ALU.add), [Ytm[c]], [msum[c]])
            for c in R4:
                P.ts("dve", nmean[c][:], msum[c][:], -1.0 / 64, None, ALU.mult, reads=[msum[c]], writes=[nmean[c]])
            for c in R4:
                for h in range(2):
                    hc = slice(h * 64, (h + 1) * 64)
                    P.ts("dve", yc[c][:, hc], Ytm[c][:, hc], nmean[c][:, h:h + 1], None, ALU.add, reads=[Ytm[c], nmean[c]], writes=[yc[c]])
            for c in R4:
                for h in range(2):
                    hc = slice(h * 64, (h + 1) * 64)
                    P.op("act", lambda e, hc=hc, h=h, c=c: e.activation(out=yn[c][:, hc], in_=yc[c][:, hc], func=AF.Square, accum_out=vs[c][:, h:h + 1]),
                         [yc[c]], [yn[c], vs[c]])
            for c in R4:
                P.ts("dve", rstd[c][:], vs[c][:], 1.0 / 64, 64e-5, ALU.mult, ALU.add, reads=[vs[c]], writes=[rstd[c]])
            for c in R4:
                P.act(rstd[c][:], rstd[c][:], AF.Ln, [rstd[c]], [rstd[c]])
            for c in R4:
                P.act(rstd[c][:], rstd[c][:], AF.Exp, [rstd[c]], [rstd[c]], scale=-0.5)
            for c in R4:
                for h in range(2):
                    hc = slice(h * 64, (h + 1) * 64)
                    P.ts("dve", yn[c][:, hc], yc[c][:, hc], rstd[c][:, h:h + 1], None, ALU.mult, reads=[yc[c], rstd[c]], writes=[yn[c]])
            for c in R4:
                P.tr(psT[c % 4][:, 0:64], yn[c][:], C["ident"][0:64, 0:64], [yn[c], cst], [psT[c % 4]])
            for c in R4:
                P.ts("dve", fin1[c][:], psT[c % 4][:, 0:64], prm[:, pb + 8:pb + 9], prm[:, pb + 9:pb + 10], ALU.mult, ALU.add, reads=[psT[c % 4], prm], writes=[fin1[c]])
            for c in R4:
                cs_ = slice(c * 64, (c + 1) * 64)
                P.tt("pool", fin1[c][:], fin1[c][:], bonus[:, cs_], ALU.add, [fin1[c], bonus], [fin1[c]])
            for c in R4:
                cs_ = slice(c * 64, (c + 1) * 64)
                P.tt("pool", oblk[fg][:, cs_], fin1[c][:], gT[:, cs_], ALU.mult, [fin1[c], gT], [oblk[fg]])
            fin.append(P.dma("sp", orw[fc, tok0:tok0 + NB], oblk[fg][:], reads=[oblk[fg]]).idx)
    P.emit(final_wait_ops=fin)
    return nc, P


def r_inputs(inp, core, xT):
    GC = 8224
    w_in = inp["w_in"]
    f0 = core * 256
    cols = []
    for base in (0, 2048, 4096):
        cols.append(np.arange(GC + base + f0, GC + base + f0 + 256))
    cols.append(np.arange(GC + 6144, GC + 6400))
    cols = np.concatenate(cols)
    wr = np.ascontiguousarray(w_in[:, cols])
    lora = np.zeros((128, 1024), np.float32)
    lora[0:96, 0:256] = inp["rwkv_w2"][:, f0:f0 + 256]
    lora[96:128, 256:512] = inp["rwkv_a2"][0:32, f0:f0 + 256]
    lora[0:64, 512:768] = inp["rwkv_a2"][32:96, f0:f0 + 256]
    lora[64:128, 768:1024] = inp["rwkv_g2"][:, f0:f0 + 256]
    mu = inp["rwkv_mu"]
    prm = np.zeros((128, 24), np.float32)
    for fg in range(2):
        fs = slice(f0 + fg * 128, f0 + (fg + 1) * 128)
        pb = fg * 10
        prm[:, pb + 0] = mu[0:2048][fs]
        prm[:, pb + 1] = mu[2048:4096][fs]
        prm[:, pb + 2] = mu[4096:6144][fs]
        prm[:, pb + 3] = inp["rwkv_w0"][fs]
        prm[:, pb + 4] = inp["rwkv_a0"][fs]
        prm[:, pb + 5] = inp["rwkv_k_k"][fs]
        prm[:, pb + 6] = inp["rwkv_k_a"][fs]
        prm[:, pb + 7] = inp["rwkv_r_k"].reshape(-1)[fs]
        prm[:, pb + 8] = inp["rwkv_lnx_g"][fs]
        prm[:, pb + 9] = inp["rwkv_lnx_b"][fs]
    prm[:, 20] = mu[6144:6272]
    prm[:, 21] = mu[6272:6400]
    return {"xT": xT, "wr": wr, "lora": lora, "prm": prm, "cst": C_ARR}


def build_g(nc, nblk, NTOK, stage=99):
    xT = nc.dram_tensor("xT", [D, NTOK], F32, kind="ExternalInput").ap()
    wg = nc.dram_tensor("wg", [D, 1024], F32, kind="ExternalInput").ap()
    wab_d = nc.dram_tensor("wab", [128, 32, 4], F32, kind="ExternalInput").ap()
    cw_d = nc.dram_tensor("cw", [128, 24], F32, kind="ExternalInput").ap()
    prm_d = nc.dram_tensor("prm", [128, 8], F32, kind="ExternalInput").ap()
    cst_d = nc.dram_tensor("cst", [128, NCST], F32, kind="ExternalInput").ap()
    og = nc.dram_tensor("og", [256, NTOK], F32, kind="ExternalOutput").ap()
    P = Prog(nc)
    fin = []
    cst, C = load_consts(P, cst_d)
    cw = P.sb([128, 24], name="cw"); P.dma("sp", cw[:], cw_d, writes=[cw])
    prm = P.sb([128, 8], name="prm"); P.dma("sp", prm[:], prm_d, writes=[prm])
    wabf = P.sb([128, 32, 4], name="wabf"); P.dma("sp", wabf[:], wab_d, writes=[wabf])
    wab = P.sb([128, 32, 4], BF16, name="wab"); P.copy("dve", wab[:], wabf[:], [wabf], [wab])
    negA = P.sb([128, 2], name="negA")
    P.act(negA[:], prm[:, 0:2], AF.Exp, [prm], [negA])
    P.ts("dve", negA[:], negA[:], -1.0, None, ALU.mult, reads=[negA], writes=[negA])
    stg = [P.sb([128, 4, 256], F32, name=f"stg{i}") for i in range(2)]
    wgb = P.sb([128, 32, 4, 256], BF16, name="wgb")
    wg_v = wg.rearrange("(c p) (a n) -> p c a n", p=128, a=4)
    for g in range(32):
        st = stg[g % 2]
        P.dma("sp" if g % 2 == 0 else "act", st[:], wg_v[:, g], writes=[st])
        P.copy("act" if g % 2 == 0 else "dve", wgb[:, g], st[:], [st], [wgb])
    xb_t = [P.sb([128, 32, NB], BF16, name=f"xb{i}") for i in range(2)]
    xb_v = [[view(xb_t[p][:, g * 4:(g + 1) * 4, :], f"xb{p}_{g}") for g in range(8)] for p in range(2)]
    xT_v = xT.rearrange("(c p) t -> p c t", p=128)

    banks = [P.ps([128, 512], name=f"bank{i}") for i in range(8)]
    psA = [SubBuf(banks[0], banks[0][:, 0:256], "psA0"), SubBuf(banks[1], banks[1][:, 0:256], "psA1")]
    psS = SubBuf(banks[1], banks[1][:, 256:512], "psS")
    eng = Engine(P, 8, cst, C, banks[2:7])
    bSA, bSB = banks[6], banks[7]
    psT = [SubBuf(banks[2], banks[2][:, j * 128:(j + 1) * 128], f"psT{j}") for j in range(4)]
    bS_slots = [[SubBuf(bSA, bSA[0:64, h * 128:(h + 1) * 128], f"psX{h}"), SubBuf(bSA, bSA[0:64, 256 + h * 128:256 + (h + 1) * 128], f"psU{h}")] for h in range(2)]
    psYo = [SubBuf(bSB, bSB[0:64, h * 128:(h + 1) * 128], f"psYo{h}") for h in range(2)]
    psMg = [SubBuf(bSB, bSB[:, 256 + h * 128:256 + (h + 1) * 128], f"psMg{h}") for h in range(2)]
    scb = scan_bufs(P, None, 2, 128)

    def mk(shape, name, dt=F32):
        return P.sb(shape, dt, name=name)
    hbuf = [mk([128, NB + 3], f"hbuf{c}") for c in range(6)]
    acc = [mk([128, NB], f"acc{c}") for c in range(2)]
    cs = [mk([128, NB], f"cs{c}") for c in range(6)]
    qk = [mk([128, NB], f"qk{c}") for c in range(4)]
    szT = [mk([128, NB], f"szT{c}") for c in range(2)]
    sq = mk([128, NB], "sq"); rn = mk([128, NB], "rn")
    ab = mk([128, 4], "ab"); vals = mk([128, 6], "vals"); g1 = mk([128, 2], "g1")
    st = [mk([128, 6], f"st{c}") for c in range(NCH)]
    gcb = [mk([128, 2], f"gcb{c}") for c in range(NCH)]
    eg = mk([128, 2], "eg"); tsc = mk([128, 2], "tsc")
    rs = [mk([128, 2], f"rs{c}") for c in range(NCH)]
    eGc = [mk([128, 2], f"eGc{c}") for c in range(NCH)]; eGm = mk([128, 2], "eGmc"); dG = mk([128, 2], "dG")
    eRv = mk([128, 2], "eRv"); bks = [mk([128, 2], f"bks{c}") for c in range(NCH)]; gCe = [mk([128, 2], f"gCe{c}") for c in range(NCH)]
    gB = mk([128, 128], "gB"); Dl = mk([128, 128], "Dl"); diagM = mk([128, 128], "diagM")
    Dm = [mk([128, 128], f"Dm{i}") for i in range(8)]
    LTg = [mk([128, 128], f"LTg{i}") for i in range(8)]
    RTg = [mk([128, 128], f"RTg{i}") for i in range(8)]
    RTu = [mk([128, 128], f"RTu{i}") for i in range(8)]
    VTg = mk([128, 128], "VTg"); P.memset("pool", VTg[:], 0.0, [VTg])
    BKh = [mk([128, 128], f"BKh{i}") for i in range(8)]
    UV = [mk([128, 128], f"UV{i}") for i in range(8)]
    for i in range(8):
        P.memset("pool", UV[i][:], 0.0, [UV[i]])
    Mst = [mk([128, 128], f"Mst{h}") for h in range(2)]
    o2 = [mk([64, 128], f"o2_{i}") for i in range(8)]; on = [mk([64, 128], f"on_{i}") for i in range(8)]
    ssq = [mk([64, 1], f"ssq{i}") for i in range(8)]; rstd = [mk([64, 1], f"rstd{i}") for i in range(8)]; f1 = [mk([128, 64], f"f1_{i}") for i in range(8)]
    oblk = [mk([128, NB], f"oblk{h}") for h in range(2)]

    for blk in range(nblk):
        par = blk % 2
        tok0 = blk * NB
        seq_start = (tok0 % T) == 0
        for g in range(8):
            s_ = stg[g % 2]
            P.dma("sp" if g % 2 == 0 else "act", s_[:], xT_v[:, g * 4:(g + 1) * 4, tok0:tok0 + NB], writes=[s_])
            P.copy("pool", xb_v[par][g][:], s_[:], [s_], [xb_v[par][g]])
        xb = xb_t[par]; xbr = xb_v[par]
        if seq_start:
            for h in range(2):
                P.memset("pool", Mst[h][:], 0.0, [Mst[h]])
        for cg in range(8):
            ps = psA[cg % 2]
            for dc in range(32):
                P.mm(ps[:], wgb[:, dc, cg // 2, (cg % 2) * 128:(cg % 2 + 1) * 128], xb[:, dc, :], [wgb, xbr[dc // 4]], [ps], start=(dc == 0), stop=(dc == 31))
            if cg >= 6:
                P.act(szT[cg - 6][:], ps[:], AF.Silu, [ps], [szT[cg - 6]])
                continue
            hb = hbuf[cg]
            if seq_start:
                P.memset("pool", hb[:, 0:3], 0.0, [hb])
            P.copy("act", hb[:, 3:NB + 3], ps[:], [ps], [hb])
            a = acc[cg % 2]
            P.ts("dve", a[:], hb[:, 0:NB], cw[:, cg * 4:cg * 4 + 1], None, ALU.mult, reads=[hb, cw], writes=[a])
            for j in range(1, 4):
                P.stt("dve", a[:], hb[:, j:j + NB], cw[:, cg * 4 + j:cg * 4 + j + 1], a[:], ALU.mult, ALU.add, [hb, cw, a], [a])
            P.copy("pool", hb[:, 0:3], hb[:, NB:NB + 3], [hb], [hb])
            P.act(cs[cg][:], a[:], AF.Silu, [a], [cs[cg]])
        for cc in range(4):
            P.tt("pool", sq[:], cs[cc][:], cs[cc][:], ALU.mult, [cs[cc]], [sq])
            P.mm(psS[:], C["ones"], sq[:], [cst, sq], [psS])
            P.ts("dve", rn[:], psS[:], 1e-6, None, ALU.add, reads=[psS], writes=[rn])
            P.act(rn[:], rn[:], AF.Ln, [rn], [rn])
            P.act(rn[:], rn[:], AF.Exp, [rn], [rn], scale=-0.5)
            if cc < 2:
                P.stt("dve", qk[cc][:], cs[cc][:], float(128 ** -0.5), rn[:], ALU.mult, ALU.mult, [cs[cc], rn], [qk[cc]])
            else:
                P.tt("dve", qk[cc][:], cs[cc][:], rn[:], ALU.mult, [cs[cc], rn], [qk[cc]])
        if stage == 1:
            fin.append(P.dma("sp", og[0:128, tok0:tok0 + NB], qk[0][:], reads=[qk[0]]).idx)
            fin.append(P.dma("sp", og[128:256, tok0:tok0 + NB], qk[2][:], reads=[qk[2]]).idx)
            continue
        for s2 in range(NB // 128):
            tsl = slice(s2 * 128, (s2 + 1) * 128)
            pab = SubBuf(banks[1], banks[1][:, 256:260], "pab")
            for dc in range(32):
                P.mm(pab[:], xb[:, dc, tsl], wab[:, dc, :], [xbr[dc // 4], wab], [pab], start=(dc == 0), stop=(dc == 31))
            P.copy("dve", ab[:], pab[:], [pab], [ab])
            P.tt("dve", g1[:], ab[:, 0:2], prm[:, 2:4], ALU.add, [ab, prm], [g1])
            P.act(g1[:], g1[:], AF.Exp, [g1], [g1])
            P.ts("dve", g1[:], g1[:], 1.0, None, ALU.add, reads=[g1], writes=[g1])
            P.act(g1[:], g1[:], AF.Ln, [g1], [g1])
            P.tt("dve", vals[:, 0:2], g1[:], negA[:], ALU.mult, [g1, negA], [vals])
            P.act(vals[:, 4:6], ab[:, 2:4], AF.Sigmoid, [ab], [vals])
            pG = SubBuf(banks[1], banks[1][:, 264:266], "pG")
            P.mm(pG[:], C["UblkI"], vals[:, 0:2], [cst, vals], [pG])
            P.copy("dve", vals[:, 2:4], pG[:], [pG], [vals])
            for c2 in range(2):
                c = s2 * 2 + c2
                pst = SubBuf(banks[1], banks[1][:, 272:278], "pst")
                P.mm(pst[:], C["Dup" if c2 == 0 else "Dup1"], vals[:], [cst, vals], [pst])
                P.copy("dve", st[c][:], pst[:], [pst], [st[c]])
                pgc = SubBuf(banks[1], banks[1][:, 280:282], "pgc")
                P.mm(pgc[:], C["SelL0"], st[c][:, 2:4], [cst, st[c]], [pgc])
                P.copy("dve", gcb[c][:], pgc[:], [pgc], [gcb[c]])
                P.act(eg[:], st[c][:, 0:2], AF.Exp, [st[c]], [eg])
                P.ts("dve", tsc[:], eg[:], C["sgn"][:, 0:1], C["sgn"][:, 1:2], ALU.mult, ALU.add, reads=[eg, cst], writes=[tsc])
                P.tt("dve", rs[c][:], tsc[:], st[c][:, 4:6], ALU.mult, [tsc, st[c]], [rs[c]])
                P.act(eGc[c][:], st[c][:, 2:4], AF.Exp, [st[c]], [eGc[c]])
                P.tt("dve", dG[:], st[c][:, 2:4], st[c][:, 0:2], ALU.subtract, [st[c]], [dG])
                P.act(eGm[:], dG[:], AF.Exp, [dG], [eGm])
                P.tt("dve", dG[:], gcb[c][:], st[c][:, 2:4], ALU.subtract, [gcb[c], st[c]], [dG])
                P.act(eRv[:], dG[:], AF.Exp, [dG], [eRv])
                P.tt("dve", bks[c][:], rs[c][:], eRv[:], ALU.mult, [rs[c], eRv], [bks[c]])
                P.act(gCe[c][:], gcb[c][:], AF.Exp, [gcb[c]], [gCe[c]])
                for h in range(2):
                    i = c * 2 + h
                    csl = slice(c * 64, (c + 1) * 64)
                    qT = qk[h]; kT = qk[2 + h]; vT = cs[4 + h]
                    P.ts("pool", gB[:], C["ones"], st[c][:, h:h + 1], None, ALU.mult, reads=[cst, st[c]], writes=[gB])
                    pd = psT[i % 4]
                    P.mm(pd[:], gB[:], C["UU"], [gB, cst], [pd], start=True, stop=False)
                    P.mm(pd[:], C["negUU"], gB[:], [gB, cst], [pd], start=False, stop=True)
                    P.tt("dve", Dl[:], pd[:], C["NegM"], ALU.add, [pd, cst], [Dl])
                    P.act(Dl[:], Dl[:], AF.Exp, [Dl], [Dl])
                    P.ts("dve", Dm[i][:], Dl[:], rs[c][:, h:h + 1], None, ALU.mult, reads=[Dl, rs[c]], writes=[Dm[i]])
                    P.ts("dve", diagM[:, 0:64], C["Dup"][:, 0:64], eGm[:, h:h + 1], None, ALU.mult, reads=[cst, eGm], writes=[diagM])
                    P.ts("dve", diagM[:, 64:128], C["Dup"][:, 64:128], eGc[c][:, h:h + 1], None, ALU.mult, reads=[cst, eGc[c]], writes=[diagM])
                    pb_ = psT[(i + 1) % 4]
                    P.mm(pb_[:], C["ones"], diagM[:], [cst, diagM], [pb_])
                    P.tt("dve", RTg[i][:, 0:64], pb_[:, 0:64], kT[:, csl], ALU.mult, [pb_, kT], [RTg[i]])
                    P.tt("dve", RTg[i][:, 64:128], pb_[:, 64:128], qT[:, csl], ALU.mult, [pb_, qT], [RTg[i]])
                    P.copy("pool", LTg[i][:, 0:64], kT[:, csl], [kT], [LTg[i]])
                    P.copy("pool", LTg[i][:, 64:128], kT[:, csl], [kT], [LTg[i]])
                    P.copy("pool", RTu[i][:, 0:64], kT[:, csl], [kT], [RTu[i]])
                    P.copy("pool", RTu[i][:, 64:128], qT[:, csl], [qT], [RTu[i]])
                    pt = psT[(i + 2) % 4]
                    P.tr(pt[:], LTg[i][:], C["ident"], [LTg[i], cst], [pt])
                    P.ts("dve", BKh[i][:], pt[:], bks[c][:, h:h + 1], None, ALU.mult, reads=[pt, bks[c]], writes=[BKh[i]])
                    P.copy("pool", VTg[:, 64:128], vT[:, csl], [vT], [VTg])
                    pt = psT[(i + 3) % 4]
                    P.tr(pt[:], VTg[:], C["ident"], [VTg, cst], [pt])
                    P.copy("act", UV[i][64:128, :], pt[64:128, :], [pt], [UV[i]])
        if stage == 2:
            fin.append(P.dma("sp", og[0:128, tok0:tok0 + 128], BKh[7][:], reads=[BKh[7]]).idx)
            fin.append(P.dma("sp", og[128:256, tok0:tok0 + 128], Dm[7][:], reads=[Dm[7]]).idx)
            fin.append(P.dma("sp", og[0:128, tok0 + 128:tok0 + 256], UV[7][:], reads=[UV[7]]).idx)
            fin.append(P.dma("sp", og[128:256, tok0 + 128:tok0 + 256], RTg[7][:], reads=[RTg[7]]).idx)
            continue
        chs = []
        for c in range(NCH):
            for h in range(2):
                i = c * 2 + h
                ch = CH()
                ch.LT = LTg[i][:]; ch.RT = RTu[i][:]; ch.rd = [LTg[i], RTg[i], RTu[i]]
                ch.Dm = Dm[i][:]; ch.Dm_rd = [Dm[i]]
                ch.AT = RTg[i][:, 0:64]; ch.RrT = RTg[i][:, 64:128]
                ch.M = Mst[h]; ch.Mrows = slice(0, 128)
                ch.UV = UV[i]; ch.UVc = slice(0, 128)
                ch.gid = h; ch.Mblk = (slice(0, 128), slice(0, 128))
                ch.gC = gCe[c][:, h:h + 1]; ch.gC_rd = [gCe[c]]
                ch.yout = psYo[h][:]; ch.yout_b = psYo[h]
                chs.append(ch)
        eng.phaseA(chs)
        for c in range(NCH):
            cc_ = chs[2 * c:2 * c + 2]
            phaseB(P, cc_, scb, bS_slots, [(BKh[2 * c + h][:], [BKh[2 * c + h]], UV[2 * c + h], psMg[h]) for h in range(2)])
            for h in range(2):
                P.copy("act", o2[2 * c + h][:], psYo[h][:], [psYo[h]], [o2[2 * c + h]])
        R8 = range(2 * NCH)
        for i in R8:
            P.op("act", lambda e, i=i: e.activation(out=on[i][:], in_=o2[i][:], func=AF.Square, accum_out=ssq[i][:]), [o2[i]], [on[i], ssq[i]])
        for i in R8:
            P.ts("dve", rstd[i][:], ssq[i][:], 1.0 / 128, 1e-6, ALU.mult, ALU.add, reads=[ssq[i]], writes=[rstd[i]])
        for i in R8:
            P.act(rstd[i][:], rstd[i][:], AF.Ln, [rstd[i]], [rstd[i]])
        for i in R8:
            P.act(rstd[i][:], rstd[i][:], AF.Exp, [rstd[i]], [rstd[i]], scale=-0.5)
        for i in R8:
            P.ts("dve", on[i][:], o2[i][:], rstd[i][:, 0:1], None, ALU.mult, reads=[o2[i], rstd[i]], writes=[on[i]])
        for i in R8:
            P.tr(psT[i % 4][:, 0:64], on[i][:], C["ident"][0:64, 0:64], [on[i], cst], [psT[i % 4]])
            P.ts("dve", f1[i][:], psT[i % 4][:, 0:64], prm[:, 4:5], None, ALU.mult, reads=[psT[i % 4], prm], writes=[f1[i]])
        for i in R8:
            c, h = i // 2, i % 2
            csl = slice(c * 64, (c + 1) * 64)
            P.tt("pool", oblk[h][:, csl], f1[i][:], szT[h][:, csl], ALU.mult, [f1[i], szT[h]], [oblk[h]])
        for h in range(2):
            fin.append(P.dma("sp", og[h * 128:(h + 1) * 128, tok0:tok0 + NB], oblk[h][:], reads=[oblk[h]]).idx)
    P.emit(final_wait_ops=fin)
    return nc, P


def g_inputs(inp, core, xT):
    w_in = inp["w_in"]
    h0, h1 = 2 * core, 2 * core + 1
    cols = []
    for base in (0, 2048, 4096, 6144):
        for h in (h0, h1):
            cols.append(np.arange(base + h * 128, base + (h + 1) * 128))
    wg = np.ascontiguousarray(w_in[:, np.concatenate(cols)])
    abc = np.array([8192 + h0, 8192 + h1, 8192 + 16 + h0, 8192 + 16 + h1])
    wab = np.ascontiguousarray(w_in[:, abc].reshape(32, 128, 4).transpose(1, 0, 2))
    cwl = inp["gdn_conv_w"]
    cw = np.zeros((128, 24), np.float32)
    cc = 0
    for base in (0, 2048, 4096):
        for h in (h0, h1):
            cw[:, cc * 4:(cc + 1) * 4] = cwl[:, base + h * 128: base + (h + 1) * 128].T
            cc += 1
    prm = np.zeros((128, 8), np.float32)
    prm[:, 0] = inp["gdn_a_log"][h0]; prm[:, 1] = inp["gdn_a_log"][h1]
    prm[:, 2] = inp["gdn_dt_bias"][h0]; prm[:, 3] = inp["gdn_dt_bias"][h1]
    prm[:, 4] = inp["gdn_norm_w"]
    return {"xT": xT, "wg": wg, "wab": wab, "cw": cw, "prm": prm, "cst": C_ARR}


DM = 4096
NE = 32
ALPHA = float((2 * 4) ** 0.25)
BIG = 10000.0


def f_consts():
    k = np.arange(128)[:, None]
    i = np.arange(128)[None, :]
    return np.ascontiguousarray(np.concatenate([(k == i), np.ones((128, 128))], axis=1).astype(np.float32))


CF_ARR = f_consts()


def build_f(nc, NTOKC=1024, NT=4, n_exp=NE, stage=99):
    TB = NT * 128
    nblk = NTOKC // TB
    oT = nc.dram_tensor("oT", [DM, NTOKC], F32, kind="ExternalInput").ap()
    xres = nc.dram_tensor("xres", [NTOKC, DM], F32, kind="ExternalInput").ap()
    pT = nc.dram_tensor("pT", [256, NTOKC], F32, kind="ExternalInput").ap()
    w_out = nc.dram_tensor("w_out", [DM, DM], F32, kind="ExternalInput").ap()
    lnp = nc.dram_tensor("lnp", [4, 128, DM], F32, kind="ExternalInput").ap()
    wrt = nc.dram_tensor("wrt", [DM, 36], F32, kind="ExternalInput").ap()
    brt = nc.dram_tensor("brt", [128, 36], F32, kind="ExternalInput").ap()
    wgate = nc.dram_tensor("wgate", [NE, DM, 256], F32, kind="ExternalInput").ap()
    wup = nc.dram_tensor("wup", [NE, DM, 256], F32, kind="ExternalInput").ap()
    wdown = nc.dram_tensor("wdown", [NE, 256, DM], F32, kind="ExternalInput").ap()
    pw_proj = nc.dram_tensor("pw_proj", [256, DM], F32, kind="ExternalInput").ap()
    pw_gd = nc.dram_tensor("pw_gd", [DM, 256], F32, kind="ExternalInput").ap()
    pw_gu = nc.dram_tensor("pw_gu", [256, DM], F32, kind="ExternalInput").ap()
    cst_d = nc.dram_tensor("cst", [128, 256], F32, kind="ExternalInput").ap()
    xo = nc.dram_tensor("xo", [NTOKC, DM], F32, kind="ExternalOutput").ap()
    P = Prog(nc)
    fin = []
    Y1 = [P.dram(f"Y1_{t}", [128, DM]) for t in range(NT)]
    X1 = [P.dram(f"X1_{t}", [128, DM]) for t in range(NT)]
    Y2 = [P.dram(f"Y2_{t}", [128, DM]) for t in range(NT)]

    cst = P.sb([128, 256], name="cst"); P.dma("sp", cst[:], cst_d, writes=[cst])
    ident = cst[:, 0:128]; ones = cst[:, 128:256]
    wr32 = P.sb([128, 32, 36], name="wr32")
    P.dma("sp", wr32[:], wrt.rearrange("(c p) n -> p c n", p=128), writes=[wr32])
    bias = P.sb([128, 36], name="bias"); P.dma("sp", bias[:], brt, writes=[bias])

    def mk(shape, name, dt=F32):
        return P.sb(shape, dt, name=name)
    actT = mk([128, 32, TB], "actT", BF16)
    hact = mk([128, 2 * NE, TB], "hact", BF16)
    wa = mk([128, DM], "wa")
    NWS = 3
    wst = [mk([128, 1024], f"wst{i}") for i in range(NWS)]
    wbf = [mk([128, 1024], f"wbf{i}", BF16) for i in range(NWS)]
    wcnt = [0]

    def wload(src, a):
        j = wcnt[0] % NWS
        wcnt[0] += 1
        st, bf = wst[j], wbf[j]
        q = "sp" if wcnt[0] % 2 else "act"
        dst = st[:] if a == 1 else st[:].rearrange("p (a n) -> p a n", a=a)
        P.dma(q, dst, src, writes=[st])
        eng = ("act", "dve", "pool")[wcnt[0] % 3]
        P.copy(eng, bf[:], st[:], [st], [bf])
        return bf

    banks = [P.ps([128, 512], name=f"bank{i}") for i in range(8)]
    accA = banks[0:4]
    accB = banks[4:8]
    xs = [mk([128, 512], f"xs{i}") for i in range(2)]
    ev = [mk([128, 512], f"ev{i}") for i in range(2)]
    yv = [mk([128, 512], f"yv{i}") for i in range(2)]
    gpc = [mk([128, 512], f"gpc{i}") for i in range(2)]
    bpc = [mk([128, 512], f"bpc{i}") for i in range(2)]
    xf = [mk([128, 128], f"xf{i}") for i in range(4)]
    junk = mk([128, 512], "junk")
    s1 = mk([128, 1], "s1"); nmean = mk([128, 1], "nmean"); ssq8 = mk([128, 8], "ssq8"); ssq = mk([128, 1], "ssq"); rstd = mk([128, 1], "rstd")
    combT = mk([128, TB], "combT"); P.memset("pool", combT[:], 0.0, [combT])
    sel = [mk([128, 128], f"sel{i}") for i in range(2)]
    cb = mk([128, TB], "cb")
    sil = [mk([128, TB], f"sil{i}") for i in range(2)]
    tmu = [mk([128, TB], f"tmu{i}") for i in range(2)]
    pst = mk([128, 2, TB], "pst"); pTb = mk([128, 2, TB], "pTb", BF16); gdT = mk([128, 2, TB], "gdT", BF16)
    pr = [mk([128, 512], f"pr{i}") for i in range(2)]; sg = [mk([128, 512], f"sg{i}") for i in range(2)]
    lg = mk([128, 36], "lg"); gmax = mk([128, 1], "gmax"); ngmax = mk([128, 1], "ngmax"); gmask = mk([128, 4], "gmask")
    eg4 = mk([128, 4], "eg4"); gsum = mk([128, 1], "gsum"); pg = mk([128, 1], "pg"); pen = mk([128, 4], "pen")
    ml = mk([128, 32], "ml"); v1 = mk([128, 1], "v1"); m1 = mk([128, 32], "m1"); ml2 = mk([128, 32], "ml2")
    v2 = mk([128, 1], "v2"); m2 = mk([128, 32], "m2"); dd = mk([128, 1], "dd"); w1 = mk([128, 1], "w1"); w2 = mk([128, 1], "w2")
    g1 = mk([128, 1], "g1"); g2 = mk([128, 1], "g2"); comb = mk([128, 32], "comb"); comb2 = mk([128, 32], "comb2")

    oT_v = oT.rearrange("(c p) t -> p c t", p=128)
    w_out_v = w_out.rearrange("(c p) n -> p c n", p=128)
    pT_v = pT.rearrange("(c p) t -> p c t", p=128)

    def layer_norm_tile(src, which, t, tok0):
        P.dma("sp", wa[:], src[:], reads=[src], writes=[wa])
        P.op("dve", lambda e: e.tensor_reduce(out=s1[:], in_=wa[:], axis=AX.X, op=ALU.add), [wa], [s1])
        P.ts("dve", nmean[:], s1[:], -1.0 / DM, None, ALU.mult, reads=[s1], writes=[nmean])
        for ng in range(8):
            P.op("act", lambda e, ng=ng: e.activation(out=junk[:], in_=wa[:, ng * 512:(ng + 1) * 512], func=AF.Square,
                                                      bias=nmean[:, 0:1], accum_out=ssq8[:, ng:ng + 1]), [wa, nmean], [junk, ssq8])
        P.op("dve", lambda e: e.tensor_reduce(out=ssq[:], in_=ssq8[:], axis=AX.X, op=ALU.add), [ssq8], [ssq])
        P.ts("dve", rstd[:], ssq[:], 1.0 / DM, 1e-5, ALU.mult, ALU.add, reads=[ssq], writes=[rstd])
        P.act(rstd[:], rstd[:], AF.Ln, [rstd], [rstd])
        P.act(rstd[:], rstd[:], AF.Exp, [rstd], [rstd], scale=-0.5)
        psR = banks[5]
        for ng in range(8):
            gs = slice(ng * 512, (ng + 1) * 512)
            gp = gpc[ng % 2]; bp = bpc[ng % 2]; y = yv[ng % 2]; e_ = ev[ng % 2]
            P.dma("act", gp[:], lnp[2 * which, :, gs], writes=[gp])
            P.dma("act", bp[:], lnp[2 * which + 1, :, gs], writes=[bp])
            P.ts("dve", e_[:], wa[:, gs], nmean[:, 0:1], rstd[:, 0:1], ALU.add, ALU.mult, reads=[wa, nmean, rstd], writes=[e_])
            P.tt("pool", y[:], e_[:], gp[:], ALU.mult, [e_, gp], [y])
            P.tt("pool", y[:], y[:], bp[:], ALU.add, [y, bp], [y])
            if which == 1:
                fin.append(P.dma("sp", xo[tok0 + t * 128:tok0 + (t + 1) * 128, gs], y[:], reads=[y]).idx)
                continue
            P.dma("sp", X1[t][:, gs], y[:], reads=[y], writes=[X1[t]])
            for j in range(4):
                kc = ng * 4 + j
                pt = banks[4] if kc % 2 == 0 else banks[7]
                P.tr(pt[:, 0:128], y[:, j * 128:(j + 1) * 128], ident, [y, cst], [pt])
                f = xf[kc % 4]
                P.copy("act", f[:], pt[:, 0:128], [pt], [f])
                P.copy("pool", actT[:, kc, t * 128:(t + 1) * 128], f[:], [f], [actT])
                P.mm(psR[:, 0:36], f[:], wr32[:, kc, :], [f, wr32], [psR], start=(kc == 0), stop=(kc == 31))
        if which == 1:
            return
        P.tt("dve", lg[:], psR[:, 0:36], bias[:], ALU.add, [psR, bias], [lg])
        P.op("dve", lambda e: e.tensor_reduce(out=gmax[:], in_=lg[:, 0:4], axis=AX.X, op=ALU.max), [lg], [gmax])
        P.ts("dve", gmask[:], lg[:, 0:4], gmax[:, 0:1], None, ALU.is_ge, reads=[lg, gmax], writes=[gmask])
        P.ts("dve", ngmax[:], gmax[:], -1.0, None, ALU.mult, reads=[gmax], writes=[ngmax])
        P.op("act", lambda e: e.activation(out=eg4[:], in_=lg[:, 0:4], func=AF.Exp, bias=ngmax[:, 0:1], accum_out=gsum[:]), [lg, ngmax], [eg4, gsum])
        P.op("dve", lambda e: e.reciprocal(out=pg[:], in_=gsum[:]), [gsum], [pg])
        P.ts("dve", pen[:], gmask[:], BIG, -BIG, ALU.mult, ALU.add, reads=[gmask], writes=[pen])
        for g in range(4):
            P.ts("dve", ml[:, g * 8:(g + 1) * 8], lg[:, 4 + g * 8:4 + (g + 1) * 8], pen[:, g:g + 1], None, ALU.add, reads=[lg, pen], writes=[ml])
        P.op("dve", lambda e: e.tensor_reduce(out=v1[:], in_=ml[:], axis=AX.X, op=ALU.max), [ml], [v1])
        P.ts("dve", m1[:], ml[:], v1[:, 0:1], None, ALU.is_ge, reads=[ml, v1], writes=[m1])
        P.stt("dve", ml2[:], m1[:], -BIG, ml[:], ALU.mult, ALU.add, [m1, ml], [ml2])
        P.op("dve", lambda e: e.tensor_reduce(out=v2[:], in_=ml2[:], axis=AX.X, op=ALU.max), [ml2], [v2])
        P.ts("dve", m2[:], ml2[:], v2[:, 0:1], None, ALU.is_ge, reads=[ml2, v2], writes=[m2])
        P.tt("dve", dd[:], v1[:], v2[:], ALU.subtract, [v1, v2], [dd])
        P.act(w1[:], dd[:], AF.Sigmoid, [dd], [w1])
        P.ts("dve", w2[:], w1[:], -1.0, 1.0, ALU.mult, ALU.add, reads=[w1], writes=[w2])
        P.tt("dve", g1[:], w1[:], pg[:], ALU.mult, [w1, pg], [g1])
        P.tt("dve", g2[:], w2[:], pg[:], ALU.mult, [w2, pg], [g2])
        P.ts("dve", comb[:], m1[:], g1[:, 0:1], None, ALU.mult, reads=[m1, g1], writes=[comb])
        P.ts("dve", comb2[:], m2[:], g2[:, 0:1], None, ALU.mult, reads=[m2, g2], writes=[comb2])
        P.tt("dve", comb[:], comb[:], comb2[:], ALU.add, [comb, comb2], [comb])
        pt = banks[4]
        P.tr(pt[0:32, 0:128], comb[:], ident, [comb, cst], [pt])
        P.copy("act", combT[0:32, t * 128:(t + 1) * 128], pt[0:32, 0:128], [pt], [combT])

    for blk in range(nblk):
        tok0 = blk * TB
        for g in range(16):
            st = wst[g % NWS]
            sv = st[:, 0:2 * TB].rearrange("p (a n) -> p a n", a=2)
            P.dma("sp" if g % 2 == 0 else "act", sv, oT_v[:, g * 2:(g + 1) * 2, tok0:tok0 + TB], writes=[st])
            P.copy("pool" if g % 2 == 0 else "act", actT[:, g * 2:(g + 1) * 2, :], sv, [st], [actT])
        P.dma("sp", pst[:], pT_v[:, :, tok0:tok0 + TB], writes=[pst])
        P.copy("dve", pTb[:], pst[:], [pst], [pTb])
        for ng in range(8):
            gs = slice(ng * 512, (ng + 1) * 512)
            for k2 in range(16):
                bf = wload(w_out_v[:, k2 * 2:(k2 + 1) * 2, gs], 2)
                for j in range(2):
                    kc = k2 * 2 + j
                    for t in range(NT):
                        P.mm(accA[t][:], actT[:, kc, t * 128:(t + 1) * 128], bf[:, j * 512:(j + 1) * 512], [actT, bf], [accA[t]],
                             start=(kc == 0), stop=(kc == 31))
            for t in range(NT):
                x_ = xs[t % 2]; e_ = ev[t % 2]; y = yv[t % 2]
                P.dma("act", x_[:], xres[tok0 + t * 128:tok0 + (t + 1) * 128, gs], writes=[x_])
                P.copy("act", e_[:], accA[t][:], [accA[t]], [e_])
                P.stt("dve", y[:], x_[:], ALPHA, e_[:], ALU.mult, ALU.add, [x_, e_], [y])
                P.dma("sp", Y1[t][:, gs], y[:], reads=[y], writes=[Y1[t]])
        if stage == 1:
            for t in range(NT):
                P.dma("sp", wa[:], Y1[t][:], reads=[Y1[t]], writes=[wa])
                fin.append(P.dma("sp", xo[tok0 + t * 128:tok0 + (t + 1) * 128, :], wa[:], reads=[wa]).idx)
            continue
        for t in range(NT):
            layer_norm_tile(Y1[t], 0, t, tok0)
        if stage == 2:
            for t in range(NT):
                P.dma("sp", wa[:], X1[t][:], reads=[X1[t]], writes=[wa])
                fin.append(P.dma("sp", xo[tok0 + t * 128:tok0 + (t + 1) * 128, :], wa[:], reads=[wa]).idx)
            continue
        for e in range(n_exp + 1):
            mats = [(wgate[e], 0), (wup[e], 2)] if e < n_exp else [(pw_gd, 0)]
            for (wm, b0) in mats:
                wm_v = wm.rearrange("(c p) n -> p c n", p=128)
                for k4 in range(8):
                    bf = wload(wm_v[:, k4 * 4:(k4 + 1) * 4, :], 4)
                    for j in range(4):
                        kc = k4 * 4 + j
                        for fc in range(2):
                            P.mm(accA[b0 + fc][:, 0:TB], bf[:, j * 256 + fc * 128:j * 256 + (fc + 1) * 128], actT[:, kc, :], [bf, actT], [accA[b0 + fc]],
                                 start=(kc == 0), stop=(kc == 31))
            if e == n_exp:
                for fc in range(2):
                    P.copy("act", gdT[:, fc, :], accA[fc][:, 0:TB], [accA[fc]], [gdT])
                continue
            sl = sel[e % 2]
            P.ts("pool", sl[:], ones, ident[:, e:e + 1], None, ALU.mult, reads=[cst], writes=[sl])
            pc = banks[6]
            P.mm(pc[:, 0:TB], sl[:], combT[:], [sl, combT], [pc])
            P.copy("act", cb[:], pc[:, 0:TB], [pc], [cb])
            for fc in range(2):
                s_ = sil[fc]; t_ = tmu[fc]
                P.act(s_[:], accA[fc][:, 0:TB], AF.Silu, [accA[fc]], [s_])
                P.tt("dve", t_[:], accA[2 + fc][:, 0:TB], s_[:], ALU.mult, [accA[2 + fc], s_], [t_])
                P.tt("pool", hact[:, e * 2 + fc, :], t_[:], cb[:], ALU.mult, [t_, cb], [hact])
        for dg in range(8):
            gs = slice(dg * 512, (dg + 1) * 512)
            for e in range(n_exp):
                bf = wload(wdown[e].rearrange("(c p) n -> p c n", p=128)[:, :, gs], 2)
                for fc in range(2):
                    for t in range(NT):
                        P.mm(accA[t][:], hact[:, e * 2 + fc, t * 128:(t + 1) * 128], bf[:, fc * 512:(fc + 1) * 512], [hact, bf], [accA[t]],
                             start=(e == 0 and fc == 0), stop=(e == n_exp - 1 and fc == 1))
            bfp = wload(pw_proj.rearrange("(c p) n -> p c n", p=128)[:, :, gs], 2)
            for t in range(NT):
                for fc in range(2):
                    P.mm(accB[t][:], pTb[:, fc, t * 128:(t + 1) * 128], bfp[:, fc * 512:(fc + 1) * 512], [pTb, bfp], [accB[t]], start=(fc == 0), stop=(fc == 1))
            bfg = wload(pw_gu.rearrange("(c p) n -> p c n", p=128)[:, :, gs], 2)
            for t in range(NT):
                p_ = pr[t % 2]; s_ = sg[t % 2]; x_ = xs[t % 2]; e_ = ev[t % 2]; y = yv[t % 2]
                P.copy("act", p_[:], accB[t][:], [accB[t]], [p_])
                for fc in range(2):
                    P.mm(accB[t][:], gdT[:, fc, t * 128:(t + 1) * 128], bfg[:, fc * 512:(fc + 1) * 512], [gdT, bfg], [accB[t]], start=(fc == 0), stop=(fc == 1))
                P.act(s_[:], accB[t][:], AF.Sigmoid, [accB[t]], [s_])
                P.tt("pool", p_[:], p_[:], s_[:], ALU.mult, [p_, s_], [p_])
                P.dma("act", x_[:], X1[t][:, gs], reads=[X1[t]], writes=[x_])
                P.copy("act", e_[:], accA[t][:], [accA[t]], [e_])
                P.stt("dve", y[:], x_[:], ALPHA, e_[:], ALU.mult, ALU.add, [x_, e_], [y])
                P.tt("pool", y[:], y[:], p_[:], ALU.add, [y, p_], [y])
                P.dma("sp", Y2[t][:, gs], y[:], reads=[y], writes=[Y2[t]])
        for t in range(NT):
            layer_norm_tile(Y2[t], 1, t, tok0)
    P.emit(final_wait_ops=fin)
    return nc, P


def f_inputs(inp, oT_c, xres_c, pT_c):
    lnp = np.stack([np.broadcast_to(inp[k][None, :], (128, DM)) for k in ("ln1_g", "ln1_b", "ln2_g", "ln2_b")]).astype(np.float32)
    wrt = np.ascontiguousarray(np.concatenate([inp["moe_w_grp"], inp["moe_w_rt"]], axis=1))
    brt = np.ascontiguousarray(np.broadcast_to(np.concatenate([inp["moe_b_grp"], inp["moe_b_rt"]])[None, :], (128, 36))).astype(np.float32)
    return {"oT": oT_c, "xres": xres_c, "pT": pT_c, "w_out": inp["w_out"], "lnp": np.ascontiguousarray(lnp), "wrt": wrt, "brt": brt,
            "wgate": inp["moe_w_gate"], "wup": inp["moe_w_up"], "wdown": inp["moe_w_down"],
            "pw_proj": inp["ple_w_proj"], "pw_gd": inp["ple_w_gate_down"], "pw_gu": inp["ple_w_gate_up"], "cst": CF_ARR}


NCORES = 8
_PROGS = {}


def _prog(kind):
    if kind not in _PROGS:
        nc = bass.Bass("TRN2", target_bir_lowering=False)
        if kind == "g":
            nc, _ = build_g(nc, nblk=8192 // NB, NTOK=8192)
        elif kind == "r":
            nc, _ = build_r(nc, nblk=8192 // NB, NTOK=8192)
        else:
            nc, _ = build_f(nc, NTOKC=1024, NT=4)
        _PROGS[kind] = nc
    return _PROGS[kind]


def kernel(**inputs):
    x = np.asarray(inputs["x"], dtype=np.float32)
    p = np.asarray(inputs["p"], dtype=np.float32)
    B, S, Dm = x.shape
    X = np.ascontiguousarray(x.reshape(B * S, Dm))
    cores = list(range(NCORES))
    for layer in range(4):
        inpL = {k: np.asarray(v[layer], dtype=np.float32) for k, v in inputs.items() if k not in ("x", "p")}
        xT = np.ascontiguousarray(X.T)
        res = run_bass_kernel_spmd(_prog("g"), [g_inputs(inpL, c, xT) for c in cores], core_ids=cores)
        og = [np.asarray(r["og"]) for r in res.results]
        res = run_bass_kernel_spmd(_prog("r"), [r_inputs(inpL, c, xT) for c in cores], core_ids=cores)
        orw = [np.asarray(r["orw"]) for r in res.results]
        oT = np.concatenate(og + orw, axis=0)
        del og, orw, xT
        pTl = p[layer].reshape(B * S, -1).T
        maps = []
        for c in cores:
            ts_ = slice(c * 1024, (c + 1) * 1024)
            maps.append(f_inputs(inpL, np.ascontiguousarray(oT[:, ts_]), np.ascontiguousarray(X[ts_]), np.ascontiguousarray(pTl[:, ts_])))
        res = run_bass_kernel_spmd(_prog("f"), maps, core_ids=cores)
        X = np.concatenate([np.asarray(r["xo"]) for r in res.results], axis=0)
        del maps, oT
    return np.ascontiguousarray(X.reshape(B, S, Dm)).astype(np.float32)
```
